# Optimizing a Trainium2 kernel written in Bass

```python
import math
import jax, jax.numpy as jnp
from jax import lax
import numpy as np

D_MODEL = 1024
BATCH = 8
SEQ = 4096
DEPTH = 2

EPS = 1e-6
NEG_BIG = -1e30
LB_FLOOR = 1e-20
A_HEADS = 4
A_HEAD_DIM = 64
A_KV_LATENT = 128
IDX_HEADS = 4
IDX_DIM = 64
MAX_TOPK = 256
Q_BLOCK = 128
N_BUCKETS = 32
MAX_DISTANCE = 128
B_HEADS = 4
B_DK = 64
B_DV = 64
B_GATE_RANK = 16
B_GATE_TAU = 16.0
C_WIDTH = 256
C_BLOCKS = 4
C_CONV = 4
C_EXP = 8.0
D_HEADS = 4
D_DK = 64
D_DV = 64
CHUNK = 64
N_BRANCH = 4
D_FF = 4 * D_MODEL

IN_SPLITS = (
    A_HEADS * A_HEAD_DIM, A_KV_LATENT, IDX_HEADS * IDX_DIM, IDX_DIM, IDX_HEADS,
    B_HEADS * B_DK, B_HEADS * B_DK, B_HEADS * B_DV, B_GATE_RANK, B_HEADS * B_DV,
    C_WIDTH, C_WIDTH,
    D_HEADS * D_DK, D_HEADS * D_DK, D_HEADS * D_DV, D_HEADS * D_DV,
)
IN_COLS = sum(IN_SPLITS)

kernel_name = 'hybrid_dsa_gla_rglru_hgrn2_block'


def rms_norm(x, g):
    xf = x.astype(jnp.float32)
    y = xf * lax.rsqrt(jnp.mean(xf * xf, axis=-1, keepdims=True) + EPS)
    return (y * g.astype(jnp.float32)).astype(x.dtype)


def split_heads(t, n):
    return t.reshape(t.shape[0], t.shape[1], n, -1)


def t5_bucket(dist):
    n = jnp.maximum(dist, 0)
    max_exact = N_BUCKETS // 2
    nf = jnp.maximum(n, max_exact).astype(jnp.float32)
    large = max_exact + (jnp.log(nf / max_exact) / math.log(MAX_DISTANCE / max_exact)
                         * (N_BUCKETS - max_exact)).astype(jnp.int32)
    large = jnp.minimum(large, N_BUCKETS - 1)
    return jnp.where(n < max_exact, n, large)


def dsa_sparse_attention(q, c_kv, iq, ik, iw, w_uk, w_uv, rel_bias):
    Bsz, T = q.shape[0], q.shape[1]
    topk = min(MAX_TOPK, T // 4)
    n_blk = T // Q_BLOCK
    q_lat = jnp.einsum('bthd,hdc->bthc', q, w_uk) * (A_HEAD_DIM ** -0.5)
    iw = iw * (IDX_HEADS ** -0.5 * IDX_DIM ** -0.5)
    key_pos = jnp.arange(T, dtype=jnp.int32)
    b_idx = jnp.arange(Bsz)[:, None, None]

    def to_blocks(a):
        return jnp.swapaxes(a.reshape(Bsz, n_blk, Q_BLOCK, *a.shape[2:]), 0, 1)

    def one_block(blk):
        ql, iqb, iwb, qpos = blk
        rel = jax.nn.relu(jnp.einsum('bqhd,bsd->bqhs', iqb, ik))
        score = jnp.einsum('bqhs,bqh->bqs', rel, iwb).astype(jnp.float32)
        score = jnp.where((key_pos[None, :] <= qpos[:, None])[None], score, NEG_BIG)
        _, idx = lax.top_k(score, topk)
        c_sel = c_kv[b_idx, idx]
        dist = qpos[None, :, None] - idx
        bias = jnp.swapaxes(rel_bias[t5_bucket(dist)], -1, -2).astype(jnp.float32)
        logits = jnp.einsum('bqhc,bqkc->bqhk', ql, c_sel).astype(jnp.float32) + bias
        logits = jnp.where((dist >= 0)[:, :, None, :], logits, NEG_BIG)
        p = jax.nn.softmax(logits, axis=-1).astype(c_sel.dtype)
        return jnp.einsum('bqhk,bqkc->bqhc', p, c_sel)

    o_lat = lax.map(one_block, (to_blocks(q_lat), to_blocks(iq), to_blocks(iw),
                                key_pos.reshape(n_blk, Q_BLOCK)))
    o_lat = jnp.swapaxes(o_lat, 0, 1).reshape(Bsz, T, A_HEADS, A_KV_LATENT)
    return jnp.einsum('bthc,hcd->bthd', o_lat, w_uv)


def chunked_gated_linear_attention(q, k, v, log_g):
    Bsz, T, H, dk = q.shape
    dv = v.shape[-1]
    n = T // CHUNK

    def to_chunks(a):
        return a.reshape(Bsz, n, CHUNK, H, a.shape[-1]).transpose(1, 0, 3, 2, 4)

    xs = (to_chunks(q), to_chunks(k), to_chunks(v), to_chunks(log_g.astype(jnp.float32)))
    causal = jnp.tril(jnp.ones((CHUNK, CHUNK), dtype=bool))[None, None, :, :, None]

    def step(S, inp):
        qi, ki, vi, gi = inp
        b = jnp.cumsum(gi, axis=2)
        b_last = b[:, :, -1, :]
        diff = b[:, :, :, None, :] - b[:, :, None, :, :]
        decay = jnp.where(causal, jnp.exp(jnp.where(causal, diff, 0.0)), 0.0)
        attn = jnp.einsum('bhid,bhjd,bhijd->bhij', qi, ki, decay)
        o = (jnp.einsum('bhij,bhjv->bhiv', attn, vi)
             + jnp.einsum('bhid,bhdv->bhiv', qi * jnp.exp(b), S))
        k_tail = ki * jnp.exp(b_last[:, :, None, :] - b)
        S_new = jnp.exp(b_last)[..., None] * S + jnp.einsum('bhjd,bhjv->bhdv', k_tail, vi)
        return S_new, o

    S0 = jnp.zeros((Bsz, H, dk, dv), jnp.float32)
    _, o = lax.scan(step, S0, xs)
    return o.transpose(1, 0, 3, 2, 4).reshape(Bsz, T, H, dv).astype(v.dtype)


def causal_depthwise_conv(x, w, b):
    T = x.shape[1]
    xp = jnp.pad(x, ((0, 0), (C_CONV - 1, 0), (0, 0)))
    y = xp[:, 0:T] * w[0]
    for j in range(1, C_CONV):
        y = y + xp[:, j:j + T] * w[j]
    return y + b


def block_diag_linear(x, w, b):
    xb = x.reshape(x.shape[0], x.shape[1], C_BLOCKS, -1)
    return jnp.einsum('btni,nij->btnj', xb, w).reshape(x.shape) + b


def rg_lru(x, w_a, b_a, w_x, b_x, lam):
    r = jax.nn.sigmoid(block_diag_linear(x, w_a, b_a)).astype(jnp.float32)
    i = jax.nn.sigmoid(block_diag_linear(x, w_x, b_x)).astype(jnp.float32)
    log_a = -C_EXP * r * jax.nn.softplus(-lam.astype(jnp.float32))
    a = jnp.exp(log_a)
    u = jnp.sqrt(jnp.maximum(-jnp.expm1(2.0 * log_a), 0.0)) * (i * x.astype(jnp.float32))

    def combine(c1, c2):
        a1, u1 = c1
        a2, u2 = c2
        return a1 * a2, a2 * u1 + u2

    _, h = lax.associative_scan(combine, (a, u), axis=1)
    return h.astype(x.dtype)


def setup_inputs(seed: int = 0) -> dict:
    key = jax.random.key(seed)
    ks = iter(jax.random.split(key, 40))
    f32 = jnp.float32
    L, D = DEPTH, D_MODEL
    bs = C_WIDTH // C_BLOCKS

    def nrm(shape, scale):
        return scale * jax.random.normal(next(ks), shape, f32)

    def gain(shape):
        return 1.0 + nrm(shape, 0.02)

    a_pow = jax.random.uniform(next(ks), (L, C_WIDTH), f32, 0.9, 0.999)
    a = a_pow ** (1.0 / C_EXP)
    lru_lambda = jnp.log(a) - jnp.log1p(-a)
    wa = A_HEADS * A_HEAD_DIM
    wb = B_HEADS * B_DV
    wd = D_HEADS * D_DV
    return {
        'x': nrm((BATCH, SEQ, D), 1.0),
        'norm1_g': gain((L, D)),
        'w_in': nrm((L, D, IN_COLS), D ** -0.5),
        'w_gate': nrm((L, D, N_BRANCH * D), D ** -0.5),
        'b_gate': nrm((L, N_BRANCH * D), 0.02),
        'kv_norm_g': gain((L, A_KV_LATENT)),
        'w_uk': nrm((L, A_HEADS, A_HEAD_DIM, A_KV_LATENT), A_HEAD_DIM ** -0.5),
        'w_uv': nrm((L, A_HEADS, A_KV_LATENT, A_HEAD_DIM), A_KV_LATENT ** -0.5),
        'rel_bias': nrm((N_BUCKETS, A_HEADS), 0.2),
        'w_gk2': nrm((L, B_GATE_RANK, B_HEADS * B_DK), B_GATE_RANK ** -0.5),
        'b_gk': nrm((L, B_HEADS * B_DK), 0.1),
        'gla_norm_g': gain((L, B_DV)),
        'conv_w': nrm((L, C_CONV, C_WIDTH), C_CONV ** -0.5),
        'conv_b': nrm((L, C_WIDTH), 0.02),
        'w_rg_a': nrm((L, C_BLOCKS, bs, bs), bs ** -0.5),
        'b_rg_a': nrm((L, C_WIDTH), 0.02),
        'w_rg_x': nrm((L, C_BLOCKS, bs, bs), bs ** -0.5),
        'b_rg_x': nrm((L, C_WIDTH), 0.02),
        'lru_lambda': lru_lambda,
        'lb_param': nrm((L, D_HEADS * D_DK), 0.1),
        'hgrn_norm_g': gain((L, D_DV)),
        'w_br_a': nrm((L, wa, D), wa ** -0.5),
        'w_br_b': nrm((L, wb, D), wb ** -0.5),
        'w_br_c': nrm((L, C_WIDTH, D), C_WIDTH ** -0.5),
        'w_br_d': nrm((L, wd, D), wd ** -0.5),
        'w_out': nrm((L, D, D), D ** -0.5),
        'norm2_g': gain((L, D)),
        'w_ff1': nrm((L, D, D_FF), D ** -0.5),
        'w_ff2': nrm((L, D_FF, D), D_FF ** -0.5),
        'final_norm_g': gain((D,)),
    }


def reference(x, norm1_g, w_in, w_gate, b_gate, kv_norm_g, w_uk, w_uv, rel_bias,
              w_gk2, b_gk, gla_norm_g, conv_w, conv_b, w_rg_a, b_rg_a, w_rg_x, b_rg_x,
              lru_lambda, lb_param, hgrn_norm_g, w_br_a, w_br_b, w_br_c, w_br_d,
              w_out, norm2_g, w_ff1, w_ff2, final_norm_g):
    Bsz, T, D = x.shape
    offsets = np.cumsum(IN_SPLITS)[:-1].tolist()
    lb_soft = jax.nn.softmax(lb_param.astype(jnp.float32), axis=0)
    lower_bounds = jnp.cumsum(lb_soft, axis=0) - lb_soft[0]
    for l in range(DEPTH):
        h = rms_norm(x, norm1_g[l])
        (a_q, a_ckv, a_iq, a_ik, a_iw,
         b_q, b_k, b_v, b_lr, b_r,
         c_x, c_y,
         d_q, d_f, d_i, d_g) = jnp.split(h @ w_in[l], offsets, axis=-1)

        y_a = dsa_sparse_attention(split_heads(a_q, A_HEADS), rms_norm(a_ckv, kv_norm_g[l]),
                                   split_heads(a_iq, IDX_HEADS), a_ik, a_iw,
                                   w_uk[l], w_uv[l], rel_bias).reshape(Bsz, T, -1)

        log_alpha = jax.nn.log_sigmoid((b_lr @ w_gk2[l] + b_gk[l]).astype(jnp.float32)) / B_GATE_TAU
        o_b = chunked_gated_linear_attention(split_heads(b_q, B_HEADS) * (B_DK ** -0.5),
                                             split_heads(b_k, B_HEADS), split_heads(b_v, B_HEADS),
                                             split_heads(log_alpha, B_HEADS))
        y_b = (rms_norm(o_b, gla_norm_g[l]) * jax.nn.silu(split_heads(b_r, B_HEADS))).reshape(Bsz, T, -1)

        xc = causal_depthwise_conv(c_x, conv_w[l], conv_b[l])
        y_c = rg_lru(xc, w_rg_a[l], b_rg_a[l], w_rg_x[l], b_rg_x[l], lru_lambda[l]) * jax.nn.gelu(c_y)

        lb = lower_bounds[l]
        log_f = jnp.logaddexp(jnp.log(jnp.maximum(lb, LB_FLOOR)),
                              jnp.log1p(-lb) + jax.nn.log_sigmoid(d_f.astype(jnp.float32)))
        o_d = chunked_gated_linear_attention(split_heads(jax.nn.silu(d_q), D_HEADS),
                                             split_heads(-jnp.expm1(log_f), D_HEADS),
                                             split_heads(d_i, D_HEADS), split_heads(log_f, D_HEADS))
        y_d = (rms_norm(o_d, hgrn_norm_g[l]) * jax.nn.silu(split_heads(d_g, D_HEADS))).reshape(Bsz, T, -1)

        gates = jax.nn.sigmoid(h @ w_gate[l] + b_gate[l]).reshape(Bsz, T, N_BRANCH, D)
        merged = (gates[:, :, 0] * (y_a @ w_br_a[l]) + gates[:, :, 1] * (y_b @ w_br_b[l])
                  + gates[:, :, 2] * (y_c @ w_br_c[l]) + gates[:, :, 3] * (y_d @ w_br_d[l]))
        x = x + merged @ w_out[l]

        h2 = rms_norm(x, norm2_g[l])
        x = x + jnp.square(jax.nn.relu(h2 @ w_ff1[l])) @ w_ff2[l]
    return rms_norm(x, final_norm_g)
```

```python
import numpy as np
import concourse.bass as bass
import concourse.mybir as mybir
import os
from contextlib import ExitStack
from concourse.bass_utils import run_bass_kernel_spmd

F32 = mybir.dt.float32
BF16 = mybir.dt.bfloat16
AF = mybir.ActivationFunctionType
ALU = mybir.AluOpType
AX = mybir.AxisListType

D_MODEL = 1024
DEPTH = 2
EPS = 1e-6
D_FF = 4096
IN_COLS = 3284
O_AQ, O_CKV, O_IQ, O_IK, O_IW = 0, 256, 384, 640, 704
O_BQ, O_BK, O_BV, O_BLR, O_BR = 708, 964, 1220, 1476, 1492
O_CX, O_CY = 1748, 2004
O_DQ, O_DF, O_DI, O_DG = 2260, 2516, 2772, 3028


class Sched:
    ENG = ('pe', 'dve', 'act', 'pool', 'sp')
    LOOKBACK = 3
    SEM_MAX = 30000

    def __init__(self, nc):
        self.nc = nc
        self.q = {e: [] for e in self.ENG}
        self.sem = {}
        self.cnt = {}
        self.old = {}
        self.all_sems = []
        self.nsem = 0
        for e in self.ENG:
            if e != 'sp':
                self._new_sem(e)
        self.known = {e: {} for e in self.ENG}
        self.res = {}
        self.dma_ring = []
        self.dma_i = 0
        self.NDMA = 40
        self.pend = []
        self.pend_sems = set()
        self.pend_age = 0
        self.DEFER_LOADS = 3
        self.n_ops = 0

    def _alloc(self, name):
        self.nsem += 1
        h = self.nc.alloc_semaphore(f"{name}_{self.nsem}")
        self.all_sems.append(h)
        return h

    def _new_sem(self, e):
        if e in self.sem:
            self.old.setdefault(e, []).append((self.sem[e], self.cnt[e]))
        self.sem[e] = self._alloc('s' + e)
        self.cnt[e] = 0

    def _need(self, eng, ev, waits, use_known=True):
        if ev is None:
            return
        sem, val, src = ev
        if sem.name in self.pend_sems:
            self.flush()
        if src == eng:
            if eng == 'pe':
                return
            if sem is self.sem[eng] and val <= self.cnt[eng] - self.LOOKBACK:
                return
            if sem is not self.sem[eng]:
                return
        k = self.known[eng]
        if use_known and k.get(sem.name, 0) >= val:
            return
        cur = waits.get(sem.name)
        if cur is None or cur[1] < val:
            waits[sem.name] = (sem, val)

    def _deps(self, eng, reads, writes, use_known=True):
        waits = {}
        for r in reads:
            st = self.res.get(r)
            if st is not None:
                self._need(eng, st[0], waits, use_known)
        for w in writes:
            st = self.res.get(w)
            if st is not None:
                self._need(eng, st[0], waits, use_known)
                for ev in st[1].values():
                    self._need(eng, ev, waits, use_known)
        if use_known:
            for name, (sem, val) in waits.items():
                self.known[eng][name] = val
        return list(waits.values())

    def _commit(self, eng, ev, reads, writes):
        for r in reads:
            st = self.res.setdefault(r, [None, {}])
            st[1][eng if ev[2] != 'dma' else ('dma', ev[0].name)] = ev
        for w in writes:
            self.res[w] = [ev, {}]

    def op(self, eng, fn, reads=(), writes=()):
        waits = self._deps(eng, reads, writes)
        if self.cnt[eng] >= self.SEM_MAX:
            self._new_sem(eng)
        sem = self.sem[eng]
        self.cnt[eng] += 1
        ev = (sem, self.cnt[eng], eng)
        self._commit(eng, ev, reads, writes)
        self.q[eng].append((waits, fn, sem, 1))
        self.n_ops += 1

    def flush(self):
        k = self.known['sp']
        for (waits, fn, sem, inc) in self.pend:
            w2 = []
            for (s_, v_) in waits:
                if k.get(s_.name, 0) < v_:
                    k[s_.name] = v_
                    w2.append((s_, v_))
            self.q['sp'].append((w2, fn, sem, inc))
        self.pend = []
        self.pend_sems = set()
        self.pend_age = 0

    def dma(self, eng, out, in_, reads=(), writes=(), defer=False):
        eng = 'sp'
        if len(self.dma_ring) < self.NDMA:
            self.dma_ring.append([self._alloc('d'), 0, None])
            slot = self.dma_ring[-1]
        else:
            slot = self.dma_ring[self.dma_i % self.NDMA]
        self.dma_i += 1
        if slot[0].name in self.pend_sems:
            self.flush()
        waits = self._deps(eng, reads, writes, use_known=not defer)
        if slot[2] is not None:
            w2 = {}
            self._need(eng, slot[2], w2, use_known=not defer)
            for name, (sem, val) in w2.items():
                if not defer:
                    self.known[eng][name] = val
                waits = [w for w in waits if w[0].name != name] + [(sem, val)]
        slot[1] += 16
        ev = (slot[0], slot[1], 'dma')
        slot[2] = ev
        self._commit(eng, ev, reads, writes)
        ent = (waits, lambda e, o=out, i=in_: e.dma_start(out=o, in_=i), slot[0], 16)
        if defer:
            self.pend.append(ent)
            self.pend_sems.add(slot[0].name)
        else:
            self.q[eng].append(ent)
            if self.pend:
                self.pend_age += 1
                if self.pend_age >= self.DEFER_LOADS:
                    self.flush()
        self.n_ops += 1

    def barrier(self):
        self.flush()
        for e in self.ENG:
            waits = []
            k = self.known[e]
            for f in self.ENG:
                if f == 'sp' or (f == e and e == 'pe'):
                    continue
                for (sem, c) in self.old.get(f, []) + [(self.sem[f], self.cnt[f])]:
                    if c > 0 and k.get(sem.name, 0) < c:
                        waits.append((sem, c))
                        k[sem.name] = c
            for slot in self.dma_ring:
                if slot[2] is not None and k.get(slot[0].name, 0) < slot[1]:
                    waits.append((slot[0], slot[1]))
                    k[slot[0].name] = slot[1]
            self.q[e].append((waits, None, None, 0))
        self.res = {}

    def finish(self, eng='sp'):
        self.flush()
        waits = {}
        for slot in self.dma_ring:
            if slot[2] is not None:
                self._need(eng, slot[2], waits)
        self.q[eng].append((list(waits.values()), None, None, 0))

    def emit(self):
        nc = self.nc
        q = self.q

        def run(e, lst):
            for waits, fn, sem, inc in lst:
                for (s, v) in waits:
                    e.wait_ge(s, v)
                if fn is not None:
                    ins = fn(e)
                    ins.then_inc(sem, inc)

        for h in self.all_sems:
            nc.sync.sem_clear(h)
        with nc.Block() as block:
            @block.tensor
            def _(e):
                run(e, q['pe'])

            @block.vector
            def _(e):
                run(e, q['dve'])

            @block.scalar
            def _(e):
                run(e, q['act'])

            @block.gpsimd
            def _(e):
                run(e, q['pool'])

            @block.sync
            def _(e):
                run(e, q['sp'])


class Ring:
    def __init__(self, nc, name, n, shape, dtype, psum=False):
        self.tiles = []
        for i in range(n):
            if psum:
                t = nc.alloc_psum_tensor(f"{name}{i}", shape, dtype)
            else:
                t = nc.alloc_sbuf_tensor(f"{name}{i}", shape, dtype)
            self.tiles.append((t, f"{name}{i}"))
        self.i = 0

    def next(self):
        t = self.tiles[self.i % len(self.tiles)]
        self.i += 1
        return t


NPV = 24


def pack_pvec(inp, l):
    def c2(v):
        return np.ascontiguousarray(np.asarray(v, np.float32).reshape(2, 128).T)
    cols = [c2(inp['b_gk'][l]), c2(inp['lb_param'][0]), c2(inp['lb_param'][1]), c2(inp['conv_b'][l])]
    for j in range(4):
        cols.append(c2(inp['conv_w'][l][j]))
    cols += [c2(inp['b_rg_a'][l]), c2(inp['b_rg_x'][l]), c2(inp['lru_lambda'][l])]
    pv = np.concatenate(cols, axis=1)
    out = np.zeros((128, NPV), np.float32)
    out[:, :pv.shape[1]] = pv
    return out


BV_N1, BV_KV, BV_GLA, BV_HG, BV_BG, BV_N2, BV_FN = 0, 1024, 1152, 1408, 1664, 5760, 6784
NBV = 7808


def pack_bvec(inp, l):
    v = np.concatenate([
        inp['norm1_g'][l], inp['kv_norm_g'][l], np.tile(inp['gla_norm_g'][l], 4),
        np.tile(inp['hgrn_norm_g'][l], 4), inp['b_gate'][l], inp['norm2_g'][l], inp['final_norm_g']
    ]).astype(np.float32)
    assert v.shape[0] == NBV
    return v.reshape(1, NBV)


class Prog:
    def __init__(self, T, depth=DEPTH, debug=()):
        self.T = T
        self.NT = T // 128
        self.depth = depth
        self.debug = set(debug)
        nc = self.nc = bass.Bass("TRN2", target_bir_lowering=False)
        self.S = Sched(nc)
        self.uid = 0
        dt = nc.dram_tensor
        self.x_in = dt("x", [T, D_MODEL], F32, kind="ExternalInput").ap()
        self.out = dt("out", [T, D_MODEL], F32, kind="ExternalOutput").ap()
        self.w = []
        for l in range(depth):
            d = {}
            for name, shape in [('w_in', [D_MODEL, IN_COLS]), ('w_gate', [D_MODEL, 4096]),
                                ('w_uk', [4, 64, 128]), ('w_uv', [4, 128, 64]), ('w_gk2', [16, 256]),
                                ('w_rg_a', [4, 64, 64]), ('w_rg_x', [4, 64, 64]),
                                ('w_br_a', [256, 1024]), ('w_br_b', [256, 1024]), ('w_br_c', [256, 1024]),
                                ('w_br_d', [256, 1024]), ('w_out', [1024, 1024]), ('w_ff1', [1024, 4096]),
                                ('w_ff2', [4096, 1024]), ('pvec', [128, NPV]), ('bvec', [1, NBV])]:
                d[name] = dt(f"{name}{l}", shape, F32, kind="ExternalInput").ap()
            self.w.append(d)
        self.biasT = dt("biasT", [128, 4, 256], F32, kind="ExternalInput").ap()
        self.scr = {}

    def scratch(self, name, shape, dtype):
        kind = "ExternalOutput" if name in self.debug else "Internal"
        t = self.nc.dram_tensor(name, shape, dtype, kind=kind).ap()
        self.scr[name] = t
        return t

    def u(self, p):
        self.uid += 1
        return f"{p}{self.uid}"

    def act(self, out, in_, func, reads, writes, bias=0.0, scale=1.0, accum=None):
        kw = {}
        if accum is not None:
            kw['accum_out'] = accum
        self.S.op('act', lambda e: e.activation(out=out, in_=in_, func=func, bias=bias, scale=scale, **kw),
                  reads, writes)

    def mm(self, out, lhsT, rhs, start, stop, reads, writes):
        self.S.op('pe', lambda e: e.matmul(out, lhsT, rhs, start=start, stop=stop), reads, writes)

    def tr(self, out, in_, ident, reads, writes):
        self.S.op('pe', lambda e: e.transpose(out, in_, ident), reads, writes)

    def tt(self, eng, out, in0, in1, op, reads, writes):
        self.S.op(eng, lambda e: e.tensor_tensor(out=out, in0=in0, in1=in1, op=op), reads, writes)

    def ts(self, eng, out, in0, s1, s2, op0, op1, reads, writes, accum=None):
        if op1 is None:
            self.S.op(eng, lambda e: e.tensor_scalar(out=out, in0=in0, scalar1=s1, scalar2=None, op0=op0),
                      reads, writes)
        elif accum is not None:
            self.S.op(eng, lambda e: e.tensor_scalar(out=out, in0=in0, scalar1=s1, scalar2=s2, op0=op0, op1=op1,
                                                     accum_out=accum), reads, writes)
        else:
            self.S.op(eng, lambda e: e.tensor_scalar(out=out, in0=in0, scalar1=s1, scalar2=s2, op0=op0, op1=op1),
                      reads, writes)

    def stt(self, out, in0, scalar, in1, op0, op1, reads, writes):
        self.S.op('dve', lambda e: e.scalar_tensor_tensor(out=out, in0=in0, scalar=scalar, in1=in1, op0=op0, op1=op1),
                  reads, writes)

    def cp(self, eng, out, in_, reads, writes):
        if eng == 'act':
            self.S.op('act', lambda e: e.copy(out=out, in_=in_), reads, writes)
        else:
            self.S.op(eng, lambda e: e.tensor_copy(out=out, in_=in_), reads, writes)

    def load_cast(self, ring, dst, dkey, src, shape, engs=('pool', 'dve'), pbase=0):
        st, sk = ring.next()
        if len(shape) == 1:
            sv = st[:, 0:shape[0]]
        else:
            n = shape[0] * shape[1]
            sv = st[:, 0:n].rearrange("p (a b) -> p a b", a=shape[0])
        np_ = dst.shape[0]
        self.S.dma('sp', sv[pbase:pbase + np_], src, writes=[sk])
        self.cast_i = getattr(self, 'cast_i', 0) + 1
        self.cp(engs[self.cast_i % len(engs)], dst, sv[pbase:pbase + np_], [sk], [dkey])

    def alloc_scratch(self):
        T = self.T
        self.hT_d = self.scratch("hT_d", [128, 8, T], BF16)
        self.fmf_d = self.scratch("fmf_d", [128, 16, T], F32)
        self.fmb_d = self.scratch("fmb_d", [128, 7, T], BF16)
        self.tmb_d = self.scratch("tmb_d", [T, 641], BF16)
        self.tmf_d = self.scratch("tmf_d", [T, 512], F32)
        self.ckvT_d = self.scratch("ckvT_d", [128, T], BF16)
        self.iw_d = self.scratch("iw_d", [128, self.NT, 4], F32)
        self.yT_d = self.scratch("yT_d", [128, 8, T], BF16)
        self.xmid_d = self.scratch("xmid_d", [T, D_MODEL], F32)
        self.xres_d = self.scratch("xres_d", [T, D_MODEL], F32)

    def consts(self, es):
        nc, S = self.nc, self.S
        self.ident_in = nc.dram_tensor("ident", [128, 128], F32, kind="ExternalInput").ap()
        self.cmask_in = nc.dram_tensor("cmask", [128, 128], F32, kind="ExternalInput").ap()
        self.bdmask_in = nc.dram_tensor("bdmask", [128, 128], F32, kind="ExternalInput").ap()
        self.scanm_in = nc.dram_tensor("scanm", [128, 128], F32, kind="ExternalInput").ap()
        self.cneg_in = nc.dram_tensor("cneg", [128, 128], F32, kind="ExternalInput").ap()
        self.relb_in = nc.dram_tensor("relb", [32, 4], F32, kind="ExternalInput").ap()
        self.identb = es.enter_context(nc.sbuf_tensor("identb", [128, 128], BF16))
        self.identf = es.enter_context(nc.sbuf_tensor("identf", [128, 128], F32))
        S.dma('sp', self.identf[:], self.ident_in, writes=['identf'])
        self.cp('dve', self.identb[:], self.identf[:], ['identf'], ['identb'])

    def phase1(self, l, x_src):
        nc, S, T = self.nc, self.S, self.T
        W = self.w[l]
        with ExitStack() as es:
            def sb(name, shape, dt):
                return es.enter_context(nc.sbuf_tensor(self.u(name), shape, dt))

            def pt(name, shape, dt):
                return es.enter_context(nc.psum_tensor(self.u(name), shape, dt))

            def ring(name, n, shape, dt, psum=False):
                tiles = []
                for i in range(n):
                    nm = self.u(name)
                    t = pt(nm, shape, dt) if psum else sb(nm, shape, dt)
                    tiles.append((t, nm))
                r = Ring.__new__(Ring)
                r.tiles, r.i = tiles, 0
                return r

            wfm = sb('wfm', [128, 8, 2304], BF16)
            wtm = sb('wtm', [128, 8, 1156], BF16)
            wuk = sb('wuk', [128, 2, 128], BF16)
            wgk = sb('wgk', [16, 256], BF16)
            pv = sb('pv', [128, NPV], F32)
            lbc = sb('lbc', [128, 8], F32)
            nbgk = sb('nbgk', [128, 2], F32)
            g1b = sb('g1b', [128, 1024], F32)
            gkvb = sb('gkvb', [128, 128], F32)
            iw_all = sb('iw_all', [128, self.NT, 4], F32)
            stage = ring('stg', 3, [128, 2304], F32)

            S.dma('sp', pv[:], W['pvec'], writes=['pv'])
            S.dma('sp', g1b[:], W['bvec'][:, BV_N1:BV_N1 + 1024].partition_broadcast(128), writes=['g1b'])
            S.dma('sp', gkvb[:], W['bvec'][:, BV_KV:BV_KV + 128].partition_broadcast(128), writes=['gkvb'])
            if l == 0:
                S.op('dve', lambda e: e.memset(lbc[:, 0:2], 0.0), [], ['lbc'])
            else:
                self.tt('dve', lbc[:, 6:8], pv[:, 4:6], pv[:, 2:4], ALU.subtract, ['pv'], ['lbc'])
                self.act(lbc[:, 0:2], lbc[:, 6:8], AF.Sigmoid, ['lbc'], ['lbc'])
            self.ts('dve', lbc[:, 2:4], lbc[:, 0:2], -1.0, 1.0, ALU.mult, ALU.add, ['lbc'], ['lbc'])
            self.ts('dve', lbc[:, 4:6], lbc[:, 0:2], 1e-20, None, ALU.max, None, ['lbc'], ['lbc'])
            self.ts('dve', nbgk[:], pv[:, 0:2], -1.0, None, ALU.mult, None, ['pv'], ['nbgk'])

            fm_src = [(O_AQ, 256), (O_IQ, 256), (O_IK, 64), (O_IK, 64), (O_BQ, 256), (O_BK, 256), (O_BLR, 16),
                      (O_CX, 256), (O_CY, 256), (O_DQ, 256), (O_DF, 256)]
            fm_dst = [0, 256, 512, 576, 640, 896, 1152, 1280, 1536, 1792, 2048]
            tm_src = [(O_CKV, 128), (O_IW, 4), (O_BV, 256), (O_BR, 256), (O_DI, 256), (O_DG, 256)]
            tm_dst = [0, 128, 132, 388, 644, 900]
            for k in range(8):
                for (so, n), do in zip(fm_src, fm_dst):
                    self.load_cast(stage, wfm[:, k, do:do + n], ('wfm', k), W['w_in'][k * 128:(k + 1) * 128, so:so + n], [n])
                for (so, n), do in zip(tm_src, tm_dst):
                    self.load_cast(stage, wtm[:, k, do:do + n], ('wtm', k), W['w_in'][k * 128:(k + 1) * 128, so:so + n], [n])
            self.load_cast(stage, wuk[:], 'wuk', W['w_uk'].rearrange("(hp e) d c -> (e d) hp c", e=2), [2, 128])
            self.load_cast(stage, wgk[:], 'wgk', W['w_gk2'], [256])

            xr = ring('xt', 3, [128, 1024], F32)
            junk = sb('junk', [128, 1024], F32)
            hr = ring('h', 2, [128, 1024], BF16)
            hTr = ring('hT', 2, [128, 8, 512], BF16)
            st1 = ring('st1', 4, [128, 4], F32)
            ptr = ring('ptr', 2, [128, 8, 128], BF16, psum=True)
            psr = ring('ps', 4, [128, 512], F32, psum=True)
            ptc = ring('ptc', 1, [128, 128], BF16, psum=True)
            tmbr = ring('tmb', 3, [128, 641], BF16)
            tmfr = ring('tmf', 3, [128, 512], F32)
            ckvTr = ring('ckvT', 2, [128, 512], BF16)
            qTr = ring('qT', 2, [128, 2, 512], BF16)
            blrr = ring('blr', 2, [16, 512], BF16)
            ofr = ring('of', 6, [128, 512], F32)
            obr = ring('ob', 4, [128, 512], BF16)
            for t_, _k in tmbr.tiles:
                S.op('pool', lambda e, t_=t_: e.memset(t_[:, 128:129], 1.0), [], [_k])

            for st_i in range(T // 512):
                tok0 = st_i * 512
                hT, hTk = hTr.next()
                ckvT, ckvTk = ckvTr.next()
                for j in range(4):
                    t0 = tok0 + j * 128
                    tt_i = t0 // 128
                    xt, xk = xr.next()
                    S.dma('sp', xt[:], x_src[t0:t0 + 128, :], writes=[xk])
                    s1, s1k = st1.next()
                    self.act(junk[:], xt[:], AF.Square, [xk], ['junk', s1k], accum=s1[:, 0:1])
                    self.act(s1[:, 1:2], s1[:, 0:1], AF.Sqrt, [s1k], [s1k], bias=EPS, scale=1.0 / 1024)
                    S.op('dve', lambda e, s1=s1: e.reciprocal(out=s1[:, 2:3], in_=s1[:, 1:2]), [s1k], [s1k])
                    h, hk = hr.next()
                    self.stt(h[:], xt[:], s1[:, 2:3], g1b[:], ALU.mult, ALU.mult, [xk, s1k, 'g1b'], [hk])
                    p, pk = ptr.next()
                    for k in range(8):
                        self.tr(p[:, k, :], h[:, k * 128:(k + 1) * 128], self.identb[:], [hk, 'identb'], [pk])
                    self.cp('act', hT[:, :, j * 128:(j + 1) * 128], p[:], [pk], [hTk])
                    tmb, tmbk = tmbr.next()
                    tmf, tmfk = tmfr.next()
                    pa, pak = psr.next()
                    for k in range(8):
                        self.mm(pa[:, 0:388], hT[:, k, j * 128:(j + 1) * 128], wtm[:, k, 0:388], k == 0, k == 7,
                                [hTk, ('wtm', k)], [pak])
                    s2, s2k = st1.next()
                    self.act(junk[:, 0:128], pa[:, 0:128], AF.Square, [pak], ['junk', s2k], accum=s2[:, 0:1])
                    self.act(s2[:, 1:2], s2[:, 0:1], AF.Sqrt, [s2k], [s2k], bias=EPS, scale=1.0 / 128)
                    S.op('dve', lambda e, s2=s2: e.reciprocal(out=s2[:, 2:3], in_=s2[:, 1:2]), [s2k], [s2k])
                    self.stt(tmb[:, 0:128], pa[:, 0:128], s2[:, 2:3], gkvb[:], ALU.mult, ALU.mult,
                             [pak, s2k, 'gkvb'], [tmbk])
                    self.ts('dve', iw_all[:, tt_i, :], pa[:, 128:132], 1.0 / 16, None, ALU.mult, None, [pak], ['iw_all'])
                    self.cp('act', tmb[:, 129:385], pa[:, 132:388], [pak], [tmbk])
                    pc, pck = ptc.next()
                    self.tr(pc[:], tmb[:, 0:128], self.identb[:], [tmbk, 'identb'], [pck])
                    self.cp('dve', ckvT[:, j * 128:(j + 1) * 128], pc[:], [pck], [ckvTk])
                    pb, pbk = psr.next()
                    for k in range(8):
                        self.mm(pb[:, 0:512], hT[:, k, j * 128:(j + 1) * 128], wtm[:, k, 388:900], k == 0, k == 7,
                                [hTk, ('wtm', k)], [pbk])
                    self.act(tmf[:, 0:256], pb[:, 0:256], AF.Silu, [pbk], [tmfk])
                    self.cp('dve', tmb[:, 385:641], pb[:, 256:512], [pbk], [tmbk])
                    pcc, pcck = psr.next()
                    for k in range(8):
                        self.mm(pcc[:, 0:256], hT[:, k, j * 128:(j + 1) * 128], wtm[:, k, 900:1156], k == 0, k == 7,
                                [hTk, ('wtm', k)], [pcck])
                    self.act(tmf[:, 256:512], pcc[:, 0:256], AF.Silu, [pcck], [tmfk])
                    S.dma('sp', self.tmb_d[t0:t0 + 128, :], tmb[:], reads=[tmbk], defer=True)
                    S.dma('sp', self.tmf_d[t0:t0 + 128, :], tmf[:], reads=[tmfk], defer=True)
                S.dma('sp', self.hT_d[:, :, tok0:tok0 + 512], hT[:], reads=[hTk], defer=True)
                S.dma('sp', self.ckvT_d[:, tok0:tok0 + 512], ckvT[:], reads=[ckvTk], defer=True)

                def fm_block(blk, M=128):
                    pf, pfk = psr.next()
                    for k in range(8):
                        self.mm(pf[0:M, :], wfm[:, k, blk * 128:blk * 128 + M], hT[:, k, :], k == 0, k == 7,
                                [hTk, ('wfm', k)], [pfk])
                    return pf, pfk

                def out_f(slot):
                    o, ok = ofr.next()
                    return o, ok, self.fmf_d[:, slot, tok0:tok0 + 512]

                def out_b(slot):
                    o, ok = obr.next()
                    return o, ok, self.fmb_d[:, slot, tok0:tok0 + 512]

                qT, qTk = qTr.next()
                for hp in range(2):
                    pf, pfk = fm_block(0 + hp)
                    self.cp('act', qT[:, hp, :], pf[:], [pfk], [qTk])
                for h in range(4):
                    hp, e_ = h // 2, h % 2
                    pf, pfk = psr.next()
                    self.mm(pf[:], wuk[e_ * 64:(e_ + 1) * 64, hp, :], qT[e_ * 64:(e_ + 1) * 64, hp, :], True, True,
                            ['wuk', qTk], [pfk])
                    o, ok, dst = out_b(3 + h)
                    self.ts('dve', o[:], pf[:], 0.125, None, ALU.mult, None, [pfk], [ok])
                    S.dma('sp', dst, o[:], reads=[ok], defer=True)
                for hp in range(2):
                    pf, pfk = fm_block(2 + hp)
                    o, ok, dst = out_b(0 + hp)
                    self.cp('act', o[:], pf[:], [pfk], [ok])
                    S.dma('sp', dst, o[:], reads=[ok], defer=True)
                pf, pfk = fm_block(4)
                o, ok, dst = out_b(2)
                self.cp('dve', o[:], pf[:], [pfk], [ok])
                S.dma('sp', dst, o[:], reads=[ok], defer=True)
                for hp in range(2):
                    pf, pfk = fm_block(5 + hp)
                    o, ok, dst = out_f(0 + hp)
                    self.ts('dve', o[:], pf[:], 0.125, None, ALU.mult, None, [pfk], [ok])
                    S.dma('sp', dst, o[:], reads=[ok], defer=True)
                for hp in range(2):
                    pf, pfk = fm_block(7 + hp)
                    o, ok, dst = out_f(2 + hp)
                    self.cp('act', o[:], pf[:], [pfk], [ok])
                    S.dma('sp', dst, o[:], reads=[ok], defer=True)
                pf, pfk = fm_block(9, M=16)
                blr, blrk = blrr.next()
                self.cp('dve', blr[:], pf[0:16, :], [pfk], [blrk])
                for hp in range(2):
                    pg, pgk = psr.next()
                    self.mm(pg[:], wgk[0:16, hp * 128:(hp + 1) * 128], blr[0:16, :], True, True, ['wgk', blrk], [pgk])
                    o, ok, dst = out_f(4 + hp)
                    self.act(o[:], pg[:], AF.Exp, [pgk, 'nbgk'], [ok], bias=nbgk[:, hp:hp + 1], scale=-1.0)
                    self.act(o[:], o[:], AF.Ln, [ok], [ok], bias=1.0)
                    self.ts('dve', o[:], o[:], -1.0 / 16, None, ALU.mult, None, [ok], [ok])
                    S.dma('sp', dst, o[:], reads=[ok], defer=True)
                for i_, slot0 in ((10, 6), (12, 8)):
                    for hp in range(2):
                        pf, pfk = fm_block(i_ + hp)
                        o, ok, dst = out_f(slot0 + hp)
                        self.cp('act' if hp else 'dve', o[:], pf[:], [pfk], [ok])
                        S.dma('sp', dst, o[:], reads=[ok], defer=True)
                for hp in range(2):
                    pf, pfk = fm_block(14 + hp)
                    o, ok, dst = out_f(10 + hp)
                    self.act(o[:], pf[:], AF.Silu, [pfk], [ok])
                    S.dma('sp', dst, o[:], reads=[ok], defer=True)
                for hp in range(2):
                    pf, pfk = fm_block(16 + hp)
                    o, ok, dst = out_f(12 + hp)
                    self.act(o[:], pf[:], AF.Sigmoid, [pfk], [ok], scale=-1.0)
                    self.ts('dve', o[:], o[:], lbc[:, 2 + hp:3 + hp], None, ALU.mult, None, [ok, 'lbc'], [ok])
                    S.dma('sp', dst, o[:], reads=[ok], defer=True)
                    o2, ok2, dst2 = out_f(14 + hp)
                    self.act(o2[:], pf[:], AF.Sigmoid, [pfk], [ok2])
                    self.ts('dve', o2[:], o2[:], lbc[:, 2 + hp:3 + hp], lbc[:, 4 + hp:5 + hp], ALU.mult, ALU.add,
                            [ok2, 'lbc'], [ok2])
                    self.act(o2[:], o2[:], AF.Ln, [ok2], [ok2])
                    S.dma('sp', dst2, o2[:], reads=[ok2], defer=True)
            S.dma('sp', self.iw_d, iw_all[:], reads=['iw_all'], defer=True)
            S.barrier()

    def build(self, phases=None):
        self.es = ExitStack()
        self.alloc_scratch()
        self.consts(self.es)
        self.S.barrier()
        x_src = self.x_in
        for l in range(self.depth):
            last = l == self.depth - 1
            def on(p):
                return phases is None or p in phases
            if on('p1'):
                self.phase1(l, x_src)
            if on('dsa'):
                self.phase_dsa(l)
            if on('gla'):
                self.phase_gla(l, 1)
            if on('lru'):
                self.phase_lru(l)
            if on('hgrn'):
                self.phase_gla(l, 3)
            if on('merge'):
                self.phase_merge(l, x_src)
            if on('ffn'):
                self.phase_ffn(l, self.out if last else self.xres_d, last)
            x_src = self.xres_d
        self.S.finish('sp')
        self.S.emit()
        self.es.close()
        return self.nc


def const_inputs():
    ident = np.eye(128, dtype=np.float32)
    j = np.arange(128)[:, None]
    i = np.arange(128)[None, :]
    cmask = (j <= i).astype(np.float32)
    bdmask = ((j <= i) & (j // 64 == i // 64)).astype(np.float32)
    scanm = np.ones((128, 128), np.float32)
    scanm[:, 0] = 0
    scanm[:, 64] = 0
    cneg = np.where(i <= j, 0.0, -1e30).astype(np.float32)
    return {'ident': ident, 'cmask': cmask, 'bdmask': bdmask, 'scanm': scanm, 'cneg': cneg}


def t5_bucket_np(dist):
    n = np.maximum(dist, 0)
    nf = np.maximum(n, 16).astype(np.float32)
    large = 16 + (np.log(nf / np.float32(16)) / np.float32(np.log(128 / 16)) * np.float32(16)).astype(np.int32)
    large = np.minimum(large, 31)
    return np.where(n < 16, n, large)


def bias_table(rel_bias):
    kk = np.arange(128)[:, None]
    qq = np.arange(128)[None, :]
    bp = t5_bucket_np(qq - kk + 128)
    bd = t5_bucket_np(qq - kk)
    idx = np.concatenate([bp, bd], axis=1)
    rb = np.asarray(rel_bias, np.float32)
    return np.ascontiguousarray(rb[idx].transpose(0, 2, 1))


def make_in_map(inp, b, depth=DEPTH):
    m = {'x': np.ascontiguousarray(inp['x'][b], dtype=np.float32)}
    for l in range(depth):
        for name in ('w_in', 'w_gate', 'w_uk', 'w_uv', 'w_gk2', 'w_rg_a', 'w_rg_x', 'w_br_a', 'w_br_b', 'w_br_c',
                     'w_br_d', 'w_out', 'w_ff1', 'w_ff2'):
            m[f"{name}{l}"] = np.ascontiguousarray(inp[name][l], dtype=np.float32)
        m[f"pvec{l}"] = pack_pvec(inp, l)
        m[f"bvec{l}"] = pack_bvec(inp, l)
    m['biasT'] = bias_table(inp['rel_bias'])
    m['relb'] = np.ascontiguousarray(inp['rel_bias'], dtype=np.float32)
    m.update(const_inputs())
    return m


def _phase_tools(self, es):
    nc = self.nc

    def sb(name, shape, dt):
        return es.enter_context(nc.sbuf_tensor(self.u(name), shape, dt))

    def pt(name, shape, dt):
        return es.enter_context(nc.psum_tensor(self.u(name), shape, dt))

    def ring(name, n, shape, dt, psum=False):
        tiles = []
        for i in range(n):
            nm = self.u(name)
            t = pt(nm, shape, dt) if psum else sb(nm, shape, dt)
            tiles.append((t, nm))
        r = Ring.__new__(Ring)
        r.tiles, r.i = tiles, 0
        return r
    return sb, pt, ring


NBIS = 18


def phase_dsa(self, l):
    nc, S, T, NT = self.nc, self.S, self.T, self.NT
    W = self.w[l]
    topk = min(256, T // 4)
    with ExitStack() as es:
        sb, pt, ring = _phase_tools(self, es)
        ckvT = sb('ckvT', [128, T], BF16)
        ckv1 = sb('ckv1', [128, NT, 129], BF16)
        ikT = sb('ikT', [128, T], BF16)
        iw = sb('iw', [128, NT, 4], F32)
        wuv = sb('wuv', [128, 4, 64], BF16)
        I4 = sb('I4', [128, 4, 128], BF16)
        cneg = sb('cneg', [128, 128], F32)
        bT = sb('bT', [128, 4, 256], F32)
        bhi = sb('bhi', [128, 4, 256], BF16)
        blo = sb('blo', [128, 4, 256], BF16)
        bhf = sb('bhf', [128, 4, 256], F32)
        b31 = sb('b31', [128, 4], F32)
        stage = ring('stg', 2, [128, 256], F32)
        S.dma('sp', ckvT[:], self.ckvT_d, writes=['ckvT'])
        S.dma('sp', ckv1[:], self.tmb_d[:, 0:129].rearrange("(t p) c -> p t c", p=128), writes=['ckv1'])
        S.dma('sp', ikT[:], self.fmb_d[:, 2, :], writes=['ikT'])
        S.dma('sp', iw[:], self.iw_d, writes=['iw'])
        S.dma('sp', cneg[:], self.cneg_in, writes=['cneg'])
        S.dma('sp', bT[:], self.biasT, writes=['bT'])
        S.dma('sp', b31[:], self.relb_in[31:32, :].partition_broadcast(128), writes=['b31'])
        self.load_cast(stage, wuv[:], 'wuv', W['w_uv'].rearrange("h c d -> c h d"), [4, 64])
        for h in range(4):
            self.cp('pool', I4[:, h, :], self.identb[:], ['identb'], ['I4'])
            self.ts('dve', bT[:, h, :], bT[:, h, :], b31[:, h:h + 1], None, ALU.subtract, None, ['bT', 'b31'], ['bT'])
        self.cp('dve', bhi[:], bT[:], ['bT'], ['bhi'])
        self.cp('dve', bhf[:], bhi[:], ['bhi'], ['bhf'])
        self.tt('dve', bhf[:], bT[:], bhf[:], ALU.subtract, ['bT', 'bhf'], ['bhf'])
        self.cp('dve', blo[:], bhf[:], ['bhf'], ['blo'])

        iqr = ring('iq', 2, [128, 2, 128], BF16)
        qlr = ring('ql', 2, [128, 4, 128], BF16)
        scr = ring('sc', 2, [128, T], F32)
        mnr = ring('mn', 2, [128, T], BF16)
        junk = sb('junkb', [128, T], BF16)
        rlr = ring('rl', 3, [128, 512], F32)
        bs = ring('bs', 2, [128, 8], F32)
        pss = ring('pss', 2, [128, 512], F32, psum=True)
        psl = ring('psl', 2, [128, 512], F32, psum=True)
        poA = ring('poA', 1, [128, 2, 129], F32, psum=True)
        poB = ring('poB', 1, [128, 2, 129], F32, psum=True)
        ptr = ring('ptr', 1, [128, 4, 128], BF16, psum=True)
        pyr = ring('py', 1, [128, 2, 128], F32, psum=True)
        pr_ = ring('p', 3, [128, 512], BF16)
        olr = ring('ol', 2, [128, 4, 128], BF16)
        olTr = ring('olT', 2, [128, 4, 128], BF16)
        yr = ring('ya', 2, [128, 2, 128], BF16)
        rdr = ring('rd', 2, [128, 4], F32)

        for qi in range(NT):
            q0 = qi * 128
            nk = (qi + 1) * 128
            iq, iqk = iqr.next()
            ql, qlk = qlr.next()
            S.dma('sp', iq[:], self.fmb_d[:, 0:2, q0:q0 + 128], writes=[iqk])
            S.dma('sp', ql[:], self.fmb_d[:, 3:7, q0:q0 + 128], writes=[qlk])
            sc, sck = scr.next()
            mn, mnk = mnr.next()
            for k0 in range(0, nk, 512):
                n = min(512, nk - k0)
                for h in range(4):
                    hp, e_ = h // 2, h % 2
                    ps, psk = pss.next()
                    self.mm(ps[:, 0:n], iq[e_ * 64:(e_ + 1) * 64, hp, :], ikT[e_ * 64:(e_ + 1) * 64, k0:k0 + n],
                            True, True, [iqk, 'ikT'], [psk])
                    if h == 0:
                        self.ts('dve', sc[:, k0:k0 + n], ps[:, 0:n], 0.0, iw[:, qi, 0:1], ALU.max, ALU.mult,
                                [psk, 'iw'], [sck])
                    else:
                        rl, rlk = rlr.next()
                        self.act(rl[:, 0:n], ps[:, 0:n], AF.Relu, [psk], [rlk])
                        self.stt(sc[:, k0:k0 + n], rl[:, 0:n], iw[:, qi, h:h + 1], sc[:, k0:k0 + n], ALU.mult, ALU.add,
                                 [rlk, 'iw', sck], [sck])
            self.tt('dve', sc[:, q0:q0 + 128], sc[:, q0:q0 + 128], cneg[:], ALU.add, [sck, 'cneg'], [sck])
            b, bk = bs.next()
            if nk <= topk:
                S.op('dve', lambda e, b=b: e.memset(b[:, 0:1], -1e29), [], [bk])
            else:
                S.op('dve', lambda e, b=b, sc=sc, nk=nk: e.tensor_reduce(out=b[:, 1:2], in_=sc[:, 0:nk], axis=AX.X, op=ALU.max),
                     [sck], [bk])
                self.ts('dve', b[:, 0:1], b[:, 1:2], -40.0, None, ALU.add, None, [bk], [bk])
                for it in range(NBIS):
                    self.tt('dve', b[:, 2:3], b[:, 0:1], b[:, 1:2], ALU.add, [bk], [bk])
                    self.ts('dve', b[:, 2:3], b[:, 2:3], 0.5, None, ALU.mult, None, [bk], [bk])
                    self.ts('dve', junk[:, 0:nk], sc[:, 0:nk], b[:, 2:3], 0.0, ALU.is_ge, ALU.add, [sck, bk], ['junkb', bk],
                            accum=b[:, 3:4])
                    self.ts('dve', b[:, 4:5], b[:, 3:4], float(topk), None, ALU.is_ge, None, [bk], [bk])
                    self.tt('dve', b[:, 5:6], b[:, 2:3], b[:, 0:1], ALU.subtract, [bk], [bk])
                    self.stt(b[:, 0:1], b[:, 5:6], b[:, 4:5], b[:, 0:1], ALU.mult, ALU.add, [bk], [bk])
                    self.tt('dve', b[:, 5:6], b[:, 1:2], b[:, 2:3], ALU.subtract, [bk], [bk])
                    self.stt(b[:, 1:2], b[:, 5:6], b[:, 4:5], b[:, 2:3], ALU.mult, ALU.add, [bk], [bk])
            self.ts('dve', mn[:, 0:nk], sc[:, 0:nk], b[:, 0:1], -30000.0, ALU.is_lt, ALU.mult, [sck, bk], [mnk])
            pa, pak = poA.next()
            pb, pbk = poB.next()
            qlf = ql[:].rearrange("p h q -> p (h q)")
            for kb in range(qi + 1):
                pl, plk = psl.next()
                near = kb >= qi - 1
                self.mm(pl[:], ckvT[:, kb * 128:(kb + 1) * 128], qlf, True, False, ['ckvT', qlk], [plk])
                self.mm(pl[:], mn[:, kb * 128:(kb + 1) * 128], I4[:].rearrange("p h q -> p (h q)"), False, not near,
                        [mnk, 'I4'], [plk])
                if near:
                    o_ = 128 if kb == qi else 0
                    for bt_, bkey, last in ((bhi, 'bhi', False), (blo, 'blo', True)):
                        for h in range(4):
                            self.mm(pl[:, h * 128:(h + 1) * 128], self.identb[:], bt_[:, h, o_:o_ + 128], False,
                                    last and h == 3, ['identb', bkey], [plk])
                p, pk = pr_.next()
                self.act(p[:], pl[:], AF.Exp, [plk], [pk])
                for h in range(4):
                    po, pok = (pa, pak) if h < 2 else (pb, pbk)
                    self.mm(po[:, h % 2, :], p[:, h * 128:(h + 1) * 128], ckv1[:, kb, :], kb == 0 and h % 2 == 0, kb == qi,
                            [pk, 'ckv1'], [pok])
            rd, rdk = rdr.next()
            ol, olk = olr.next()
            for h in range(4):
                po, pok = (pa, pak) if h < 2 else (pb, pbk)
                S.op('dve', lambda e, rd=rd, po=po, h=h: e.reciprocal(out=rd[:, h:h + 1], in_=po[:, h % 2, 128:129]),
                     [pok], [rdk])
                self.ts('dve', ol[:, h, :], po[:, h % 2, 0:128], rd[:, h:h + 1], None, ALU.mult, None, [pok, rdk], [olk])
            ptt, pttk = ptr.next()
            for h in range(4):
                self.tr(ptt[:, h, :], ol[:, h, :], self.identb[:], [olk, 'identb'], [pttk])
            olT, olTk = olTr.next()
            self.cp('act', olT[:], ptt[:], [pttk], [olTk])
            py, pyk = pyr.next()
            for h in range(4):
                hp, e_ = h // 2, h % 2
                self.mm(py[e_ * 64:(e_ + 1) * 64, hp, :], wuv[:, h, :], olT[:, h, :], True, True, ['wuv', olTk], [pyk])
            ya, yak = yr.next()
            self.cp('act', ya[:], py[:], [pyk], [yak])
            S.dma('sp', self.yT_d[:, 0:2, q0:q0 + 128], ya[:], reads=[yak], defer=True)
        S.barrier()


Prog.phase_dsa = phase_dsa


def phase_gla(self, l, br):
    nc, S, T, NT = self.nc, self.S, self.T, self.NT
    W = self.w[l]
    if br == 1:
        sq_, sk_, sg_, vcol, gcol, bvo = 0, 2, 4, 129, 0, BV_GLA
    else:
        sq_, sk_, sg_, vcol, gcol, bvo = 10, 12, 14, 385, 256, BV_HG
    with ExitStack() as es:
        sb, pt, ring = _phase_tools(self, es)
        scanm = sb('scanm', [128, 128], F32)
        bdm = sb('bdm', [128, 2, 128], F32)
        gnb = sb('gnb', [128, 256], F32)
        Sf = sb('Sf', [128, 2, 64], F32)
        S.dma('sp', scanm[:], self.scanm_in, writes=['scanm'])
        S.dma('sp', bdm[:, 0, :], self.bdmask_in, writes=['bdm'])
        S.dma('sp', bdm[:, 1, :], self.bdmask_in, writes=['bdm'])
        S.dma('sp', gnb[:], W['bvec'][:, bvo:bvo + 256].partition_broadcast(128), writes=['gnb'])
        S.op('pool', lambda e: e.memset(Sf[:], 0.0), [], ['Sf'])
        Sbr = ring('Sb', 4, [128, 2, 64], BF16)
        Sb, Sbk = Sbr.next()
        S.op('pool', lambda e, Sb=Sb: e.memset(Sb[:], 0.0), [], [Sbk])
        qr = ring('q', 2, [128, 2, 128], F32)
        kr = ring('k', 2, [128, 2, 128], F32)
        gr = ring('g', 2, [128, 2, 128], F32)
        vr = ring('v', 2, [128, 256], BF16)
        gtr = ring('gt', 2, [128, 256], F32)
        br_ = ring('b', 2, [128, 128], F32)
        bmr = ring('bm', 2, [128, 128], F32)
        e3r = ring('e3', 2, [128, 3, 128], F32)
        qer = ring('qe', 2, [128, 128], BF16)
        ker = ring('ke', 2, [128, 128], BF16)
        q1r = ring('q1', 2, [128, 128], BF16)
        ebr = ring('eb', 2, [128, 4], F32)
        atr = ring('at', 2, [128, 2, 128], BF16)
        ktr = ring('kt', 2, [128, 128], BF16)
        tmr = ring('tm', 2, [128, 64], F32)
        jk = sb('jk', [128, 256], F32)
        ssr = ring('ss', 2, [128, 12], F32)
        yr = ring('y', 2, [128, 256], F32)
        ybr = ring('yb', 2, [128, 256], BF16)
        yTr = ring('yT', 2, [128, 2, 128], BF16)
        patE = [ring('pat', 1, [128, 512], F32, psum=True) for _ in range(2)]
        pkt = ring('pkt', 1, [128, 1024], BF16, psum=True)
        pdsC = [ring('pds', 1, [128, 512], F32, psum=True) for _ in range(2)]
        porE = [ring('po', 1, [128, 512], F32, psum=True) for _ in range(2)]
        pyt = ring('pyt', 1, [128, 8, 128], BF16, psum=True)

        for ti in range(NT):
            t0 = ti * 128
            q, qk = qr.next()
            k, kk = kr.next()
            g, gk = gr.next()
            v, vk = vr.next()
            gt, gtk = gtr.next()
            S.dma('sp', g[:], self.fmf_d[:, sg_:sg_ + 2, t0:t0 + 128], writes=[gk])
            S.dma('sp', q[:], self.fmf_d[:, sq_:sq_ + 2, t0:t0 + 128], writes=[qk])
            S.dma('sp', k[:], self.fmf_d[:, sk_:sk_ + 2, t0:t0 + 128], writes=[kk])
            S.dma('sp', v[:], self.tmb_d[t0:t0 + 128, vcol:vcol + 256], writes=[vk])
            S.dma('sp', gt[:], self.tmf_d[t0:t0 + 128, gcol:gcol + 256], writes=[gtk])
            poE = [porE[0].next(), porE[1].next()]
            Sb0, Sb0k = Sb, Sbk
            Sb1, Sb1k = Sbr.next()
            Sb2, Sb2k = Sbr.next()
            for hp in range(2):
                b, bk = br_.next()
                bm, bmk = bmr.next()
                S.op('dve', lambda e, b=b, g=g, hp=hp: e.tensor_tensor_scan(
                    out=b[:], data0=scanm[:], data1=g[:, hp, :], initial=0.0, op0=ALU.mult, op1=ALU.add),
                    ['scanm', gk], [bk])
                for c in range(2):
                    self.ts('dve', bm[:, c * 64:(c + 1) * 64], b[:, c * 64:(c + 1) * 64], b[:, c * 64 + 31:c * 64 + 32],
                            None, ALU.subtract, None, [bk], [bmk])
                e3, e3k = e3r.next()
                self.act(e3[:, 0, :], bm[:], AF.Exp, [bmk], [e3k])
                self.act(e3[:, 1, :], bm[:], AF.Exp, [bmk], [e3k], scale=-1.0)
                self.act(e3[:, 2, :], b[:], AF.Exp, [bk], [e3k])
                eb, ebk = ebr.next()
                for c in range(2):
                    self.act(eb[:, c:c + 1], b[:, c * 64 + 63:c * 64 + 64], AF.Exp, [bk], [ebk])
                    self.act(eb[:, 2 + c:3 + c], bm[:, c * 64 + 63:c * 64 + 64], AF.Exp, [bmk], [ebk])
                qe, qek = qer.next()
                ke, kek = ker.next()
                q1, q1k = q1r.next()
                self.tt('dve', qe[:], q[:, hp, :], e3[:, 0, :], ALU.mult, [qk, e3k], [qek])
                self.tt('pool', ke[:], k[:, hp, :], e3[:, 1, :], ALU.mult, [kk, e3k], [kek])
                self.tt('pool', q1[:], q[:, hp, :], e3[:, 2, :], ALU.mult, [qk, e3k], [q1k])
                at, atk = atr.next()
                for e_ in range(2):
                    sl = slice(e_ * 64, (e_ + 1) * 64)
                    pa, pak = patE[e_].next()
                    self.mm(pa[:, 0:128], ke[sl, :], qe[sl, :], True, True, [kek, qek], [pak])
                    self.tt('dve', at[:, e_, :], pa[:, 0:128], bdm[:, 0, :], ALU.mult, [pak, 'bdm'], [(atk, e_)])
                pk_, pkk = pkt.next()
                self.tr(pk_[:, 0:128], ke[:], self.identb[:], [kek, 'identb'], [pkk])
                kt, ktk = ktr.next()
                self.cp('act', kt[:], pk_[:, 0:128], [pkk], [ktk])
                pdc = [pdsC[0].next(), pdsC[1].next()]
                for c in range(2):
                    cs = slice(c * 64, (c + 1) * 64)
                    pd, pdk = pdc[c]
                    for e_ in range(2):
                        sl = slice(e_ * 64, (e_ + 1) * 64)
                        h = hp * 2 + e_
                        self.mm(pd[sl, 0:64], kt[cs, sl], v[cs, h * 64:(h + 1) * 64], True, True, [ktk, vk], [pdk])
                for c, (Sn, Snk) in enumerate(((Sb1, Sb1k), (Sb2, Sb2k))):
                    tm, tmk = tmr.next()
                    pd, pdk = pdc[c]
                    self.ts('dve', tm[:], pd[:, 0:64], eb[:, 2 + c:3 + c], None, ALU.mult, None, [pdk, ebk], [tmk])
                    self.stt(Sf[:, hp, :], Sf[:, hp, :], eb[:, c:c + 1], tm[:], ALU.mult, ALU.add, ['Sf', ebk, tmk], ['Sf'])
                    self.cp('act', Sn[:, hp, :], Sf[:, hp, :], ['Sf'], [Snk])
                for e_ in range(2):
                    sl = slice(e_ * 64, (e_ + 1) * 64)
                    h = hp * 2 + e_
                    hs = slice(h * 64, (h + 1) * 64)
                    po, pok = poE[e_]
                    ps_ = slice(hp * 64, (hp + 1) * 64)
                    self.mm(po[:, ps_], at[:, e_, :], v[:, hs], True, False, [(atk, e_), vk], [pok])
                    self.mm(po[0:64, ps_], q1[sl, 0:64], Sb0[sl, hp, :], False, False, [q1k, Sb0k], [pok])
                    self.mm(po[64:128, ps_], q1[sl, 64:128], Sb1[sl, hp, :], False, True, [q1k, Sb1k], [pok])
            Sb, Sbk = Sb2, Sb2k
            ss, ssk = ssr.next()
            for e_ in range(2):
                self.act(jk[:, e_ * 128:(e_ + 1) * 128], poE[e_][0][:, 0:128], AF.Square, [poE[e_][1]], ['jk'])
            S.op('dve', lambda e, ss=ss: e.tensor_reduce(out=ss[:, 0:4], in_=jk[:].rearrange("p (h v) -> p h v", h=4),
                                                         axis=AX.X, op=ALU.add), ['jk'], [ssk])
            self.act(ss[:, 4:8], ss[:, 0:4], AF.Sqrt, [ssk], [ssk], bias=EPS, scale=1.0 / 64)
            S.op('dve', lambda e, ss=ss: e.reciprocal(out=ss[:, 8:12], in_=ss[:, 4:8]), [ssk], [ssk])
            y, yk = yr.next()
            for h in range(4):
                hs = slice(h * 64, (h + 1) * 64)
                hp, e_ = h // 2, h % 2
                si = 8 + e_ * 2 + hp
                self.stt(y[:, hs], poE[e_][0][:, hp * 64:(hp + 1) * 64], ss[:, si:si + 1], gnb[:, hs], ALU.mult, ALU.mult,
                         [poE[e_][1], ssk, 'gnb'], [yk])
            yb, ybk = ybr.next()
            self.tt('pool', yb[:], y[:], gt[:], ALU.mult, [yk, gtk], [ybk])
            py, pyk = pyt.next()
            for hp in range(2):
                self.tr(py[:, hp, :], yb[:, hp * 128:(hp + 1) * 128], self.identb[:], [ybk, 'identb'], [pyk])
            yT, yTk = yTr.next()
            self.cp('act', yT[:], py[:, 0:2, :], [pyk], [yTk])
            S.dma('sp', self.yT_d[:, br * 2:br * 2 + 2, t0:t0 + 128], yT[:], reads=[yTk], defer=True)
        S.barrier()


Prog.phase_gla = phase_gla


def phase_lru(self, l):
    nc, S, T = self.nc, self.S, self.T
    W = self.w[l]
    with ExitStack() as es:
        sb, pt, ring = _phase_tools(self, es)
        pv = sb('pv', [128, NPV], F32)
        cc_ = sb('cc', [128, 8], F32)
        wa = sb('wa', [128, 2, 128], BF16)
        wx = sb('wx', [128, 2, 128], BF16)
        stage = ring('stg', 2, [128, 64], F32)
        S.dma('sp', pv[:], W['pvec'], writes=['pv'])
        self.act(cc_[:, 0:2], pv[:, 20:22], AF.Exp, ['pv'], ['cc'], scale=-1.0)
        self.act(cc_[:, 0:2], cc_[:, 0:2], AF.Ln, ['cc'], ['cc'], bias=1.0)
        self.ts('dve', cc_[:, 2:4], cc_[:, 0:2], -8.0, None, ALU.mult, None, ['cc'], ['cc'])
        self.ts('dve', cc_[:, 4:6], cc_[:, 0:2], -16.0, None, ALU.mult, None, ['cc'], ['cc'])
        S.op('pool', lambda e: e.memset(wa[:], 0.0), [], ['wa'])
        S.op('pool', lambda e: e.memset(wx[:], 0.0), [], ['wx'])
        for n in range(4):
            cc, e_ = n // 2, n % 2
            sl = slice(e_ * 64, (e_ + 1) * 64)
            self.load_cast(stage, wa[sl, cc, e_ * 64:(e_ + 1) * 64], 'wa', W['w_rg_a'][n], [64], pbase=e_ * 64)
            self.load_cast(stage, wx[sl, cc, e_ * 64:(e_ + 1) * 64], 'wx', W['w_rg_x'][n], [64], pbase=e_ * 64)
        LV = int(os.environ.get('DBG_LRU', '9'))
        cxr = [ring('cx', 2, [128, 515], F32) for _ in range(2)]
        hhr = [ring('hh', 2, [128, 512], F32) for _ in range(2)]
        cyr = ring('cy', 2, [128, 512], F32)
        xcr = ring('xc', 2, [128, 512], F32)
        xbr = ring('xb', 2, [128, 512], BF16)
        rr = ring('r', 2, [128, 512], F32)
        ir = ring('i', 2, [128, 512], F32)
        ar = ring('a', 2, [128, 512], F32)
        a2r = ring('a2', 2, [128, 512], F32)
        ur = ring('u', 2, [128, 512], F32)
        gr = ring('gg', 2, [128, 512], F32)
        g2r = ring('g2', 2, [128, 512], F32)
        ycr = ring('yc', 2, [128, 512], BF16)
        psr = ring('ps', 4, [128, 512], F32, psum=True)
        prev = [None, None]
        prevh = [None, None]
        for bi in range(T // 512 if LV > 0 else 0):
            tok0 = bi * 512
            for cc in range(2):
                cx, cxk = cxr[cc].next()
                cy, cyk = cyr.next()
                S.dma('sp', cx[:, 3:515], self.fmf_d[:, 6 + cc, tok0:tok0 + 512], writes=[cxk])
                S.dma('sp', cy[:], self.fmf_d[:, 8 + cc, tok0:tok0 + 512], writes=[cyk])
                if prev[cc] is None:
                    S.op('pool', lambda e, cx=cx: e.memset(cx[:, 0:3], 0.0), [], [cxk])
                else:
                    self.cp('pool', cx[:, 0:3], prev[cc][0][:, 512:515], [prev[cc][1]], [cxk])
                prev[cc] = (cx, cxk)
                xc, xck = xcr.next()
                self.ts('dve', xc[:], cx[:, 3:515], pv[:, 14 + cc:15 + cc], pv[:, 6 + cc:7 + cc], ALU.mult, ALU.add,
                        [cxk, 'pv'], [xck])
                for j in range(3):
                    self.stt(xc[:], cx[:, j:j + 512], pv[:, 8 + 2 * j + cc:9 + 2 * j + cc], xc[:], ALU.mult, ALU.add,
                             [cxk, 'pv', xck], [xck])
                if LV < 2:
                    continue
                xb, xbk = xbr.next()
                self.cp('act', xb[:], xc[:], [xck], [xbk])
                p1, p1k = psr.next()
                p2, p2k = psr.next()
                self.mm(p1[:], wa[:, cc, :], xb[:], True, True, ['wa', xbk], [p1k])
                self.mm(p2[:], wx[:, cc, :], xb[:], True, True, ['wx', xbk], [p2k])
                r, rk = rr.next()
                i_, ik = ir.next()
                self.act(r[:], p1[:], AF.Sigmoid, [p1k, 'pv'], [rk], bias=pv[:, 16 + cc:17 + cc])
                self.act(i_[:], p2[:], AF.Sigmoid, [p2k, 'pv'], [ik], bias=pv[:, 18 + cc:19 + cc])
                a, ak = ar.next()
                a2, a2k = a2r.next()
                self.act(a[:], r[:], AF.Exp, [rk, 'cc'], [ak], scale=cc_[:, 2 + cc:3 + cc])
                self.act(a2[:], r[:], AF.Exp, [rk, 'cc'], [a2k], scale=cc_[:, 4 + cc:5 + cc])
                self.act(a2[:], a2[:], AF.Sqrt, [a2k], [a2k], bias=1.0, scale=-1.0)
                u, uk = ur.next()
                self.tt('dve', u[:], a2[:], i_[:], ALU.mult, [a2k, ik], [uk])
                self.tt('pool', u[:], u[:], xc[:], ALU.mult, [uk, xck], [uk])
                if LV < 3:
                    continue
                hh, hhk = hhr[cc].next()
                init = 0.0 if prevh[cc] is None else prevh[cc][0][:, 511:512]
                rd = [ak, uk] + ([] if prevh[cc] is None else [prevh[cc][1]])
                S.op('dve', lambda e, hh=hh, a=a, u=u, init=init: e.tensor_tensor_scan(
                    out=hh[:], data0=a[:], data1=u[:], initial=init, op0=ALU.mult, op1=ALU.add), rd, [hhk])
                prevh[cc] = (hh, hhk)
                if LV < 4:
                    continue
                g, gk = gr.next()
                g2, g2k = g2r.next()
                self.tt('pool', g[:], cy[:], cy[:], ALU.mult, [cyk], [gk])
                self.ts('dve', g[:], g[:], 0.044715, 1.0, ALU.mult, ALU.add, [gk], [gk])
                self.tt('pool', g[:], g[:], cy[:], ALU.mult, [gk, cyk], [gk])
                self.act(g2[:], g[:], AF.Sigmoid, [gk], [g2k], scale=1.5957691216057308)
                self.tt('dve', g2[:], g2[:], cy[:], ALU.mult, [g2k, cyk], [g2k])
                yc, yck = ycr.next()
                self.tt('pool', yc[:], g2[:], hh[:], ALU.mult, [g2k, hhk], [yck])
                S.dma('sp', self.yT_d[:, 4 + cc, tok0:tok0 + 512], yc[:], reads=[yck], defer=True)
        S.barrier()


Prog.phase_lru = phase_lru


def phase_merge(self, l, x_src):
    nc, S, T, NT = self.nc, self.S, self.T, self.NT
    W = self.w[l]
    with ExitStack() as es:
        sb, pt, ring = _phase_tools(self, es)
        wg = sb('wg', [128, 8, 4096], BF16)
        wbr = sb('wbr', [128, 8, 1024], BF16)
        wo = sb('wo', [128, 8, 1024], BF16)
        bgb = sb('bgb', [128, 4096], F32)
        stage = ring('stg', 2, [128, 2048], F32)
        S.dma('sp', bgb[:], W['bvec'][:, BV_BG:BV_BG + 4096].partition_broadcast(128), writes=['bgb'])
        for k in range(8):
            for c0 in (0, 2048):
                self.load_cast(stage, wg[:, k, c0:c0 + 2048], ('wg', k), W['w_gate'][k * 128:(k + 1) * 128, c0:c0 + 2048], [2048])
            self.load_cast(stage, wo[:, k, :], ('wo', k), W['w_out'][k * 128:(k + 1) * 128, :], [1024])
        for b_, nm in enumerate(('w_br_a', 'w_br_b', 'w_br_c', 'w_br_d')):
            for kk in range(2):
                self.load_cast(stage, wbr[:, b_ * 2 + kk, :], ('wbr', b_), W[nm][kk * 128:(kk + 1) * 128, :], [1024])
        hr = ring('hT', 2, [128, 8, 128], BF16)
        yr = ring('yT', 2, [128, 8, 128], BF16)
        xr = ring('x', 2, [128, 1024], F32)
        mr = ring('m', 2, [128, 1024], F32)
        mbr = ring('mb', 2, [128, 1024], BF16)
        mTr = ring('mT', 2, [128, 8, 128], BF16)
        gsr = ring('gs', 3, [128, 512], F32)
        tpr = ring('tp', 2, [128, 512], F32)
        xor_ = ring('xo', 2, [128, 1024], F32)
        pgr = ring('pg', 3, [128, 512], F32, psum=True)
        pyr = ring('py', 2, [128, 512], F32, psum=True)
        ptr = ring('pt', 1, [128, 8, 128], BF16, psum=True)
        for ti in range(NT):
            t0 = ti * 128
            hT, hTk = hr.next()
            yT, yTk = yr.next()
            x, xk = xr.next()
            S.dma('sp', hT[:], self.hT_d[:, :, t0:t0 + 128], writes=[hTk])
            S.dma('sp', yT[:], self.yT_d[:, :, t0:t0 + 128], writes=[yTk])
            S.dma('sp', x[:], x_src[t0:t0 + 128, :], writes=[xk])
            m, mk = mr.next()
            for b_ in range(4):
                for cb in range(2):
                    c0 = b_ * 1024 + cb * 512
                    pg, pgk = pgr.next()
                    for k in range(8):
                        self.mm(pg[:], hT[:, k, :], wg[:, k, c0:c0 + 512], k == 0, k == 7, [hTk, ('wg', k)], [pgk])
                    py, pyk = pyr.next()
                    for kk in range(2):
                        self.mm(py[:], yT[:, b_ * 2 + kk, :], wbr[:, b_ * 2 + kk, cb * 512:(cb + 1) * 512], kk == 0, kk == 1,
                                [yTk, ('wbr', b_)], [pyk])
                    gs, gsk = gsr.next()
                    self.tt('dve', gs[:], pg[:], bgb[:, c0:c0 + 512], ALU.add, [pgk, 'bgb'], [gsk])
                    self.act(gs[:], gs[:], AF.Sigmoid, [gsk], [gsk])
                    ms = m[:, cb * 512:(cb + 1) * 512]
                    if b_ == 0:
                        self.tt('dve', ms, gs[:], py[:], ALU.mult, [gsk, pyk], [(mk, cb)])
                    else:
                        tp, tpk = tpr.next()
                        self.tt('dve', tp[:], gs[:], py[:], ALU.mult, [gsk, pyk], [tpk])
                        self.tt('pool', ms, ms, tp[:], ALU.add, [(mk, cb), tpk], [(mk, cb)])
            mb, mbk = mbr.next()
            self.cp('act', mb[:], m[:], [(mk, 0), (mk, 1)], [mbk])
            p, pk = ptr.next()
            for k in range(8):
                self.tr(p[:, k, :], mb[:, k * 128:(k + 1) * 128], self.identb[:], [mbk, 'identb'], [pk])
            mT, mTk = mTr.next()
            self.cp('act', mT[:], p[:], [pk], [mTk])
            xo, xok = xor_.next()
            for cb in range(2):
                po, pok = pgr.next()
                for k in range(8):
                    self.mm(po[:], mT[:, k, :], wo[:, k, cb * 512:(cb + 1) * 512], k == 0, k == 7, [mTk, ('wo', k)], [pok])
                self.tt('dve', xo[:, cb * 512:(cb + 1) * 512], x[:, cb * 512:(cb + 1) * 512], po[:], ALU.add,
                        [xk, pok], [xok])
            S.dma('sp', self.xmid_d[t0:t0 + 128, :], xo[:], reads=[xok], defer=True)
        S.barrier()


Prog.phase_merge = phase_merge


def phase_ffn(self, l, x_dst, final):
    nc, S, T = self.nc, self.S, self.T
    W = self.w[l]
    NS = 256
    with ExitStack() as es:
        sb, pt, ring = _phase_tools(self, es)
        wf1 = sb('wf1', [128, 8, 4096], BF16)
        wf2 = sb('wf2', [128, 32, 1024], BF16)
        g2b = sb('g2b', [128, 1024], F32)
        gfb = sb('gfb', [128, 1024], F32)
        stage = ring('stg', 2, [128, 1024], F32)
        S.dma('sp', g2b[:], W['bvec'][:, BV_N2:BV_N2 + 1024].partition_broadcast(128), writes=['g2b'])
        S.dma('sp', gfb[:], W['bvec'][:, BV_FN:BV_FN + 1024].partition_broadcast(128), writes=['gfb'])
        for k in range(8):
            for c0 in range(0, 4096, 1024):
                self.load_cast(stage, wf1[:, k, c0:c0 + 1024], ('wf1', k), W['w_ff1'][k * 128:(k + 1) * 128, c0:c0 + 1024], [1024])
        for f in range(32):
            self.load_cast(stage, wf2[:, f, :], ('wf2', f), W['w_ff2'][f * 128:(f + 1) * 128, :], [1024])
        xr = ring('xm', 3, [128, 1024], F32)
        junk = sb('junk', [128, 1024], F32)
        hr = ring('h2', 2, [128, 1024], BF16)
        hTr = ring('h2T', 1, [128, 8, NS], BF16)
        uTr = ring('uT', 1, [128, 32, NS], BF16)
        sqr = ring('sq', 2, [128, NS], F32)
        st1 = ring('st', 4, [128, 4], F32)
        xor_ = ring('xo', 2, [128, 1024], F32)
        ptr = ring('pt', 2, [128, 8, 128], BF16, psum=True)
        pur = ring('pu', 3, [128, 512], F32, psum=True)
        por = ring('po', 3, [128, 512], F32, psum=True)
        for si in range(T // NS):
            tok0 = si * NS
            hT, hTk = hTr.next()
            xs = []
            for j in range(NS // 128):
                t0 = tok0 + j * 128
                x, xk = xr.next()
                xs.append((x, xk))
                S.dma('sp', x[:], self.xmid_d[t0:t0 + 128, :], writes=[xk])
                s1, s1k = st1.next()
                self.act(junk[:], x[:], AF.Square, [xk], ['junk', s1k], accum=s1[:, 0:1])
                self.act(s1[:, 1:2], s1[:, 0:1], AF.Sqrt, [s1k], [s1k], bias=EPS, scale=1.0 / 1024)
                S.op('dve', lambda e, s1=s1: e.reciprocal(out=s1[:, 2:3], in_=s1[:, 1:2]), [s1k], [s1k])
                h, hk = hr.next()
                self.stt(h[:], x[:], s1[:, 2:3], g2b[:], ALU.mult, ALU.mult, [xk, s1k, 'g2b'], [hk])
                p, pk = ptr.next()
                for k in range(8):
                    self.tr(p[:, k, :], h[:, k * 128:(k + 1) * 128], self.identb[:], [hk, 'identb'], [pk])
                self.cp('act', hT[:, :, j * 128:(j + 1) * 128], p[:], [pk], [hTk])
            uT, uTk = uTr.next()
            for f in range(32):
                pu, puk = pur.next()
                for k in range(8):
                    self.mm(pu[:, 0:NS], wf1[:, k, f * 128:(f + 1) * 128], hT[:, k, :], k == 0, k == 7,
                            [hTk, ('wf1', k)], [puk])
                sq, sqk = sqr.next()
                self.act(sq[:], pu[:, 0:NS], AF.Square, [puk], [sqk])
                self.stt(uT[:, f, :], pu[:, 0:NS], 0.0, sq[:], ALU.is_gt, ALU.mult, [puk, sqk], [(uTk, f)])
            for j in range(NS // 128):
                t0 = tok0 + j * 128
                x, xk = xs[j]
                xo, xok = xor_.next()
                for cb in range(2):
                    po, pok = por.next()
                    for f in range(32):
                        self.mm(po[:], uT[:, f, j * 128:(j + 1) * 128], wf2[:, f, cb * 512:(cb + 1) * 512], f == 0, f == 31,
                                [(uTk, f), ('wf2', f)], [pok])
                    self.tt('dve', xo[:, cb * 512:(cb + 1) * 512], x[:, cb * 512:(cb + 1) * 512], po[:], ALU.add,
                            [xk, pok], [xok])
                if final:
                    s1, s1k = st1.next()
                    self.act(junk[:], xo[:], AF.Square, [xok], ['junk', s1k], accum=s1[:, 0:1])
                    self.act(s1[:, 1:2], s1[:, 0:1], AF.Sqrt, [s1k], [s1k], bias=EPS, scale=1.0 / 1024)
                    S.op('dve', lambda e, s1=s1: e.reciprocal(out=s1[:, 2:3], in_=s1[:, 1:2]), [s1k], [s1k])
                    self.stt(xo[:], xo[:], s1[:, 2:3], gfb[:], ALU.mult, ALU.mult, [xok, s1k, 'gfb'], [xok])
                S.dma('sp', x_dst[t0:t0 + 128, :], xo[:], reads=[xok], defer=True)
        S.barrier()


Prog.phase_ffn = phase_ffn


_CACHE = {}


def kernel(**inputs):
    inp = {k: np.asarray(v) for k, v in inputs.items()}
    B, T, D = inp['x'].shape
    P = Prog(T, depth=DEPTH)
    nc = P.build()
    in_maps = [make_in_map(inp, b) for b in range(B)]
    res = run_bass_kernel_spmd(nc, in_maps, core_ids=list(range(B)))
    out = np.stack([np.asarray(r['out'], dtype=np.float32) for r in res.results], axis=0)
    return out
```

```python
import numpy as np
import concourse.bass as bass
import concourse.mybir as mybir
import os
from contextlib import ExitStack
from concourse.bass_utils import run_bass_kernel_spmd

F32 = mybir.dt.float32
BF16 = mybir.dt.bfloat16
AF = mybir.ActivationFunctionType
ALU = mybir.AluOpType
AX = mybir.AxisListType

D_MODEL = 1024
DEPTH = 2
EPS = 1e-6
D_FF = 4096
IN_COLS = 3284
O_AQ, O_CKV, O_IQ, O_IK, O_IW = 0, 256, 384, 640, 704
O_BQ, O_BK, O_BV, O_BLR, O_BR = 708, 964, 1220, 1476, 1492
O_CX, O_CY = 1748, 2004
O_DQ, O_DF, O_DI, O_DG = 2260, 2516, 2772, 3028


class Sched:
    ENG = ('pe', 'dve', 'act', 'pool', 'sp')
    LOOKBACK = 3
    SEM_MAX = 30000

    def __init__(self, nc):
        self.nc = nc
        self.q = {e: [] for e in self.ENG}
        self.sem = {}
        self.cnt = {}
        self.old = {}
        self.all_sems = []
        self.nsem = 0
        for e in self.ENG:
            if e != 'sp':
                self._new_sem(e)
        self.known = {e: {} for e in self.ENG}
        self.res = {}
        self.dma_ring = []
        self.dma_i = 0
        self.NDMA = 40
        self.pend = []
        self.pend_sems = set()
        self.pend_age = 0
        self.DEFER_LOADS = 3
        self.n_ops = 0

    def _alloc(self, name):
        self.nsem += 1
        h = self.nc.alloc_semaphore(f"{name}_{self.nsem}")
        self.all_sems.append(h)
        return h

    def _new_sem(self, e):
        if e in self.sem:
            self.old.setdefault(e, []).append((self.sem[e], self.cnt[e]))
        self.sem[e] = self._alloc('s' + e)
        self.cnt[e] = 0

    def _need(self, eng, ev, waits, use_known=True):
        if ev is None:
            return
        sem, val, src = ev
        if sem.name in self.pend_sems:
            self.flush()
        if src == eng:
            if eng == 'pe':
                return
            if sem is self.sem[eng] and val <= self.cnt[eng] - self.LOOKBACK:
                return
            if sem is not self.sem[eng]:
                return
        k = self.known[eng]
        if use_known and k.get(sem.name, 0) >= val:
            return
        cur = waits.get(sem.name)
        if cur is None or cur[1] < val:
            waits[sem.name] = (sem, val)

    def _deps(self, eng, reads, writes, use_known=True):
        waits = {}
        for r in reads:
            st = self.res.get(r)
            if st is not None:
                self._need(eng, st[0], waits, use_known)
        for w in writes:
            st = self.res.get(w)
            if st is not None:
                self._need(eng, st[0], waits, use_known)
                for ev in st[1].values():
                    self._need(eng, ev, waits, use_known)
        if use_known:
            for name, (sem, val) in waits.items():
                self.known[eng][name] = val
        return list(waits.values())

    def _commit(self, eng, ev, reads, writes):
        for r in reads:
            st = self.res.setdefault(r, [None, {}])
            st[1][eng if ev[2] != 'dma' else ('dma', ev[0].name)] = ev
        for w in writes:
            self.res[w] = [ev, {}]

    def op(self, eng, fn, reads=(), writes=()):
        waits = self._deps(eng, reads, writes)
        if self.cnt[eng] >= self.SEM_MAX:
            self._new_sem(eng)
        sem = self.sem[eng]
        self.cnt[eng] += 1
        ev = (sem, self.cnt[eng], eng)
        self._commit(eng, ev, reads, writes)
        self.q[eng].append((waits, fn, sem, 1))
        self.n_ops += 1

    def flush(self):
        k = self.known['sp']
        for (waits, fn, sem, inc) in self.pend:
            w2 = []
            for (s_, v_) in waits:
                if k.get(s_.name, 0) < v_:
                    k[s_.name] = v_
                    w2.append((s_, v_))
            self.q['sp'].append((w2, fn, sem, inc))
        self.pend = []
        self.pend_sems = set()
        self.pend_age = 0

    def dma(self, eng, out, in_, reads=(), writes=(), defer=False):
        eng = 'sp'
        if len(self.dma_ring) < self.NDMA:
            self.dma_ring.append([self._alloc('d'), 0, None])
            slot = self.dma_ring[-1]
        else:
            slot = self.dma_ring[self.dma_i % self.NDMA]
        self.dma_i += 1
        if slot[0].name in self.pend_sems:
            self.flush()
        waits = self._deps(eng, reads, writes, use_known=not defer)
        if slot[2] is not None:
            w2 = {}
            self._need(eng, slot[2], w2, use_known=not defer)
            for name, (sem, val) in w2.items():
                if not defer:
                    self.known[eng][name] = val
                waits = [w for w in waits if w[0].name != name] + [(sem, val)]
        slot[1] += 16
        ev = (slot[0], slot[1], 'dma')
        slot[2] = ev
        self._commit(eng, ev, reads, writes)
        ent = (waits, lambda e, o=out, i=in_: e.dma_start(out=o, in_=i), slot[0], 16)
        if defer:
            self.pend.append(ent)
            self.pend_sems.add(slot[0].name)
        else:
            self.q[eng].append(ent)
            if self.pend:
                self.pend_age += 1
                if self.pend_age >= self.DEFER_LOADS:
                    self.flush()
        self.n_ops += 1

    def barrier(self):
        self.flush()
        for e in self.ENG:
            waits = []
            k = self.known[e]
            for f in self.ENG:
                if f == 'sp' or (f == e and e == 'pe'):
                    continue
                for (sem, c) in self.old.get(f, []) + [(self.sem[f], self.cnt[f])]:
                    if c > 0 and k.get(sem.name, 0) < c:
                        waits.append((sem, c))
                        k[sem.name] = c
            for slot in self.dma_ring:
                if slot[2] is not None and k.get(slot[0].name, 0) < slot[1]:
                    waits.append((slot[0], slot[1]))
                    k[slot[0].name] = slot[1]
            self.q[e].append((waits, None, None, 0))
        self.res = {}

    def finish(self, eng='sp'):
        self.flush()
        waits = {}
        for slot in self.dma_ring:
            if slot[2] is not None:
                self._need(eng, slot[2], waits)
        self.q[eng].append((list(waits.values()), None, None, 0))

    def emit(self):
        nc = self.nc
        q = self.q

        def run(e, lst):
            for waits, fn, sem, inc in lst:
                for (s, v) in waits:
                    e.wait_ge(s, v)
                if fn is not None:
                    ins = fn(e)
                    ins.then_inc(sem, inc)

        for h in self.all_sems:
            nc.sync.sem_clear(h)
        with nc.Block() as block:
            @block.tensor
            def _(e):
                run(e, q['pe'])

            @block.vector
            def _(e):
                run(e, q['dve'])

            @block.scalar
            def _(e):
                run(e, q['act'])

            @block.gpsimd
            def _(e):
                run(e, q['pool'])

            @block.sync
            def _(e):
                run(e, q['sp'])


class Ring:
    def __init__(self, nc, name, n, shape, dtype, psum=False):
        self.tiles = []
        for i in range(n):
            if psum:
                t = nc.alloc_psum_tensor(f"{name}{i}", shape, dtype)
            else:
                t = nc.alloc_sbuf_tensor(f"{name}{i}", shape, dtype)
            self.tiles.append((t, f"{name}{i}"))
        self.i = 0

    def next(self):
        t = self.tiles[self.i % len(self.tiles)]
        self.i += 1
        return t


NPV = 24


def pack_pvec(inp, l):
    def c2(v):
        return np.ascontiguousarray(np.asarray(v, np.float32).reshape(2, 128).T)
    cols = [c2(inp['b_gk'][l]), c2(inp['lb_param'][0]), c2(inp['lb_param'][1]), c2(inp['conv_b'][l])]
    for j in range(4):
        cols.append(c2(inp['conv_w'][l][j]))
    cols += [c2(inp['b_rg_a'][l]), c2(inp['b_rg_x'][l]), c2(inp['lru_lambda'][l])]
    pv = np.concatenate(cols, axis=1)
    out = np.zeros((128, NPV), np.float32)
    out[:, :pv.shape[1]] = pv
    return out


BV_N1, BV_KV, BV_GLA, BV_HG, BV_BG, BV_N2, BV_FN = 0, 1024, 1152, 1408, 1664, 5760, 6784
NBV = 7808


def pack_bvec(inp, l):
    v = np.concatenate([
        inp['norm1_g'][l], inp['kv_norm_g'][l], np.tile(inp['gla_norm_g'][l], 4),
        np.tile(inp['hgrn_norm_g'][l], 4), inp['b_gate'][l], inp['norm2_g'][l], inp['final_norm_g']
    ]).astype(np.float32)
    assert v.shape[0] == NBV
    return v.reshape(1, NBV)


class Prog:
    def __init__(self, T, depth=DEPTH, debug=()):
        self.T = T
        self.NT = T // 128
        self.depth = depth
        self.debug = set(debug)
        nc = self.nc = bass.Bass("TRN2", target_bir_lowering=False)
        self.S = Sched(nc)
        self.uid = 0
        dt = nc.dram_tensor
        self.x_in = dt("x", [T, D_MODEL], F32, kind="ExternalInput").ap()
        self.out = dt("out", [T, D_MODEL], F32, kind="ExternalOutput").ap()
        self.w = []
        for l in range(depth):
            d = {}
            for name, shape in [('w_in', [D_MODEL, IN_COLS]), ('w_gate', [D_MODEL, 4096]),
                                ('w_uk', [4, 64, 128]), ('w_uv', [4, 128, 64]), ('w_gk2', [16, 256]),
                                ('w_rg_a', [4, 64, 64]), ('w_rg_x', [4, 64, 64]),
                                ('w_br_a', [256, 1024]), ('w_br_b', [256, 1024]), ('w_br_c', [256, 1024]),
                                ('w_br_d', [256, 1024]), ('w_out', [1024, 1024]), ('w_ff1', [1024, 4096]),
                                ('w_ff2', [4096, 1024]), ('pvec', [128, NPV]), ('bvec', [1, NBV])]:
                d[name] = dt(f"{name}{l}", shape, F32, kind="ExternalInput").ap()
            self.w.append(d)
        self.biasT = dt("biasT", [128, 4, 256], F32, kind="ExternalInput").ap()
        self.scr = {}

    def scratch(self, name, shape, dtype):
        kind = "ExternalOutput" if name in self.debug else "Internal"
        t = self.nc.dram_tensor(name, shape, dtype, kind=kind).ap()
        self.scr[name] = t
        return t

    def u(self, p):
        self.uid += 1
        return f"{p}{self.uid}"

    def act(self, out, in_, func, reads, writes, bias=0.0, scale=1.0, accum=None):
        kw = {}
        if accum is not None:
            kw['accum_out'] = accum
        self.S.op('act', lambda e: e.activation(out=out, in_=in_, func=func, bias=bias, scale=scale, **kw),
                  reads, writes)

    def mm(self, out, lhsT, rhs, start, stop, reads, writes):
        self.S.op('pe', lambda e: e.matmul(out, lhsT, rhs, start=start, stop=stop), reads, writes)

    def tr(self, out, in_, ident, reads, writes):
        self.S.op('pe', lambda e: e.transpose(out, in_, ident), reads, writes)

    def tt(self, eng, out, in0, in1, op, reads, writes):
        self.S.op(eng, lambda e: e.tensor_tensor(out=out, in0=in0, in1=in1, op=op), reads, writes)

    def ts(self, eng, out, in0, s1, s2, op0, op1, reads, writes, accum=None):
        if op1 is None:
            self.S.op(eng, lambda e: e.tensor_scalar(out=out, in0=in0, scalar1=s1, scalar2=None, op0=op0),
                      reads, writes)
        elif accum is not None:
            self.S.op(eng, lambda e: e.tensor_scalar(out=out, in0=in0, scalar1=s1, scalar2=s2, op0=op0, op1=op1,
                                                     accum_out=accum), reads, writes)
        else:
            self.S.op(eng, lambda e: e.tensor_scalar(out=out, in0=in0, scalar1=s1, scalar2=s2, op0=op0, op1=op1),
                      reads, writes)

    def stt(self, out, in0, scalar, in1, op0, op1, reads, writes):
        self.S.op('dve', lambda e: e.scalar_tensor_tensor(out=out, in0=in0, scalar=scalar, in1=in1, op0=op0, op1=op1),
                  reads, writes)

    def cp(self, eng, out, in_, reads, writes):
        if eng == 'act':
            self.S.op('act', lambda e: e.copy(out=out, in_=in_), reads, writes)
        else:
            self.S.op(eng, lambda e: e.tensor_copy(out=out, in_=in_), reads, writes)

    def load_cast(self, ring, dst, dkey, src, shape, engs=('pool', 'dve'), pbase=0):
        st, sk = ring.next()
        if len(shape) == 1:
            sv = st[:, 0:shape[0]]
        else:
            n = shape[0] * shape[1]
            sv = st[:, 0:n].rearrange("p (a b) -> p a b", a=shape[0])
        np_ = dst.shape[0]
        self.S.dma('sp', sv[pbase:pbase + np_], src, writes=[sk])
        self.cast_i = getattr(self, 'cast_i', 0) + 1
        self.cp(engs[self.cast_i % len(engs)], dst, sv[pbase:pbase + np_], [sk], [dkey])

    def alloc_scratch(self):
        T = self.T
        self.hT_d = self.scratch("hT_d", [128, 8, T], BF16)
        self.fmf_d = self.scratch("fmf_d", [128, 16, T], F32)
        self.fmb_d = self.scratch("fmb_d", [128, 7, T], BF16)
        self.tmb_d = self.scratch("tmb_d", [T, 641], BF16)
        self.tmf_d = self.scratch("tmf_d", [T, 512], F32)
        self.ckvT_d = self.scratch("ckvT_d", [128, T], BF16)
        self.iw_d = self.scratch("iw_d", [128, self.NT, 4], F32)
        self.yT_d = self.scratch("yT_d", [128, 8, T], BF16)
        self.xmid_d = self.scratch("xmid_d", [T, D_MODEL], F32)
        self.xres_d = self.scratch("xres_d", [T, D_MODEL], F32)

    def consts(self, es):
        nc, S = self.nc, self.S
        self.ident_in = nc.dram_tensor("ident", [128, 128], F32, kind="ExternalInput").ap()
        self.cmask_in = nc.dram_tensor("cmask", [128, 128], F32, kind="ExternalInput").ap()
        self.bdmask_in = nc.dram_tensor("bdmask", [128, 128], F32, kind="ExternalInput").ap()
        self.scanm_in = nc.dram_tensor("scanm", [128, 128], F32, kind="ExternalInput").ap()
        self.cneg_in = nc.dram_tensor("cneg", [128, 128], F32, kind="ExternalInput").ap()
        self.relb_in = nc.dram_tensor("relb", [32, 4], F32, kind="ExternalInput").ap()
        self.identb = es.enter_context(nc.sbuf_tensor("identb", [128, 128], BF16))
        self.identf = es.enter_context(nc.sbuf_tensor("identf", [128, 128], F32))
        S.dma('sp', self.identf[:], self.ident_in, writes=['identf'])
        self.cp('dve', self.identb[:], self.identf[:], ['identf'], ['identb'])

    def phase1(self, l, x_src):
        nc, S, T = self.nc, self.S, self.T
        W = self.w[l]
        with ExitStack() as es:
            def sb(name, shape, dt):
                return es.enter_context(nc.sbuf_tensor(self.u(name), shape, dt))

            def pt(name, shape, dt):
                return es.enter_context(nc.psum_tensor(self.u(name), shape, dt))

            def ring(name, n, shape, dt, psum=False):
                tiles = []
                for i in range(n):
                    nm = self.u(name)
                    t = pt(nm, shape, dt) if psum else sb(nm, shape, dt)
                    tiles.append((t, nm))
                r = Ring.__new__(Ring)
                r.tiles, r.i = tiles, 0
                return r

            wfm = sb('wfm', [128, 8, 2304], BF16)
            wtm = sb('wtm', [128, 8, 1156], BF16)
            wuk = sb('wuk', [128, 2, 128], BF16)
            wgk = sb('wgk', [16, 256], BF16)
            pv = sb('pv', [128, NPV], F32)
            lbc = sb('lbc', [128, 8], F32)
            nbgk = sb('nbgk', [128, 2], F32)
            g1b = sb('g1b', [128, 1024], F32)
            gkvb = sb('gkvb', [128, 128], F32)
            iw_all = sb('iw_all', [128, self.NT, 4], F32)
            stage = ring('stg', 3, [128, 2304], F32)

            S.dma('sp', pv[:], W['pvec'], writes=['pv'])
            S.dma('sp', g1b[:], W['bvec'][:, BV_N1:BV_N1 + 1024].partition_broadcast(128), writes=['g1b'])
            S.dma('sp', gkvb[:], W['bvec'][:, BV_KV:BV_KV + 128].partition_broadcast(128), writes=['gkvb'])
            if l == 0:
                S.op('dve', lambda e: e.memset(lbc[:, 0:2], 0.0), [], ['lbc'])
            else:
                self.tt('dve', lbc[:, 6:8], pv[:, 4:6], pv[:, 2:4], ALU.subtract, ['pv'], ['lbc'])
                self.act(lbc[:, 0:2], lbc[:, 6:8], AF.Sigmoid, ['lbc'], ['lbc'])
            self.ts('dve', lbc[:, 2:4], lbc[:, 0:2], -1.0, 1.0, ALU.mult, ALU.add, ['lbc'], ['lbc'])
            self.ts('dve', lbc[:, 4:6], lbc[:, 0:2], 1e-20, None, ALU.max, None, ['lbc'], ['lbc'])
            self.ts('dve', nbgk[:], pv[:, 0:2], -1.0, None, ALU.mult, None, ['pv'], ['nbgk'])

            fm_src = [(O_AQ, 256), (O_IQ, 256), (O_IK, 64), (O_IK, 64), (O_BQ, 256), (O_BK, 256), (O_BLR, 16),
                      (O_CX, 256), (O_CY, 256), (O_DQ, 256), (O_DF, 256)]
            fm_dst = [0, 256, 512, 576, 640, 896, 1152, 1280, 1536, 1792, 2048]
            tm_src = [(O_CKV, 128), (O_IW, 4), (O_BV, 256), (O_BR, 256), (O_DI, 256), (O_DG, 256)]
            tm_dst = [0, 128, 132, 388, 644, 900]
            for k in range(8):
                for (so, n), do in zip(fm_src, fm_dst):
                    self.load_cast(stage, wfm[:, k, do:do + n], ('wfm', k), W['w_in'][k * 128:(k + 1) * 128, so:so + n], [n])
                for (so, n), do in zip(tm_src, tm_dst):
                    self.load_cast(stage, wtm[:, k, do:do + n], ('wtm', k), W['w_in'][k * 128:(k + 1) * 128, so:so + n], [n])
            self.load_cast(stage, wuk[:], 'wuk', W['w_uk'].rearrange("(hp e) d c -> (e d) hp c", e=2), [2, 128])
            self.load_cast(stage, wgk[:], 'wgk', W['w_gk2'], [256])

            xr = ring('xt', 3, [128, 1024], F32)
            junk = sb('junk', [128, 1024], F32)
            hr = ring('h', 2, [128, 1024], BF16)
            hTr = ring('hT', 2, [128, 8, 512], BF16)
            st1 = ring('st1', 4, [128, 4], F32)
            ptr = ring('ptr', 2, [128, 8, 128], BF16, psum=True)
            psr = ring('ps', 4, [128, 512], F32, psum=True)
            ptc = ring('ptc', 1, [128, 128], BF16, psum=True)
            tmbr = ring('tmb', 3, [128, 641], BF16)
            tmfr = ring('tmf', 3, [128, 512], F32)
            ckvTr = ring('ckvT', 2, [128, 512], BF16)
            qTr = ring('qT', 2, [128, 2, 512], BF16)
            blrr = ring('blr', 2, [16, 512], BF16)
            ofr = ring('of', 6, [128, 512], F32)
            obr = ring('ob', 4, [128, 512], BF16)
            for t_, _k in tmbr.tiles:
                S.op('pool', lambda e, t_=t_: e.memset(t_[:, 128:129], 1.0), [], [_k])

            for st_i in range(T // 512):
                tok0 = st_i * 512
                hT, hTk = hTr.next()
                ckvT, ckvTk = ckvTr.next()
                for j in range(4):
                    t0 = tok0 + j * 128
                    tt_i = t0 // 128
                    xt, xk = xr.next()
                    S.dma('sp', xt[:], x_src[t0:t0 + 128, :], writes=[xk])
                    s1, s1k = st1.next()
                    self.act(junk[:], xt[:], AF.Square, [xk], ['junk', s1k], accum=s1[:, 0:1])
                    self.act(s1[:, 1:2], s1[:, 0:1], AF.Sqrt, [s1k], [s1k], bias=EPS, scale=1.0 / 1024)
                    S.op('dve', lambda e, s1=s1: e.reciprocal(out=s1[:, 2:3], in_=s1[:, 1:2]), [s1k], [s1k])
                    h, hk = hr.next()
                    self.stt(h[:], xt[:], s1[:, 2:3], g1b[:], ALU.mult, ALU.mult, [xk, s1k, 'g1b'], [hk])
                    p, pk = ptr.next()
                    for k in range(8):
                        self.tr(p[:, k, :], h[:, k * 128:(k + 1) * 128], self.identb[:], [hk, 'identb'], [pk])
                    self.cp('act', hT[:, :, j * 128:(j + 1) * 128], p[:], [pk], [hTk])
                    tmb, tmbk = tmbr.next()
                    tmf, tmfk = tmfr.next()
                    pa, pak = psr.next()
                    for k in range(8):
                        self.mm(pa[:, 0:388], hT[:, k, j * 128:(j + 1) * 128], wtm[:, k, 0:388], k == 0, k == 7,
                                [hTk, ('wtm', k)], [pak])
                    s2, s2k = st1.next()
                    self.act(junk[:, 0:128], pa[:, 0:128], AF.Square, [pak], ['junk', s2k], accum=s2[:, 0:1])
                    self.act(s2[:, 1:2], s2[:, 0:1], AF.Sqrt, [s2k], [s2k], bias=EPS, scale=1.0 / 128)
                    S.op('dve', lambda e, s2=s2: e.reciprocal(out=s2[:, 2:3], in_=s2[:, 1:2]), [s2k], [s2k])
                    self.stt(tmb[:, 0:128], pa[:, 0:128], s2[:, 2:3], gkvb[:], ALU.mult, ALU.mult,
                             [pak, s2k, 'gkvb'], [tmbk])
                    self.ts('dve', iw_all[:, tt_i, :], pa[:, 128:132], 1.0 / 16, None, ALU.mult, None, [pak], ['iw_all'])
                    self.cp('act', tmb[:, 129:385], pa[:, 132:388], [pak], [tmbk])
                    pc, pck = ptc.next()
                    self.tr(pc[:], tmb[:, 0:128], self.identb[:], [tmbk, 'identb'], [pck])
                    self.cp('dve', ckvT[:, j * 128:(j + 1) * 128], pc[:], [pck], [ckvTk])
                    pb, pbk = psr.next()
                    for k in range(8):
                        self.mm(pb[:, 0:512], hT[:, k, j * 128:(j + 1) * 128], wtm[:, k, 388:900], k == 0, k == 7,
                                [hTk, ('wtm', k)], [pbk])
                    self.act(tmf[:, 0:256], pb[:, 0:256], AF.Silu, [pbk], [tmfk])
                    self.cp('dve', tmb[:, 385:641], pb[:, 256:512], [pbk], [tmbk])
                    pcc, pcck = psr.next()
                    for k in range(8):
                        self.mm(pcc[:, 0:256], hT[:, k, j * 128:(j + 1) * 128], wtm[:, k, 900:1156], k == 0, k == 7,
                                [hTk, ('wtm', k)], [pcck])
                    self.act(tmf[:, 256:512], pcc[:, 0:256], AF.Silu, [pcck], [tmfk])
                    S.dma('sp', self.tmb_d[t0:t0 + 128, :], tmb[:], reads=[tmbk], defer=True)
                    S.dma('sp', self.tmf_d[t0:t0 + 128, :], tmf[:], reads=[tmfk], defer=True)
                S.dma('sp', self.hT_d[:, :, tok0:tok0 + 512], hT[:], reads=[hTk], defer=True)
                S.dma('sp', self.ckvT_d[:, tok0:tok0 + 512], ckvT[:], reads=[ckvTk], defer=True)

                def fm_block(blk, M=128):
                    pf, pfk = psr.next()
                    for k in range(8):
                        self.mm(pf[0:M, :], wfm[:, k, blk * 128:blk * 128 + M], hT[:, k, :], k == 0, k == 7,
                                [hTk, ('wfm', k)], [pfk])
                    return pf, pfk

                def out_f(slot):
                    o, ok = ofr.next()
                    return o, ok, self.fmf_d[:, slot, tok0:tok0 + 512]

                def out_b(slot):
                    o, ok = obr.next()
                    return o, ok, self.fmb_d[:, slot, tok0:tok0 + 512]

                qT, qTk = qTr.next()
                for hp in range(2):
                    pf, pfk = fm_block(0 + hp)
                    self.cp('act', qT[:, hp, :], pf[:], [pfk], [qTk])
                for h in range(4):
                    hp, e_ = h // 2, h % 2
                    pf, pfk = psr.next()
                    self.mm(pf[:], wuk[e_ * 64:(e_ + 1) * 64, hp, :], qT[e_ * 64:(e_ + 1) * 64, hp, :], True, True,
                            ['wuk', qTk], [pfk])
                    o, ok, dst = out_b(3 + h)
                    self.ts('dve', o[:], pf[:], 0.125, None, ALU.mult, None, [pfk], [ok])
                    S.dma('sp', dst, o[:], reads=[ok], defer=True)
                for hp in range(2):
                    pf, pfk = fm_block(2 + hp)
                    o, ok, dst = out_b(0 + hp)
                    self.cp('act', o[:], pf[:], [pfk], [ok])
                    S.dma('sp', dst, o[:], reads=[ok], defer=True)
                pf, pfk = fm_block(4)
                o, ok, dst = out_b(2)
                self.cp('dve', o[:], pf[:], [pfk], [ok])
                S.dma('sp', dst, o[:], reads=[ok], defer=True)
                for hp in range(2):
                    pf, pfk = fm_block(5 + hp)
                    o, ok, dst = out_f(0 + hp)
                    self.ts('dve', o[:], pf[:], 0.125, None, ALU.mult, None, [pfk], [ok])
                    S.dma('sp', dst, o[:], reads=[ok], defer=True)
                for hp in range(2):
                    pf, pfk = fm_block(7 + hp)
                    o, ok, dst = out_f(2 + hp)
                    self.cp('act', o[:], pf[:], [pfk], [ok])
                    S.dma('sp', dst, o[:], reads=[ok], defer=True)
                pf, pfk = fm_block(9, M=16)
                blr, blrk = blrr.next()
                self.cp('dve', blr[:], pf[0:16, :], [pfk], [blrk])
                for hp in range(2):
                    pg, pgk = psr.next()
                    self.mm(pg[:], wgk[0:16, hp * 128:(hp + 1) * 128], blr[0:16, :], True, True, ['wgk', blrk], [pgk])
                    o, ok, dst = out_f(4 + hp)
                    self.act(o[:], pg[:], AF.Exp, [pgk, 'nbgk'], [ok], bias=nbgk[:, hp:hp + 1], scale=-1.0)
                    self.act(o[:], o[:], AF.Ln, [ok], [ok], bias=1.0)
                    self.ts('dve', o[:], o[:], -1.0 / 16, None, ALU.mult, None, [ok], [ok])
                    S.dma('sp', dst, o[:], reads=[ok], defer=True)
                for i_, slot0 in ((10, 6), (12, 8)):
                    for hp in range(2):
                        pf, pfk = fm_block(i_ + hp)
                        o, ok, dst = out_f(slot0 + hp)
                        self.cp('act' if hp else 'dve', o[:], pf[:], [pfk], [ok])
                        S.dma('sp', dst, o[:], reads=[ok], defer=True)
                for hp in range(2):
                    pf, pfk = fm_block(14 + hp)
                    o, ok, dst = out_f(10 + hp)
                    self.act(o[:], pf[:], AF.Silu, [pfk], [ok])
                    S.dma('sp', dst, o[:], reads=[ok], defer=True)
                for hp in range(2):
                    pf, pfk = fm_block(16 + hp)
                    o, ok, dst = out_f(12 + hp)
                    self.act(o[:], pf[:], AF.Sigmoid, [pfk], [ok], scale=-1.0)
                    self.ts('dve', o[:], o[:], lbc[:, 2 + hp:3 + hp], None, ALU.mult, None, [ok, 'lbc'], [ok])
                    S.dma('sp', dst, o[:], reads=[ok], defer=True)
                    o2, ok2, dst2 = out_f(14 + hp)
                    self.act(o2[:], pf[:], AF.Sigmoid, [pfk], [ok2])
                    self.ts('dve', o2[:], o2[:], lbc[:, 2 + hp:3 + hp], lbc[:, 4 + hp:5 + hp], ALU.mult, ALU.add,
                            [ok2, 'lbc'], [ok2])
                    self.act(o2[:], o2[:], AF.Ln, [ok2], [ok2])
                    S.dma('sp', dst2, o2[:], reads=[ok2], defer=True)
            S.dma('sp', self.iw_d, iw_all[:], reads=['iw_all'], defer=True)
            S.barrier()

    def build(self, phases=None):
        self.es = ExitStack()
        self.alloc_scratch()
        self.consts(self.es)
        self.S.barrier()
        x_src = self.x_in
        for l in range(self.depth):
            last = l == self.depth - 1
            def on(p):
                return phases is None or p in phases
            if on('p1'):
                self.phase1(l, x_src)
            if on('dsa'):
                self.phase_dsa(l)
            if on('gla') and on('hgrn'):
                self.phase_gla2(l)
            elif on('gla'):
                self.phase_gla2(l, (1,))
            elif on('hgrn'):
                self.phase_gla2(l, (3,))
            if on('lru'):
                self.phase_lru(l)
            if on('merge'):
                self.phase_merge(l, x_src)
            if on('ffn'):
                self.phase_ffn(l, self.out if last else self.xres_d, last)
            x_src = self.xres_d
        self.S.finish('sp')
        self.S.emit()
        self.es.close()
        return self.nc


def const_inputs():
    ident = np.eye(128, dtype=np.float32)
    j = np.arange(128)[:, None]
    i = np.arange(128)[None, :]
    cmask = (j <= i).astype(np.float32)
    bdmask = ((j <= i) & (j // 64 == i // 64)).astype(np.float32)
    scanm = np.ones((128, 128), np.float32)
    scanm[:, 0] = 0
    scanm[:, 64] = 0
    cneg = np.where(i <= j, 0.0, -1e30).astype(np.float32)
    return {'ident': ident, 'cmask': cmask, 'bdmask': bdmask, 'scanm': scanm, 'cneg': cneg}


def t5_bucket_np(dist):
    n = np.maximum(dist, 0)
    nf = np.maximum(n, 16).astype(np.float32)
    large = 16 + (np.log(nf / np.float32(16)) / np.float32(np.log(128 / 16)) * np.float32(16)).astype(np.int32)
    large = np.minimum(large, 31)
    return np.where(n < 16, n, large)


def bias_table(rel_bias):
    kk = np.arange(128)[:, None]
    qq = np.arange(128)[None, :]
    bp = t5_bucket_np(qq - kk + 128)
    bd = t5_bucket_np(qq - kk)
    idx = np.concatenate([bp, bd], axis=1)
    rb = np.asarray(rel_bias, np.float32)
    return np.ascontiguousarray(rb[idx].transpose(0, 2, 1))


def make_in_map(inp, b, depth=DEPTH):
    m = {'x': np.ascontiguousarray(inp['x'][b], dtype=np.float32)}
    for l in range(depth):
        for name in ('w_in', 'w_gate', 'w_uk', 'w_uv', 'w_gk2', 'w_rg_a', 'w_rg_x', 'w_br_a', 'w_br_b', 'w_br_c',
                     'w_br_d', 'w_out', 'w_ff1', 'w_ff2'):
            m[f"{name}{l}"] = np.ascontiguousarray(inp[name][l], dtype=np.float32)
        m[f"pvec{l}"] = pack_pvec(inp, l)
        m[f"bvec{l}"] = pack_bvec(inp, l)
    m['biasT'] = bias_table(inp['rel_bias'])
    m['relb'] = np.ascontiguousarray(inp['rel_bias'], dtype=np.float32)
    m.update(const_inputs())
    return m


def _phase_tools(self, es):
    nc = self.nc

    def sb(name, shape, dt):
        return es.enter_context(nc.sbuf_tensor(self.u(name), shape, dt))

    def pt(name, shape, dt):
        return es.enter_context(nc.psum_tensor(self.u(name), shape, dt))

    def ring(name, n, shape, dt, psum=False):
        tiles = []
        for i in range(n):
            nm = self.u(name)
            t = pt(nm, shape, dt) if psum else sb(nm, shape, dt)
            tiles.append((t, nm))
        r = Ring.__new__(Ring)
        r.tiles, r.i = tiles, 0
        return r
    return sb, pt, ring


NBIS = 16


def phase_dsa(self, l):
    nc, S, T, NT = self.nc, self.S, self.T, self.NT
    W = self.w[l]
    topk = min(256, T // 4)
    with ExitStack() as es:
        sb, pt, ring = _phase_tools(self, es)
        ckvT = sb('ckvT', [128, T], BF16)
        ckv1 = sb('ckv1', [128, NT, 129], BF16)
        ikT = sb('ikT', [128, T], BF16)
        iw = sb('iw', [128, NT, 4], F32)
        wuv = sb('wuv', [128, 4, 64], BF16)
        I4 = sb('I4', [128, 4, 128], BF16)
        cneg = sb('cneg', [128, 128], F32)
        bT = sb('bT', [128, 4, 256], F32)
        bhi = sb('bhi', [128, 4, 256], BF16)
        blo = sb('blo', [128, 4, 256], BF16)
        bhf = sb('bhf', [128, 4, 256], F32)
        b31 = sb('b31', [128, 4], F32)
        stage = ring('stg', 2, [128, 256], F32)
        S.dma('sp', ckvT[:], self.ckvT_d, writes=['ckvT'])
        S.dma('sp', ckv1[:], self.tmb_d[:, 0:129].rearrange("(t p) c -> p t c", p=128), writes=['ckv1'])
        S.dma('sp', ikT[:], self.fmb_d[:, 2, :], writes=['ikT'])
        S.dma('sp', iw[:], self.iw_d, writes=['iw'])
        S.dma('sp', cneg[:], self.cneg_in, writes=['cneg'])
        S.dma('sp', bT[:], self.biasT, writes=['bT'])
        S.dma('sp', b31[:], self.relb_in[31:32, :].partition_broadcast(128), writes=['b31'])
        self.load_cast(stage, wuv[:], 'wuv', W['w_uv'].rearrange("h c d -> c h d"), [4, 64])
        for h in range(4):
            self.cp('pool', I4[:, h, :], self.identb[:], ['identb'], ['I4'])
            self.ts('dve', bT[:, h, :], bT[:, h, :], b31[:, h:h + 1], None, ALU.subtract, None, ['bT', 'b31'], ['bT'])
        self.cp('dve', bhi[:], bT[:], ['bT'], ['bhi'])
        self.cp('dve', bhf[:], bhi[:], ['bhi'], ['bhf'])
        self.tt('dve', bhf[:], bT[:], bhf[:], ALU.subtract, ['bT', 'bhf'], ['bhf'])
        self.cp('dve', blo[:], bhf[:], ['bhf'], ['blo'])

        G = 2
        iqr = ring('iq', 3, [128, 2, 128], BF16)
        qlr = ring('ql', 3 * G, [128, 4, 128], BF16)
        scr = ring('sc', 2 * G, [128, T], F32)
        mnr = ring('mn', 2 * G, [128, T], BF16)
        junk = sb('junkb', [128, T], BF16)
        rlr = ring('rl', 3, [128, 512], F32)
        bs = ring('bs', 3, [128, 8, G], F32)
        pss = ring('pss', 2, [128, 512], F32, psum=True)
        psl = ring('psl', 2, [128, 512], F32, psum=True)
        poA = ring('poA', 1, [128, 2, 129], F32, psum=True)
        poB = ring('poB', 1, [128, 2, 129], F32, psum=True)
        ptr = ring('ptr', 1, [128, 4, 128], BF16, psum=True)
        pyr = ring('py', 1, [128, 2, 128], F32, psum=True)
        pr_ = ring('p', 3, [128, 512], BF16)
        olr = ring('ol', 2, [128, 4, 128], BF16)
        olTr = ring('olT', 2, [128, 4, 128], BF16)
        yr = ring('ya', 2, [128, 2, 128], BF16)
        rdr = ring('rd', 2, [128, 4], F32)
        NG = (NT + G - 1) // G
        info = {}

        def gen_scores(g):
            tiles = list(range(g * G, min(NT, g * G + G)))
            b, bk = bs.next()
            grp = []
            info[g] = (b, bk, grp)
            for gi, qi in enumerate(tiles):
                q0 = qi * 128
                nk = (qi + 1) * 128
                iq, iqk = iqr.next()
                ql, qlk = qlr.next()
                S.dma('sp', iq[:], self.fmb_d[:, 0:2, q0:q0 + 128], writes=[iqk])
                S.dma('sp', ql[:], self.fmb_d[:, 3:7, q0:q0 + 128], writes=[qlk])
                sc, sck = scr.next()
                mn, mnk = mnr.next()
                grp.append((qi, q0, nk, ql, qlk, sc, sck, mn, mnk))
                for k0 in range(0, nk, 512):
                    n = min(512, nk - k0)
                    for h in range(4):
                        hp, e_ = h // 2, h % 2
                        ps, psk = pss.next()
                        self.mm(ps[:, 0:n], iq[e_ * 64:(e_ + 1) * 64, hp, :], ikT[e_ * 64:(e_ + 1) * 64, k0:k0 + n],
                                True, True, [iqk, 'ikT'], [psk])
                        if h == 0:
                            self.ts('dve', sc[:, k0:k0 + n], ps[:, 0:n], 0.0, iw[:, qi, 0:1], ALU.max, ALU.mult,
                                    [psk, 'iw'], [sck])
                        else:
                            rl, rlk = rlr.next()
                            self.act(rl[:, 0:n], ps[:, 0:n], AF.Relu, [psk], [rlk])
                            self.stt(sc[:, k0:k0 + n], rl[:, 0:n], iw[:, qi, h:h + 1], sc[:, k0:k0 + n], ALU.mult, ALU.add,
                                     [rlk, 'iw', sck], [sck])
                    yield
                S.op('dve', lambda e, b=b, sc=sc, nk=nk, gi=gi: e.tensor_reduce(
                    out=b[:, 0, gi:gi + 1], in_=sc[:, 0:nk], axis=AX.X, op=ALU.min), [sck], [(bk, 'mn', gi)])
                S.op('dve', lambda e, b=b, sc=sc, nk=nk, gi=gi: e.tensor_reduce(
                    out=b[:, 1, gi:gi + 1], in_=sc[:, 0:nk], axis=AX.X, op=ALU.max), [sck], [(bk, 'mx', gi)])
                self.tt('dve', sc[:, q0:q0 + 128], sc[:, q0:q0 + 128], cneg[:], ALU.add, [sck, 'cneg'], [sck])
                yield

        def n_scores(g):
            return sum(((qi + 1) * 128 + 511) // 512 + 1 for qi in range(g * G, min(NT, g * G + G)))

        def gen_attend(g):
            for (qi, q0, nk, ql, qlk, sc, sck, mn, mnk) in info[g][2]:
                yield from self.dsa_attend(qi, q0, ql, qlk, mn, mnk, ckvT, ckv1, I4, bhi, blo, wuv,
                                           psl, poA, poB, ptr, pyr, pr_, olr, olTr, yr, rdr)

        def n_attend(g):
            return sum(qi + 2 for qi in range(g * G, min(NT, g * G + G)))

        def advance(gen, n):
            if gen is None:
                return None
            for _ in range(n):
                try:
                    next(gen)
                except StopIteration:
                    return None
            return gen

        advance(gen_scores(0), 10 ** 9)
        for g in range(NG):
            b, bk, grp = info[g]
            ng = len(grp)
            A = gen_scores(g + 1) if g + 1 < NG else None
            C = gen_attend(g - 1) if g >= 1 else None
            stepA = (n_scores(g + 1) + NBIS - 1) // NBIS if A is not None else 0
            stepC = (n_attend(g - 1) + NBIS - 1) // NBIS if C is not None else 0
            gs = slice(0, ng)
            kmm, kR, kC, kU, kT, kH = (bk, 'mm'), (bk, 'R'), (bk, 'cand'), (bk, 'u'), (bk, 'tmp'), (bk, 'thr')
            kin = [(bk, 'mn', gi) for gi in range(ng)] + [(bk, 'mx', gi) for gi in range(ng)]
            self.ts('dve', b[:, 0, gs], b[:, 0, gs], -1.0, None, ALU.add, None, kin, [kmm])
            self.tt('dve', b[:, 2, gs], b[:, 1, gs], b[:, 0, gs], ALU.subtract, kin + [kmm], [kR])
            self.stt(b[:, 3, gs], b[:, 2, gs], 0.5, b[:, 0, gs], ALU.mult, ALU.add, [kR, kmm], [kC])
            for it in range(NBIS):
                ci = 0.5 ** (it + 1)
                for gi, (qi, q0, nk, ql, qlk, sc, sck, mn, mnk) in enumerate(grp):
                    self.ts('dve', junk[:, 0:nk], sc[:, 0:nk], b[:, 3, gi:gi + 1], 0.0, ALU.is_ge, ALU.add,
                            [sck, kC], [(bk, 'cnt', gi)], accum=b[:, 4, gi:gi + 1])
                self.ts('dve', b[:, 5, gs], b[:, 4, gs], float(topk), -0.5, ALU.is_ge, ALU.add,
                        [(bk, 'cnt', gi) for gi in range(ng)], [kU])
                self.tt('dve', b[:, 6, gs], b[:, 5, gs], b[:, 2, gs], ALU.mult, [kU, kR], [kT])
                self.stt(b[:, 3, gs], b[:, 6, gs], ci, b[:, 3, gs], ALU.mult, ALU.add, [kT, kC], [kC])
                A = advance(A, stepA)
                C = advance(C, stepC)
            advance(A, 10 ** 9)
            advance(C, 10 ** 9)
            self.stt(b[:, 7, gs], b[:, 2, gs], -(0.5 ** (NBIS + 1)), b[:, 3, gs], ALU.mult, ALU.add, [kR, kC], [kH])
            for gi, (qi, q0, nk, ql, qlk, sc, sck, mn, mnk) in enumerate(grp):
                self.ts('dve', mn[:, 0:nk], sc[:, 0:nk], b[:, 7, gi:gi + 1], -30000.0, ALU.is_lt, ALU.mult,
                        [sck, kH], [mnk])
        advance(gen_attend(NG - 1), 10 ** 9)
        S.barrier()


def dsa_attend(self, qi, q0, ql, qlk, mn, mnk, ckvT, ckv1, I4, bhi, blo, wuv,
               psl, poA, poB, ptr, pyr, pr_, olr, olTr, yr, rdr):
    S = self.S
    if True:
        if True:
            pa, pak = poA.next()
            pb, pbk = poB.next()
            qlf = ql[:].rearrange("p h q -> p (h q)")
            for kb in range(qi + 1):
                pl, plk = psl.next()
                near = kb >= qi - 1
                self.mm(pl[:], ckvT[:, kb * 128:(kb + 1) * 128], qlf, True, False, ['ckvT', qlk], [plk])
                self.mm(pl[:], mn[:, kb * 128:(kb + 1) * 128], I4[:].rearrange("p h q -> p (h q)"), False, not near,
                        [mnk, 'I4'], [plk])
                if near:
                    o_ = 128 if kb == qi else 0
                    for bt_, bkey, last in ((bhi, 'bhi', False), (blo, 'blo', True)):
                        for h in range(4):
                            self.mm(pl[:, h * 128:(h + 1) * 128], self.identb[:], bt_[:, h, o_:o_ + 128], False,
                                    last and h == 3, ['identb', bkey], [plk])
                p, pk = pr_.next()
                self.act(p[:], pl[:], AF.Exp, [plk], [pk])
                for h in range(4):
                    po, pok = (pa, pak) if h < 2 else (pb, pbk)
                    self.mm(po[:, h % 2, :], p[:, h * 128:(h + 1) * 128], ckv1[:, kb, :], kb == 0 and h % 2 == 0, kb == qi,
                            [pk, 'ckv1'], [pok])
                yield
            rd, rdk = rdr.next()
            ol, olk = olr.next()
            for h in range(4):
                po, pok = (pa, pak) if h < 2 else (pb, pbk)
                S.op('dve', lambda e, rd=rd, po=po, h=h: e.reciprocal(out=rd[:, h:h + 1], in_=po[:, h % 2, 128:129]),
                     [pok], [rdk])
                self.ts('dve', ol[:, h, :], po[:, h % 2, 0:128], rd[:, h:h + 1], None, ALU.mult, None, [pok, rdk], [olk])
            ptt, pttk = ptr.next()
            for h in range(4):
                self.tr(ptt[:, h, :], ol[:, h, :], self.identb[:], [olk, 'identb'], [pttk])
            olT, olTk = olTr.next()
            self.cp('act', olT[:], ptt[:], [pttk], [olTk])
            py, pyk = pyr.next()
            for h in range(4):
                hp, e_ = h // 2, h % 2
                self.mm(py[e_ * 64:(e_ + 1) * 64, hp, :], wuv[:, h, :], olT[:, h, :], True, True, ['wuv', olTk], [pyk])
            ya, yak = yr.next()
            self.cp('act', ya[:], py[:], [pyk], [yak])
            S.dma('sp', self.yT_d[:, 0:2, q0:q0 + 128], ya[:], reads=[yak], defer=True)
            yield


Prog.dsa_attend = dsa_attend
Prog.phase_dsa = phase_dsa


def phase_gla(self, l, br, shared):
    nc, S, T, NT = self.nc, self.S, self.T, self.NT
    W = self.w[l]
    if br == 1:
        sq_, sk_, sg_, vcol, gcol, bvo = 0, 2, 4, 129, 0, BV_GLA
    else:
        sq_, sk_, sg_, vcol, gcol, bvo = 10, 12, 14, 385, 256, BV_HG
    with ExitStack() as es:
        sb, pt, ring = _phase_tools(self, es)
        scanm = sb('scanm', [128, 128], F32)
        bdm = sb('bdm', [128, 2, 128], F32)
        gnb = sb('gnb', [128, 256], F32)
        Sf = sb('Sf', [128, 2, 64], F32)
        S.dma('sp', scanm[:], self.scanm_in, writes=['scanm'])
        S.dma('sp', bdm[:, 0, :], self.bdmask_in, writes=['bdm'])
        S.dma('sp', bdm[:, 1, :], self.bdmask_in, writes=['bdm'])
        S.dma('sp', gnb[:], W['bvec'][:, bvo:bvo + 256].partition_broadcast(128), writes=['gnb'])
        S.op('pool', lambda e: e.memset(Sf[:], 0.0), [], ['Sf'])
        Sbr = ring('Sb', 4, [128, 2, 64], BF16)
        Sb, Sbk = Sbr.next()
        S.op('pool', lambda e, Sb=Sb: e.memset(Sb[:], 0.0), [], [Sbk])
        qr = ring('q', 2, [128, 2, 128], F32)
        kr = ring('k', 2, [128, 2, 128], F32)
        gr = ring('g', 2, [128, 2, 128], F32)
        vr = ring('v', 2, [128, 256], BF16)
        gtr = ring('gt', 2, [128, 256], F32)
        br_ = ring('b', 2, [128, 128], F32)
        bmr = ring('bm', 2, [128, 128], F32)
        e3r = ring('e3', 2, [128, 3, 128], F32)
        qer = ring('qe', 2, [128, 128], BF16)
        ker = ring('ke', 2, [128, 128], BF16)
        q1r = ring('q1', 2, [128, 128], BF16)
        ebr = ring('eb', 2, [128, 4], F32)
        atr = ring('at', 2, [128, 2, 128], BF16)
        ktr = ring('kt', 2, [128, 128], BF16)
        tmr = ring('tm', 2, [128, 64], F32)
        jk = sb('jk', [128, 256], F32)
        ssr = ring('ss', 2, [128, 12], F32)
        yr = ring('y', 2, [128, 256], F32)
        ybr = ring('yb', 2, [128, 256], BF16)
        yTr = ring('yT', 2, [128, 2, 128], BF16)
        patE, pkt, pdsC, porE, pyt = shared

        for ti in range(NT):
            t0 = ti * 128
            q, qk = qr.next()
            k, kk = kr.next()
            g, gk = gr.next()
            v, vk = vr.next()
            gt, gtk = gtr.next()
            S.dma('sp', g[:], self.fmf_d[:, sg_:sg_ + 2, t0:t0 + 128], writes=[gk])
            S.dma('sp', q[:], self.fmf_d[:, sq_:sq_ + 2, t0:t0 + 128], writes=[qk])
            S.dma('sp', k[:], self.fmf_d[:, sk_:sk_ + 2, t0:t0 + 128], writes=[kk])
            S.dma('sp', v[:], self.tmb_d[t0:t0 + 128, vcol:vcol + 256], writes=[vk])
            S.dma('sp', gt[:], self.tmf_d[t0:t0 + 128, gcol:gcol + 256], writes=[gtk])
            poE = [porE[0].next(), porE[1].next()]
            Sb0, Sb0k = Sb, Sbk
            Sb1, Sb1k = Sbr.next()
            Sb2, Sb2k = Sbr.next()
            for hp in range(2):
                b, bk = br_.next()
                bm, bmk = bmr.next()
                S.op('dve', lambda e, b=b, g=g, hp=hp: e.tensor_tensor_scan(
                    out=b[:], data0=scanm[:], data1=g[:, hp, :], initial=0.0, op0=ALU.mult, op1=ALU.add),
                    ['scanm', gk], [bk])
                for c in range(2):
                    self.ts('dve', bm[:, c * 64:(c + 1) * 64], b[:, c * 64:(c + 1) * 64], b[:, c * 64 + 31:c * 64 + 32],
                            None, ALU.subtract, None, [bk], [bmk])
                e3, e3k = e3r.next()
                self.act(e3[:, 0, :], bm[:], AF.Exp, [bmk], [e3k])
                self.act(e3[:, 1, :], bm[:], AF.Exp, [bmk], [e3k], scale=-1.0)
                self.act(e3[:, 2, :], b[:], AF.Exp, [bk], [e3k])
                eb, ebk = ebr.next()
                for c in range(2):
                    self.act(eb[:, c:c + 1], b[:, c * 64 + 63:c * 64 + 64], AF.Exp, [bk], [ebk])
                    self.act(eb[:, 2 + c:3 + c], bm[:, c * 64 + 63:c * 64 + 64], AF.Exp, [bmk], [ebk])
                qe, qek = qer.next()
                ke, kek = ker.next()
                q1, q1k = q1r.next()
                self.tt('dve', qe[:], q[:, hp, :], e3[:, 0, :], ALU.mult, [qk, e3k], [qek])
                self.tt('pool', ke[:], k[:, hp, :], e3[:, 1, :], ALU.mult, [kk, e3k], [kek])
                self.tt('pool', q1[:], q[:, hp, :], e3[:, 2, :], ALU.mult, [qk, e3k], [q1k])
                at, atk = atr.next()
                for e_ in range(2):
                    sl = slice(e_ * 64, (e_ + 1) * 64)
                    pa, pak = patE[e_].next()
                    self.mm(pa[:, 0:128], ke[sl, :], qe[sl, :], True, True, [kek, qek], [pak])
                    self.tt('dve', at[:, e_, :], pa[:, 0:128], bdm[:, 0, :], ALU.mult, [pak, 'bdm'], [(atk, e_)])
                pk_, pkk = pkt.next()
                self.tr(pk_[:, 0:128], ke[:], self.identb[:], [kek, 'identb'], [pkk])
                kt, ktk = ktr.next()
                self.cp('act', kt[:], pk_[:, 0:128], [pkk], [ktk])
                pdc = [pdsC[0].next(), pdsC[1].next()]
                for c in range(2):
                    cs = slice(c * 64, (c + 1) * 64)
                    pd, pdk = pdc[c]
                    for e_ in range(2):
                        sl = slice(e_ * 64, (e_ + 1) * 64)
                        h = hp * 2 + e_
                        self.mm(pd[sl, 0:64], kt[cs, sl], v[cs, h * 64:(h + 1) * 64], True, True, [ktk, vk], [pdk])
                for c, (Sn, Snk) in enumerate(((Sb1, Sb1k), (Sb2, Sb2k))):
                    tm, tmk = tmr.next()
                    pd, pdk = pdc[c]
                    self.ts('dve', tm[:], pd[:, 0:64], eb[:, 2 + c:3 + c], None, ALU.mult, None, [pdk, ebk], [tmk])
                    self.stt(Sf[:, hp, :], Sf[:, hp, :], eb[:, c:c + 1], tm[:], ALU.mult, ALU.add, ['Sf', ebk, tmk], ['Sf'])
                    self.cp('act', Sn[:, hp, :], Sf[:, hp, :], ['Sf'], [Snk])
                for e_ in range(2):
                    sl = slice(e_ * 64, (e_ + 1) * 64)
                    h = hp * 2 + e_
                    hs = slice(h * 64, (h + 1) * 64)
                    po, pok = poE[e_]
                    ps_ = slice(hp * 64, (hp + 1) * 64)
                    self.mm(po[:, ps_], at[:, e_, :], v[:, hs], True, False, [(atk, e_), vk], [pok])
                    self.mm(po[0:64, ps_], q1[sl, 0:64], Sb0[sl, hp, :], False, False, [q1k, Sb0k], [pok])
                    self.mm(po[64:128, ps_], q1[sl, 64:128], Sb1[sl, hp, :], False, True, [q1k, Sb1k], [pok])
            Sb, Sbk = Sb2, Sb2k
            ss, ssk = ssr.next()
            for e_ in range(2):
                self.act(jk[:, e_ * 128:(e_ + 1) * 128], poE[e_][0][:, 0:128], AF.Square, [poE[e_][1]], ['jk'])
            S.op('dve', lambda e, ss=ss: e.tensor_reduce(out=ss[:, 0:4], in_=jk[:].rearrange("p (h v) -> p h v", h=4),
                                                         axis=AX.X, op=ALU.add), ['jk'], [ssk])
            self.act(ss[:, 4:8], ss[:, 0:4], AF.Sqrt, [ssk], [ssk], bias=EPS, scale=1.0 / 64)
            S.op('dve', lambda e, ss=ss: e.reciprocal(out=ss[:, 8:12], in_=ss[:, 4:8]), [ssk], [ssk])
            y, yk = yr.next()
            for h in range(4):
                hs = slice(h * 64, (h + 1) * 64)
                hp, e_ = h // 2, h % 2
                si = 8 + e_ * 2 + hp
                self.stt(y[:, hs], poE[e_][0][:, hp * 64:(hp + 1) * 64], ss[:, si:si + 1], gnb[:, hs], ALU.mult, ALU.mult,
                         [poE[e_][1], ssk, 'gnb'], [yk])
            yb, ybk = ybr.next()
            self.tt('pool', yb[:], y[:], gt[:], ALU.mult, [yk, gtk], [ybk])
            py, pyk = pyt.next()
            for hp in range(2):
                self.tr(py[:, hp, :], yb[:, hp * 128:(hp + 1) * 128], self.identb[:], [ybk, 'identb'], [pyk])
            yT, yTk = yTr.next()
            self.cp('act', yT[:], py[:, 0:2, :], [pyk], [yTk])
            S.dma('sp', self.yT_d[:, br * 2:br * 2 + 2, t0:t0 + 128], yT[:], reads=[yTk], defer=True)
            yield
        yield


def phase_gla2(self, l, which=(1, 3)):
    S, NT = self.S, self.NT
    with ExitStack() as es:
        sb, pt, ring = _phase_tools(self, es)
        shared = ([ring('pat', 1, [128, 512], F32, psum=True) for _ in range(2)],
                  ring('pkt', 1, [128, 1024], BF16, psum=True),
                  [ring('pds', 1, [128, 512], F32, psum=True) for _ in range(2)],
                  [ring('po', 1, [128, 512], F32, psum=True) for _ in range(2)],
                  ring('pyt', 1, [128, 8, 128], BF16, psum=True))
        gens = [self.phase_gla(l, br, shared) for br in which]
        for ti in range(NT):
            for g_ in gens:
                next(g_)
        for g_ in gens:
            next(g_)
        for g_ in reversed(gens):
            for _ in g_:
                pass
        S.barrier()


Prog.phase_gla2 = phase_gla2
Prog.phase_gla = phase_gla


def phase_lru(self, l):
    nc, S, T = self.nc, self.S, self.T
    W = self.w[l]
    with ExitStack() as es:
        sb, pt, ring = _phase_tools(self, es)
        pv = sb('pv', [128, NPV], F32)
        cc_ = sb('cc', [128, 8], F32)
        wa = sb('wa', [128, 2, 128], BF16)
        wx = sb('wx', [128, 2, 128], BF16)
        stage = ring('stg', 2, [128, 64], F32)
        S.dma('sp', pv[:], W['pvec'], writes=['pv'])
        self.act(cc_[:, 0:2], pv[:, 20:22], AF.Exp, ['pv'], ['cc'], scale=-1.0)
        self.act(cc_[:, 0:2], cc_[:, 0:2], AF.Ln, ['cc'], ['cc'], bias=1.0)
        self.ts('dve', cc_[:, 2:4], cc_[:, 0:2], -8.0, None, ALU.mult, None, ['cc'], ['cc'])
        self.ts('dve', cc_[:, 4:6], cc_[:, 0:2], -16.0, None, ALU.mult, None, ['cc'], ['cc'])
        S.op('pool', lambda e: e.memset(wa[:], 0.0), [], ['wa'])
        S.op('pool', lambda e: e.memset(wx[:], 0.0), [], ['wx'])
        for n in range(4):
            cc, e_ = n // 2, n % 2
            sl = slice(e_ * 64, (e_ + 1) * 64)
            self.load_cast(stage, wa[sl, cc, e_ * 64:(e_ + 1) * 64], 'wa', W['w_rg_a'][n], [64], pbase=e_ * 64)
            self.load_cast(stage, wx[sl, cc, e_ * 64:(e_ + 1) * 64], 'wx', W['w_rg_x'][n], [64], pbase=e_ * 64)
        LV = int(os.environ.get('DBG_LRU', '9'))
        cxr = [ring('cx', 2, [128, 515], F32) for _ in range(2)]
        hhr = [ring('hh', 2, [128, 512], F32) for _ in range(2)]
        cyr = ring('cy', 2, [128, 512], F32)
        xcr = ring('xc', 2, [128, 512], F32)
        xbr = ring('xb', 2, [128, 512], BF16)
        rr = ring('r', 2, [128, 512], F32)
        ir = ring('i', 2, [128, 512], F32)
        ar = ring('a', 2, [128, 512], F32)
        a2r = ring('a2', 2, [128, 512], F32)
        ur = ring('u', 2, [128, 512], F32)
        gr = ring('gg', 2, [128, 512], F32)
        g2r = ring('g2', 2, [128, 512], F32)
        ycr = ring('yc', 2, [128, 512], BF16)
        psr = ring('ps', 4, [128, 512], F32, psum=True)
        prev = [None, None]
        prevh = [None, None]
        for bi in range(T // 512 if LV > 0 else 0):
            tok0 = bi * 512
            for cc in range(2):
                cx, cxk = cxr[cc].next()
                cy, cyk = cyr.next()
                S.dma('sp', cx[:, 3:515], self.fmf_d[:, 6 + cc, tok0:tok0 + 512], writes=[cxk])
                S.dma('sp', cy[:], self.fmf_d[:, 8 + cc, tok0:tok0 + 512], writes=[cyk])
                if prev[cc] is None:
                    S.op('pool', lambda e, cx=cx: e.memset(cx[:, 0:3], 0.0), [], [cxk])
                else:
                    self.cp('pool', cx[:, 0:3], prev[cc][0][:, 512:515], [prev[cc][1]], [cxk])
                prev[cc] = (cx, cxk)
                xc, xck = xcr.next()
                self.ts('dve', xc[:], cx[:, 3:515], pv[:, 14 + cc:15 + cc], pv[:, 6 + cc:7 + cc], ALU.mult, ALU.add,
                        [cxk, 'pv'], [xck])
                for j in range(3):
                    self.stt(xc[:], cx[:, j:j + 512], pv[:, 8 + 2 * j + cc:9 + 2 * j + cc], xc[:], ALU.mult, ALU.add,
                             [cxk, 'pv', xck], [xck])
                if LV < 2:
                    continue
                xb, xbk = xbr.next()
                self.cp('act', xb[:], xc[:], [xck], [xbk])
                p1, p1k = psr.next()
                p2, p2k = psr.next()
                self.mm(p1[:], wa[:, cc, :], xb[:], True, True, ['wa', xbk], [p1k])
                self.mm(p2[:], wx[:, cc, :], xb[:], True, True, ['wx', xbk], [p2k])
                r, rk = rr.next()
                i_, ik = ir.next()
                self.act(r[:], p1[:], AF.Sigmoid, [p1k, 'pv'], [rk], bias=pv[:, 16 + cc:17 + cc])
                self.act(i_[:], p2[:], AF.Sigmoid, [p2k, 'pv'], [ik], bias=pv[:, 18 + cc:19 + cc])
                a, ak = ar.next()
                a2, a2k = a2r.next()
                self.act(a[:], r[:], AF.Exp, [rk, 'cc'], [ak], scale=cc_[:, 2 + cc:3 + cc])
                self.act(a2[:], r[:], AF.Exp, [rk, 'cc'], [a2k], scale=cc_[:, 4 + cc:5 + cc])
                self.act(a2[:], a2[:], AF.Sqrt, [a2k], [a2k], bias=1.0, scale=-1.0)
                u, uk = ur.next()
                self.tt('dve', u[:], a2[:], i_[:], ALU.mult, [a2k, ik], [uk])
                self.tt('pool', u[:], u[:], xc[:], ALU.mult, [uk, xck], [uk])
                if LV < 3:
                    continue
                hh, hhk = hhr[cc].next()
                init = 0.0 if prevh[cc] is None else prevh[cc][0][:, 511:512]
                rd = [ak, uk] + ([] if prevh[cc] is None else [prevh[cc][1]])
                S.op('dve', lambda e, hh=hh, a=a, u=u, init=init: e.tensor_tensor_scan(
                    out=hh[:], data0=a[:], data1=u[:], initial=init, op0=ALU.mult, op1=ALU.add), rd, [hhk])
                prevh[cc] = (hh, hhk)
                if LV < 4:
                    continue
                g, gk = gr.next()
                g2, g2k = g2r.next()
                self.tt('pool', g[:], cy[:], cy[:], ALU.mult, [cyk], [gk])
                self.ts('dve', g[:], g[:], 0.044715, 1.0, ALU.mult, ALU.add, [gk], [gk])
                self.tt('pool', g[:], g[:], cy[:], ALU.mult, [gk, cyk], [gk])
                self.act(g2[:], g[:], AF.Sigmoid, [gk], [g2k], scale=1.5957691216057308)
                self.tt('dve', g2[:], g2[:], cy[:], ALU.mult, [g2k, cyk], [g2k])
                yc, yck = ycr.next()
                self.tt('pool', yc[:], g2[:], hh[:], ALU.mult, [g2k, hhk], [yck])
                S.dma('sp', self.yT_d[:, 4 + cc, tok0:tok0 + 512], yc[:], reads=[yck], defer=True)
        S.barrier()


Prog.phase_lru = phase_lru


def phase_merge(self, l, x_src):
    nc, S, T, NT = self.nc, self.S, self.T, self.NT
    W = self.w[l]
    with ExitStack() as es:
        sb, pt, ring = _phase_tools(self, es)
        wg = sb('wg', [128, 8, 4096], BF16)
        wbr = sb('wbr', [128, 8, 1024], BF16)
        wo = sb('wo', [128, 8, 1024], BF16)
        bgb = sb('bgb', [128, 4096], F32)
        stage = ring('stg', 2, [128, 2048], F32)
        S.dma('sp', bgb[:], W['bvec'][:, BV_BG:BV_BG + 4096].partition_broadcast(128), writes=['bgb'])
        for k in range(8):
            for c0 in (0, 2048):
                self.load_cast(stage, wg[:, k, c0:c0 + 2048], ('wg', k), W['w_gate'][k * 128:(k + 1) * 128, c0:c0 + 2048], [2048])
            self.load_cast(stage, wo[:, k, :], ('wo', k), W['w_out'][k * 128:(k + 1) * 128, :], [1024])
        for b_, nm in enumerate(('w_br_a', 'w_br_b', 'w_br_c', 'w_br_d')):
            for kk in range(2):
                self.load_cast(stage, wbr[:, b_ * 2 + kk, :], ('wbr', b_), W[nm][kk * 128:(kk + 1) * 128, :], [1024])
        hr = ring('hT', 2, [128, 8, 128], BF16)
        yr = ring('yT', 2, [128, 8, 128], BF16)
        xr = ring('x', 2, [128, 1024], F32)
        mr = ring('m', 2, [128, 1024], F32)
        mbr = ring('mb', 2, [128, 1024], BF16)
        mTr = ring('mT', 2, [128, 8, 128], BF16)
        gsr = ring('gs', 3, [128, 512], F32)
        tpr = ring('tp', 2, [128, 512], F32)
        xor_ = ring('xo', 2, [128, 1024], F32)
        pgr = ring('pg', 3, [128, 512], F32, psum=True)
        pyr = ring('py', 2, [128, 512], F32, psum=True)
        ptr = ring('pt', 1, [128, 8, 128], BF16, psum=True)
        for ti in range(NT):
            t0 = ti * 128
            hT, hTk = hr.next()
            yT, yTk = yr.next()
            x, xk = xr.next()
            S.dma('sp', hT[:], self.hT_d[:, :, t0:t0 + 128], writes=[hTk])
            S.dma('sp', yT[:], self.yT_d[:, :, t0:t0 + 128], writes=[yTk])
            S.dma('sp', x[:], x_src[t0:t0 + 128, :], writes=[xk])
            m, mk = mr.next()
            for b_ in range(4):
                for cb in range(2):
                    c0 = b_ * 1024 + cb * 512
                    pg, pgk = pgr.next()
                    for k in range(8):
                        self.mm(pg[:], hT[:, k, :], wg[:, k, c0:c0 + 512], k == 0, k == 7, [hTk, ('wg', k)], [pgk])
                    py, pyk = pyr.next()
                    for kk in range(2):
                        self.mm(py[:], yT[:, b_ * 2 + kk, :], wbr[:, b_ * 2 + kk, cb * 512:(cb + 1) * 512], kk == 0, kk == 1,
                                [yTk, ('wbr', b_)], [pyk])
                    gs, gsk = gsr.next()
                    self.tt('dve', gs[:], pg[:], bgb[:, c0:c0 + 512], ALU.add, [pgk, 'bgb'], [gsk])
                    self.act(gs[:], gs[:], AF.Sigmoid, [gsk], [gsk])
                    ms = m[:, cb * 512:(cb + 1) * 512]
                    if b_ == 0:
                        self.tt('dve', ms, gs[:], py[:], ALU.mult, [gsk, pyk], [(mk, cb)])
                    else:
                        tp, tpk = tpr.next()
                        self.tt('dve', tp[:], gs[:], py[:], ALU.mult, [gsk, pyk], [tpk])
                        self.tt('pool', ms, ms, tp[:], ALU.add, [(mk, cb), tpk], [(mk, cb)])
            mb, mbk = mbr.next()
            self.cp('act', mb[:], m[:], [(mk, 0), (mk, 1)], [mbk])
            p, pk = ptr.next()
            for k in range(8):
                self.tr(p[:, k, :], mb[:, k * 128:(k + 1) * 128], self.identb[:], [mbk, 'identb'], [pk])
            mT, mTk = mTr.next()
            self.cp('act', mT[:], p[:], [pk], [mTk])
            xo, xok = xor_.next()
            for cb in range(2):
                po, pok = pgr.next()
                for k in range(8):
                    self.mm(po[:], mT[:, k, :], wo[:, k, cb * 512:(cb + 1) * 512], k == 0, k == 7, [mTk, ('wo', k)], [pok])
                self.tt('dve', xo[:, cb * 512:(cb + 1) * 512], x[:, cb * 512:(cb + 1) * 512], po[:], ALU.add,
                        [xk, pok], [xok])
            S.dma('sp', self.xmid_d[t0:t0 + 128, :], xo[:], reads=[xok], defer=True)
        S.barrier()


Prog.phase_merge = phase_merge


def phase_ffn(self, l, x_dst, final):
    nc, S, T = self.nc, self.S, self.T
    W = self.w[l]
    NS = 256
    with ExitStack() as es:
        sb, pt, ring = _phase_tools(self, es)
        wf1 = sb('wf1', [128, 8, 4096], BF16)
        wf2 = sb('wf2', [128, 32, 1024], BF16)
        g2b = sb('g2b', [128, 1024], F32)
        gfb = sb('gfb', [128, 1024], F32)
        stage = ring('stg', 2, [128, 1024], F32)
        S.dma('sp', g2b[:], W['bvec'][:, BV_N2:BV_N2 + 1024].partition_broadcast(128), writes=['g2b'])
        S.dma('sp', gfb[:], W['bvec'][:, BV_FN:BV_FN + 1024].partition_broadcast(128), writes=['gfb'])
        for k in range(8):
            for c0 in range(0, 4096, 1024):
                self.load_cast(stage, wf1[:, k, c0:c0 + 1024], ('wf1', k), W['w_ff1'][k * 128:(k + 1) * 128, c0:c0 + 1024], [1024])
        for f in range(32):
            self.load_cast(stage, wf2[:, f, :], ('wf2', f), W['w_ff2'][f * 128:(f + 1) * 128, :], [1024])
        xr = ring('xm', 3, [128, 1024], F32)
        junk = sb('junk', [128, 1024], F32)
        hr = ring('h2', 2, [128, 1024], BF16)
        hTr = ring('h2T', 1, [128, 8, NS], BF16)
        uTr = ring('uT', 1, [128, 32, NS], BF16)
        sqr = ring('sq', 2, [128, NS], F32)
        st1 = ring('st', 4, [128, 4], F32)
        xor_ = ring('xo', 2, [128, 1024], F32)
        ptr = ring('pt', 2, [128, 8, 128], BF16, psum=True)
        pur = ring('pu', 3, [128, 512], F32, psum=True)
        por = ring('po', 3, [128, 512], F32, psum=True)
        for si in range(T // NS):
            tok0 = si * NS
            hT, hTk = hTr.next()
            xs = []
            for j in range(NS // 128):
                t0 = tok0 + j * 128
                x, xk = xr.next()
                xs.append((x, xk))
                S.dma('sp', x[:], self.xmid_d[t0:t0 + 128, :], writes=[xk])
                s1, s1k = st1.next()
                self.act(junk[:], x[:], AF.Square, [xk], ['junk', s1k], accum=s1[:, 0:1])
                self.act(s1[:, 1:2], s1[:, 0:1], AF.Sqrt, [s1k], [s1k], bias=EPS, scale=1.0 / 1024)
                S.op('dve', lambda e, s1=s1: e.reciprocal(out=s1[:, 2:3], in_=s1[:, 1:2]), [s1k], [s1k])
                h, hk = hr.next()
                self.stt(h[:], x[:], s1[:, 2:3], g2b[:], ALU.mult, ALU.mult, [xk, s1k, 'g2b'], [hk])
                p, pk = ptr.next()
                for k in range(8):
                    self.tr(p[:, k, :], h[:, k * 128:(k + 1) * 128], self.identb[:], [hk, 'identb'], [pk])
                self.cp('act', hT[:, :, j * 128:(j + 1) * 128], p[:], [pk], [hTk])
            uT, uTk = uTr.next()
            for f in range(32):
                pu, puk = pur.next()
                for k in range(8):
                    self.mm(pu[:, 0:NS], wf1[:, k, f * 128:(f + 1) * 128], hT[:, k, :], k == 0, k == 7,
                            [hTk, ('wf1', k)], [puk])
                sq, sqk = sqr.next()
                self.act(sq[:], pu[:, 0:NS], AF.Square, [puk], [sqk])
                self.stt(uT[:, f, :], pu[:, 0:NS], 0.0, sq[:], ALU.is_gt, ALU.mult, [puk, sqk], [(uTk, f)])
            for j in range(NS // 128):
                t0 = tok0 + j * 128
                x, xk = xs[j]
                xo, xok = xor_.next()
                for cb in range(2):
                    po, pok = por.next()
                    for f in range(32):
                        self.mm(po[:], uT[:, f, j * 128:(j + 1) * 128], wf2[:, f, cb * 512:(cb + 1) * 512], f == 0, f == 31,
                                [(uTk, f), ('wf2', f)], [pok])
                    self.tt('dve', xo[:, cb * 512:(cb + 1) * 512], x[:, cb * 512:(cb + 1) * 512], po[:], ALU.add,
                            [xk, pok], [xok])
                if final:
                    s1, s1k = st1.next()
                    self.act(junk[:], xo[:], AF.Square, [xok], ['junk', s1k], accum=s1[:, 0:1])
                    self.act(s1[:, 1:2], s1[:, 0:1], AF.Sqrt, [s1k], [s1k], bias=EPS, scale=1.0 / 1024)
                    S.op('dve', lambda e, s1=s1: e.reciprocal(out=s1[:, 2:3], in_=s1[:, 1:2]), [s1k], [s1k])
                    self.stt(xo[:], xo[:], s1[:, 2:3], gfb[:], ALU.mult, ALU.mult, [xok, s1k, 'gfb'], [xok])
                S.dma('sp', x_dst[t0:t0 + 128, :], xo[:], reads=[xok], defer=True)
        S.barrier()


Prog.phase_ffn = phase_ffn


_CACHE = {}


def kernel(**inputs):
    inp = {k: np.asarray(v) for k, v in inputs.items()}
    B, T, D = inp['x'].shape
    P = Prog(T, depth=DEPTH)
    nc = P.build()
    in_maps = [make_in_map(inp, b) for b in range(B)]
    res = run_bass_kernel_spmd(nc, in_maps, core_ids=list(range(B)))
    out = np.stack([np.asarray(r['out'], dtype=np.float32) for r in res.results], axis=0)
    return out
```

```python
import numpy as np
import concourse.bass as bass
import concourse.mybir as mybir
import os
from contextlib import ExitStack
from concourse.bass_utils import run_bass_kernel_spmd

F32 = mybir.dt.float32
BF16 = mybir.dt.bfloat16
AF = mybir.ActivationFunctionType
ALU = mybir.AluOpType
AX = mybir.AxisListType

D_MODEL = 1024
DEPTH = 2
EPS = 1e-6
D_FF = 4096
IN_COLS = 3284
O_AQ, O_CKV, O_IQ, O_IK, O_IW = 0, 256, 384, 640, 704
O_BQ, O_BK, O_BV, O_BLR, O_BR = 708, 964, 1220, 1476, 1492
O_CX, O_CY = 1748, 2004
O_DQ, O_DF, O_DI, O_DG = 2260, 2516, 2772, 3028


class Sched:
    ENG = ('pe', 'dve', 'act', 'pool', 'sp')
    LOOKBACK = 3
    SEM_MAX = 30000

    def __init__(self, nc):
        self.nc = nc
        self.q = {e: [] for e in self.ENG}
        self.sem = {}
        self.cnt = {}
        self.old = {}
        self.all_sems = []
        self.nsem = 0
        for e in self.ENG:
            if e != 'sp':
                self._new_sem(e)
        self.known = {e: {} for e in self.ENG}
        self.res = {}
        self.dma_ring = []
        self.dma_i = 0
        self.NDMA = 40
        self.pend = []
        self.pend_sems = set()
        self.pend_age = 0
        self.DEFER_LOADS = 3
        self.n_ops = 0

    def _alloc(self, name):
        self.nsem += 1
        h = self.nc.alloc_semaphore(f"{name}_{self.nsem}")
        self.all_sems.append(h)
        return h

    def _new_sem(self, e):
        if e in self.sem:
            self.old.setdefault(e, []).append((self.sem[e], self.cnt[e]))
        self.sem[e] = self._alloc('s' + e)
        self.cnt[e] = 0

    def _need(self, eng, ev, waits, use_known=True):
        if ev is None:
            return
        sem, val, src = ev
        if sem.name in self.pend_sems:
            self.flush()
        if src == eng:
            if eng == 'pe':
                return
            if sem is self.sem[eng] and val <= self.cnt[eng] - self.LOOKBACK:
                return
            if sem is not self.sem[eng]:
                return
        k = self.known[eng]
        if use_known and k.get(sem.name, 0) >= val:
            return
        cur = waits.get(sem.name)
        if cur is None or cur[1] < val:
            waits[sem.name] = (sem, val)

    def _deps(self, eng, reads, writes, use_known=True):
        waits = {}
        for r in reads:
            st = self.res.get(r)
            if st is not None:
                self._need(eng, st[0], waits, use_known)
        for w in writes:
            st = self.res.get(w)
            if st is not None:
                self._need(eng, st[0], waits, use_known)
                for ev in st[1].values():
                    self._need(eng, ev, waits, use_known)
        if use_known:
            for name, (sem, val) in waits.items():
                self.known[eng][name] = val
        return list(waits.values())

    def _commit(self, eng, ev, reads, writes):
        for r in reads:
            st = self.res.setdefault(r, [None, {}])
            st[1][eng if ev[2] != 'dma' else ('dma', ev[0].name)] = ev
        for w in writes:
            self.res[w] = [ev, {}]

    def op(self, eng, fn, reads=(), writes=()):
        waits = self._deps(eng, reads, writes)
        if self.cnt[eng] >= self.SEM_MAX:
            self._new_sem(eng)
        sem = self.sem[eng]
        self.cnt[eng] += 1
        ev = (sem, self.cnt[eng], eng)
        self._commit(eng, ev, reads, writes)
        self.q[eng].append((waits, fn, sem, 1))
        self.n_ops += 1

    def flush(self):
        k = self.known['sp']
        for (waits, fn, sem, inc) in self.pend:
            w2 = []
            for (s_, v_) in waits:
                if k.get(s_.name, 0) < v_:
                    k[s_.name] = v_
                    w2.append((s_, v_))
            self.q['sp'].append((w2, fn, sem, inc))
        self.pend = []
        self.pend_sems = set()
        self.pend_age = 0

    def dma(self, eng, out, in_, reads=(), writes=(), defer=False):
        eng = 'sp'
        if len(self.dma_ring) < self.NDMA:
            self.dma_ring.append([self._alloc('d'), 0, None])
            slot = self.dma_ring[-1]
        else:
            slot = self.dma_ring[self.dma_i % self.NDMA]
        self.dma_i += 1
        if slot[0].name in self.pend_sems:
            self.flush()
        waits = self._deps(eng, reads, writes, use_known=not defer)
        if slot[2] is not None:
            w2 = {}
            self._need(eng, slot[2], w2, use_known=not defer)
            for name, (sem, val) in w2.items():
                if not defer:
                    self.known[eng][name] = val
                waits = [w for w in waits if w[0].name != name] + [(sem, val)]
        slot[1] += 16
        ev = (slot[0], slot[1], 'dma')
        slot[2] = ev
        self._commit(eng, ev, reads, writes)
        ent = (waits, lambda e, o=out, i=in_: e.dma_start(out=o, in_=i), slot[0], 16)
        if defer:
            self.pend.append(ent)
            self.pend_sems.add(slot[0].name)
        else:
            self.q[eng].append(ent)
            if self.pend:
                self.pend_age += 1
                if self.pend_age >= self.DEFER_LOADS:
                    self.flush()
        self.n_ops += 1

    def barrier(self):
        self.flush()
        for e in self.ENG:
            waits = []
            k = self.known[e]
            for f in self.ENG:
                if f == 'sp' or (f == e and e == 'pe'):
                    continue
                for (sem, c) in self.old.get(f, []) + [(self.sem[f], self.cnt[f])]:
                    if c > 0 and k.get(sem.name, 0) < c:
                        waits.append((sem, c))
                        k[sem.name] = c
            for slot in self.dma_ring:
                if slot[2] is not None and k.get(slot[0].name, 0) < slot[1]:
                    waits.append((slot[0], slot[1]))
                    k[slot[0].name] = slot[1]
            self.q[e].append((waits, None, None, 0))
        self.res = {}

    def finish(self, eng='sp'):
        self.flush()
        waits = {}
        for slot in self.dma_ring:
            if slot[2] is not None:
                self._need(eng, slot[2], waits)
        self.q[eng].append((list(waits.values()), None, None, 0))

    def emit(self):
        nc = self.nc
        q = self.q

        def run(e, lst):
            for waits, fn, sem, inc in lst:
                for (s, v) in waits:
                    e.wait_ge(s, v)
                if fn is not None:
                    ins = fn(e)
                    ins.then_inc(sem, inc)

        for h in self.all_sems:
            nc.sync.sem_clear(h)
        with nc.Block() as block:
            @block.tensor
            def _(e):
                run(e, q['pe'])

            @block.vector
            def _(e):
                run(e, q['dve'])

            @block.scalar
            def _(e):
                run(e, q['act'])

            @block.gpsimd
            def _(e):
                run(e, q['pool'])

            @block.sync
            def _(e):
                run(e, q['sp'])


class Ring:
    def __init__(self, nc, name, n, shape, dtype, psum=False):
        self.tiles = []
        for i in range(n):
            if psum:
                t = nc.alloc_psum_tensor(f"{name}{i}", shape, dtype)
            else:
                t = nc.alloc_sbuf_tensor(f"{name}{i}", shape, dtype)
            self.tiles.append((t, f"{name}{i}"))
        self.i = 0

    def next(self):
        t = self.tiles[self.i % len(self.tiles)]
        self.i += 1
        return t


NPV = 24


def pack_pvec(inp, l):
    def c2(v):
        return np.ascontiguousarray(np.asarray(v, np.float32).reshape(2, 128).T)
    cols = [c2(inp['b_gk'][l]), c2(inp['lb_param'][0]), c2(inp['lb_param'][1]), c2(inp['conv_b'][l])]
    for j in range(4):
        cols.append(c2(inp['conv_w'][l][j]))
    cols += [c2(inp['b_rg_a'][l]), c2(inp['b_rg_x'][l]), c2(inp['lru_lambda'][l])]
    pv = np.concatenate(cols, axis=1)
    out = np.zeros((128, NPV), np.float32)
    out[:, :pv.shape[1]] = pv
    return out


BV_N1, BV_KV, BV_GLA, BV_HG, BV_BG, BV_N2, BV_FN = 0, 1024, 1152, 1408, 1664, 5760, 6784
NBV = 7808


def pack_bvec(inp, l):
    v = np.concatenate([
        inp['norm1_g'][l], inp['kv_norm_g'][l], np.tile(inp['gla_norm_g'][l], 4),
        np.tile(inp['hgrn_norm_g'][l], 4), inp['b_gate'][l], inp['norm2_g'][l], inp['final_norm_g']
    ]).astype(np.float32)
    assert v.shape[0] == NBV
    return v.reshape(1, NBV)


class Prog:
    def __init__(self, T, depth=DEPTH, debug=()):
        self.T = T
        self.NT = T // 128
        self.depth = depth
        self.debug = set(debug)
        nc = self.nc = bass.Bass("TRN2", target_bir_lowering=False)
        self.S = Sched(nc)
        self.uid = 0
        dt = nc.dram_tensor
        self.x_in = dt("x", [T, D_MODEL], F32, kind="ExternalInput").ap()
        self.out = dt("out", [T, D_MODEL], F32, kind="ExternalOutput").ap()
        self.w = []
        for l in range(depth):
            d = {}
            for name, shape in [('w_in', [D_MODEL, IN_COLS]), ('w_gate', [D_MODEL, 4096]),
                                ('w_uk', [4, 64, 128]), ('w_uv', [4, 128, 64]), ('w_gk2', [16, 256]),
                                ('w_rg_a', [4, 64, 64]), ('w_rg_x', [4, 64, 64]),
                                ('w_br_a', [256, 1024]), ('w_br_b', [256, 1024]), ('w_br_c', [256, 1024]),
                                ('w_br_d', [256, 1024]), ('w_out', [1024, 1024]), ('w_ff1', [1024, 4096]),
                                ('w_ff2', [4096, 1024]), ('pvec', [128, NPV]), ('bvec', [1, NBV])]:
                d[name] = dt(f"{name}{l}", shape, F32, kind="ExternalInput").ap()
            self.w.append(d)
        self.biasT = dt("biasT", [128, 4, 256], F32, kind="ExternalInput").ap()
        self.scr = {}

    def scratch(self, name, shape, dtype):
        kind = "ExternalOutput" if name in self.debug else "Internal"
        t = self.nc.dram_tensor(name, shape, dtype, kind=kind).ap()
        self.scr[name] = t
        return t

    def u(self, p):
        self.uid += 1
        return f"{p}{self.uid}"

    def act(self, out, in_, func, reads, writes, bias=0.0, scale=1.0, accum=None):
        kw = {}
        if accum is not None:
            kw['accum_out'] = accum
        self.S.op('act', lambda e: e.activation(out=out, in_=in_, func=func, bias=bias, scale=scale, **kw),
                  reads, writes)

    def mm(self, out, lhsT, rhs, start, stop, reads, writes):
        self.S.op('pe', lambda e: e.matmul(out, lhsT, rhs, start=start, stop=stop), reads, writes)

    def tr(self, out, in_, ident, reads, writes):
        self.S.op('pe', lambda e: e.transpose(out, in_, ident), reads, writes)

    def tt(self, eng, out, in0, in1, op, reads, writes):
        self.S.op(eng, lambda e: e.tensor_tensor(out=out, in0=in0, in1=in1, op=op), reads, writes)

    def ts(self, eng, out, in0, s1, s2, op0, op1, reads, writes, accum=None):
        if op1 is None:
            self.S.op(eng, lambda e: e.tensor_scalar(out=out, in0=in0, scalar1=s1, scalar2=None, op0=op0),
                      reads, writes)
        elif accum is not None:
            self.S.op(eng, lambda e: e.tensor_scalar(out=out, in0=in0, scalar1=s1, scalar2=s2, op0=op0, op1=op1,
                                                     accum_out=accum), reads, writes)
        else:
            self.S.op(eng, lambda e: e.tensor_scalar(out=out, in0=in0, scalar1=s1, scalar2=s2, op0=op0, op1=op1),
                      reads, writes)

    def stt(self, out, in0, scalar, in1, op0, op1, reads, writes):
        self.S.op('dve', lambda e: e.scalar_tensor_tensor(out=out, in0=in0, scalar=scalar, in1=in1, op0=op0, op1=op1),
                  reads, writes)

    def cp(self, eng, out, in_, reads, writes):
        if eng == 'act':
            self.S.op('act', lambda e: e.copy(out=out, in_=in_), reads, writes)
        else:
            self.S.op(eng, lambda e: e.tensor_copy(out=out, in_=in_), reads, writes)

    def load_cast(self, ring, dst, dkey, src, shape, engs=('pool', 'dve'), pbase=0):
        st, sk = ring.next()
        if len(shape) == 1:
            sv = st[:, 0:shape[0]]
        else:
            n = shape[0] * shape[1]
            sv = st[:, 0:n].rearrange("p (a b) -> p a b", a=shape[0])
        np_ = dst.shape[0]
        self.S.dma('sp', sv[pbase:pbase + np_], src, writes=[sk])
        self.cast_i = getattr(self, 'cast_i', 0) + 1
        self.cp(engs[self.cast_i % len(engs)], dst, sv[pbase:pbase + np_], [sk], [dkey])

    def alloc_scratch(self):
        T = self.T
        self.hT_d = self.scratch("hT_d", [128, 8, T], BF16)
        self.fmf_d = self.scratch("fmf_d", [128, 16, T], F32)
        self.fmb_d = self.scratch("fmb_d", [128, 7, T], BF16)
        self.tmb_d = self.scratch("tmb_d", [T, 641], BF16)
        self.tmf_d = self.scratch("tmf_d", [T, 512], F32)
        self.ckvT_d = self.scratch("ckvT_d", [128, T], BF16)
        self.iw_d = self.scratch("iw_d", [128, self.NT, 4], F32)
        self.yT_d = self.scratch("yT_d", [128, 8, T], BF16)
        self.xmid_d = self.scratch("xmid_d", [T, D_MODEL], F32)
        self.xres_d = self.scratch("xres_d", [T, D_MODEL], F32)

    def consts(self, es):
        nc, S = self.nc, self.S
        self.ident_in = nc.dram_tensor("ident", [128, 128], F32, kind="ExternalInput").ap()
        self.cmask_in = nc.dram_tensor("cmask", [128, 128], F32, kind="ExternalInput").ap()
        self.bdmask_in = nc.dram_tensor("bdmask", [128, 128], F32, kind="ExternalInput").ap()
        self.scanm_in = nc.dram_tensor("scanm", [128, 128], F32, kind="ExternalInput").ap()
        self.cneg_in = nc.dram_tensor("cneg", [128, 128], F32, kind="ExternalInput").ap()
        self.relb_in = nc.dram_tensor("relb", [32, 4], F32, kind="ExternalInput").ap()
        self.identb = es.enter_context(nc.sbuf_tensor("identb", [128, 128], BF16))
        self.identf = es.enter_context(nc.sbuf_tensor("identf", [128, 128], F32))
        S.dma('sp', self.identf[:], self.ident_in, writes=['identf'])
        self.cp('dve', self.identb[:], self.identf[:], ['identf'], ['identb'])

    def phase1(self, l, x_src):
        nc, S, T = self.nc, self.S, self.T
        W = self.w[l]
        with ExitStack() as es:
            def sb(name, shape, dt):
                return es.enter_context(nc.sbuf_tensor(self.u(name), shape, dt))

            def pt(name, shape, dt):
                return es.enter_context(nc.psum_tensor(self.u(name), shape, dt))

            def ring(name, n, shape, dt, psum=False):
                tiles = []
                for i in range(n):
                    nm = self.u(name)
                    t = pt(nm, shape, dt) if psum else sb(nm, shape, dt)
                    tiles.append((t, nm))
                r = Ring.__new__(Ring)
                r.tiles, r.i = tiles, 0
                return r

            wfm = sb('wfm', [128, 8, 2304], BF16)
            wtm = sb('wtm', [128, 8, 1156], BF16)
            wuk = sb('wuk', [128, 2, 128], BF16)
            wgk = sb('wgk', [16, 256], BF16)
            pv = sb('pv', [128, NPV], F32)
            lbc = sb('lbc', [128, 8], F32)
            nbgk = sb('nbgk', [128, 2], F32)
            g1b = sb('g1b', [128, 1024], F32)
            gkvb = sb('gkvb', [128, 128], F32)
            iw_all = sb('iw_all', [128, self.NT, 4], F32)
            stage = ring('stg', 3, [128, IN_COLS], F32)

            S.dma('sp', pv[:], W['pvec'], writes=['pv'])
            S.dma('sp', g1b[:], W['bvec'][:, BV_N1:BV_N1 + 1024].partition_broadcast(128), writes=['g1b'])
            S.dma('sp', gkvb[:], W['bvec'][:, BV_KV:BV_KV + 128].partition_broadcast(128), writes=['gkvb'])
            if l == 0:
                S.op('dve', lambda e: e.memset(lbc[:, 0:2], 0.0), [], ['lbc'])
            else:
                self.tt('dve', lbc[:, 6:8], pv[:, 4:6], pv[:, 2:4], ALU.subtract, ['pv'], ['lbc'])
                self.act(lbc[:, 0:2], lbc[:, 6:8], AF.Sigmoid, ['lbc'], ['lbc'])
            self.ts('dve', lbc[:, 2:4], lbc[:, 0:2], -1.0, 1.0, ALU.mult, ALU.add, ['lbc'], ['lbc'])
            self.ts('dve', lbc[:, 4:6], lbc[:, 0:2], 1e-20, None, ALU.max, None, ['lbc'], ['lbc'])
            self.ts('dve', nbgk[:], pv[:, 0:2], -1.0, None, ALU.mult, None, ['pv'], ['nbgk'])

            fm_src = [(O_AQ, 256), (O_IQ, 256), (O_IK, 64), (O_IK, 64), (O_BQ, 256), (O_BK, 256), (O_BLR, 16),
                      (O_CX, 256), (O_CY, 256), (O_DQ, 256), (O_DF, 256)]
            fm_dst = [0, 256, 512, 576, 640, 896, 1152, 1280, 1536, 1792, 2048]
            tm_src = [(O_CKV, 128), (O_IW, 4), (O_BV, 256), (O_BR, 256), (O_DI, 256), (O_DG, 256)]
            tm_dst = [0, 128, 132, 388, 644, 900]
            for k in range(8):
                st, sk = stage.next()
                S.dma('sp', st[:, 0:IN_COLS], W['w_in'][k * 128:(k + 1) * 128, :], writes=[sk])
                ci_ = 0
                for dst_t, key, srcs, dsts in ((wfm, ('wfm', k), fm_src, fm_dst), (wtm, ('wtm', k), tm_src, tm_dst)):
                    for (so, n), do in zip(srcs, dsts):
                        ci_ += 1
                        self.cp(('pool', 'dve', 'act')[ci_ % 3], dst_t[:, k, do:do + n], st[:, so:so + n], [sk], [key])
            self.load_cast(stage, wuk[:], 'wuk', W['w_uk'].rearrange("(hp e) d c -> (e d) hp c", e=2), [2, 128])
            self.load_cast(stage, wgk[:], 'wgk', W['w_gk2'], [256])

            xr = ring('xt', 3, [128, 1024], F32)
            junk = sb('junk', [128, 1024], F32)
            hr = ring('h', 2, [128, 1024], BF16)
            hTr = ring('hT', 2, [128, 8, 512], BF16)
            st1 = ring('st1', 4, [128, 4], F32)
            ptr = ring('ptr', 2, [128, 8, 128], BF16, psum=True)
            psr = ring('ps', 4, [128, 512], F32, psum=True)
            ptc = ring('ptc', 1, [128, 128], BF16, psum=True)
            tmbr = ring('tmb', 3, [128, 641], BF16)
            tmfr = ring('tmf', 3, [128, 512], F32)
            ckvTr = ring('ckvT', 2, [128, 512], BF16)
            qTr = ring('qT', 2, [128, 2, 512], BF16)
            blrr = ring('blr', 2, [16, 512], BF16)
            ofr = ring('of', 6, [128, 512], F32)
            obr = ring('ob', 4, [128, 512], BF16)
            for t_, _k in tmbr.tiles:
                S.op('pool', lambda e, t_=t_: e.memset(t_[:, 128:129], 1.0), [], [_k])

            for st_i in range(T // 512):
                tok0 = st_i * 512
                hT, hTk = hTr.next()
                ckvT, ckvTk = ckvTr.next()
                for j in range(4):
                    t0 = tok0 + j * 128
                    tt_i = t0 // 128
                    xt, xk = xr.next()
                    S.dma('sp', xt[:], x_src[t0:t0 + 128, :], writes=[xk])
                    s1, s1k = st1.next()
                    self.act(junk[:], xt[:], AF.Square, [xk], ['junk', s1k], accum=s1[:, 0:1])
                    self.act(s1[:, 1:2], s1[:, 0:1], AF.Sqrt, [s1k], [s1k], bias=EPS, scale=1.0 / 1024)
                    S.op('dve', lambda e, s1=s1: e.reciprocal(out=s1[:, 2:3], in_=s1[:, 1:2]), [s1k], [s1k])
                    h, hk = hr.next()
                    self.stt(h[:], xt[:], s1[:, 2:3], g1b[:], ALU.mult, ALU.mult, [xk, s1k, 'g1b'], [hk])
                    p, pk = ptr.next()
                    for k in range(8):
                        self.tr(p[:, k, :], h[:, k * 128:(k + 1) * 128], self.identb[:], [hk, 'identb'], [pk])
                    self.cp('act', hT[:, :, j * 128:(j + 1) * 128], p[:], [pk], [hTk])
                    tmb, tmbk = tmbr.next()
                    tmf, tmfk = tmfr.next()
                    pa, pak = psr.next()
                    for k in range(8):
                        self.mm(pa[:, 0:388], hT[:, k, j * 128:(j + 1) * 128], wtm[:, k, 0:388], k == 0, k == 7,
                                [hTk, ('wtm', k)], [pak])
                    s2, s2k = st1.next()
                    self.act(junk[:, 0:128], pa[:, 0:128], AF.Square, [pak], ['junk', s2k], accum=s2[:, 0:1])
                    self.act(s2[:, 1:2], s2[:, 0:1], AF.Sqrt, [s2k], [s2k], bias=EPS, scale=1.0 / 128)
                    S.op('dve', lambda e, s2=s2: e.reciprocal(out=s2[:, 2:3], in_=s2[:, 1:2]), [s2k], [s2k])
                    self.stt(tmb[:, 0:128], pa[:, 0:128], s2[:, 2:3], gkvb[:], ALU.mult, ALU.mult,
                             [pak, s2k, 'gkvb'], [tmbk])
                    self.ts('dve', iw_all[:, tt_i, :], pa[:, 128:132], 1.0 / 16, None, ALU.mult, None, [pak], ['iw_all'])
                    self.cp('act', tmb[:, 129:385], pa[:, 132:388], [pak], [tmbk])
                    pc, pck = ptc.next()
                    self.tr(pc[:], tmb[:, 0:128], self.identb[:], [tmbk, 'identb'], [pck])
                    self.cp('dve', ckvT[:, j * 128:(j + 1) * 128], pc[:], [pck], [ckvTk])
                    pb, pbk = psr.next()
                    for k in range(8):
                        self.mm(pb[:, 0:512], hT[:, k, j * 128:(j + 1) * 128], wtm[:, k, 388:900], k == 0, k == 7,
                                [hTk, ('wtm', k)], [pbk])
                    self.act(tmf[:, 0:256], pb[:, 0:256], AF.Silu, [pbk], [tmfk])
                    self.cp('dve', tmb[:, 385:641], pb[:, 256:512], [pbk], [tmbk])
                    pcc, pcck = psr.next()
                    for k in range(8):
                        self.mm(pcc[:, 0:256], hT[:, k, j * 128:(j + 1) * 128], wtm[:, k, 900:1156], k == 0, k == 7,
                                [hTk, ('wtm', k)], [pcck])
                    self.act(tmf[:, 256:512], pcc[:, 0:256], AF.Silu, [pcck], [tmfk])
                    S.dma('sp', self.tmb_d[t0:t0 + 128, :], tmb[:], reads=[tmbk], defer=True)
                    S.dma('sp', self.tmf_d[t0:t0 + 128, :], tmf[:], reads=[tmfk], defer=True)
                S.dma('sp', self.hT_d[:, :, tok0:tok0 + 512], hT[:], reads=[hTk], defer=True)
                S.dma('sp', self.ckvT_d[:, tok0:tok0 + 512], ckvT[:], reads=[ckvTk], defer=True)

                def fm_block(blk, M=128):
                    pf, pfk = psr.next()
                    for k in range(8):
                        self.mm(pf[0:M, :], wfm[:, k, blk * 128:blk * 128 + M], hT[:, k, :], k == 0, k == 7,
                                [hTk, ('wfm', k)], [pfk])
                    return pf, pfk

                def out_f(slot):
                    o, ok = ofr.next()
                    return o, ok, self.fmf_d[:, slot, tok0:tok0 + 512]

                def out_b(slot):
                    o, ok = obr.next()
                    return o, ok, self.fmb_d[:, slot, tok0:tok0 + 512]

                qT, qTk = qTr.next()
                for hp in range(2):
                    pf, pfk = fm_block(0 + hp)
                    self.cp('act', qT[:, hp, :], pf[:], [pfk], [qTk])
                for h in range(4):
                    hp, e_ = h // 2, h % 2
                    pf, pfk = psr.next()
                    self.mm(pf[:], wuk[e_ * 64:(e_ + 1) * 64, hp, :], qT[e_ * 64:(e_ + 1) * 64, hp, :], True, True,
                            ['wuk', qTk], [pfk])
                    o, ok, dst = out_b(3 + h)
                    self.ts('dve', o[:], pf[:], 0.125, None, ALU.mult, None, [pfk], [ok])
                    S.dma('sp', dst, o[:], reads=[ok], defer=True)
                for hp in range(2):
                    pf, pfk = fm_block(2 + hp)
                    o, ok, dst = out_b(0 + hp)
                    self.cp('act', o[:], pf[:], [pfk], [ok])
                    S.dma('sp', dst, o[:], reads=[ok], defer=True)
                pf, pfk = fm_block(4)
                o, ok, dst = out_b(2)
                self.cp('dve', o[:], pf[:], [pfk], [ok])
                S.dma('sp', dst, o[:], reads=[ok], defer=True)
                for hp in range(2):
                    pf, pfk = fm_block(5 + hp)
                    o, ok, dst = out_f(0 + hp)
                    self.ts('dve', o[:], pf[:], 0.125, None, ALU.mult, None, [pfk], [ok])
                    S.dma('sp', dst, o[:], reads=[ok], defer=True)
                for hp in range(2):
                    pf, pfk = fm_block(7 + hp)
                    o, ok, dst = out_f(2 + hp)
                    self.cp('act', o[:], pf[:], [pfk], [ok])
                    S.dma('sp', dst, o[:], reads=[ok], defer=True)
                pf, pfk = fm_block(9, M=16)
                blr, blrk = blrr.next()
                self.cp('dve', blr[:], pf[0:16, :], [pfk], [blrk])
                for hp in range(2):
                    pg, pgk = psr.next()
                    self.mm(pg[:], wgk[0:16, hp * 128:(hp + 1) * 128], blr[0:16, :], True, True, ['wgk', blrk], [pgk])
                    o, ok, dst = out_f(4 + hp)
                    self.act(o[:], pg[:], AF.Exp, [pgk, 'nbgk'], [ok], bias=nbgk[:, hp:hp + 1], scale=-1.0)
                    self.act(o[:], o[:], AF.Ln, [ok], [ok], bias=1.0)
                    self.ts('dve', o[:], o[:], -1.0 / 16, None, ALU.mult, None, [ok], [ok])
                    S.dma('sp', dst, o[:], reads=[ok], defer=True)
                for i_, slot0 in ((10, 6), (12, 8)):
                    for hp in range(2):
                        pf, pfk = fm_block(i_ + hp)
                        o, ok, dst = out_f(slot0 + hp)
                        self.cp('act' if hp else 'dve', o[:], pf[:], [pfk], [ok])
                        S.dma('sp', dst, o[:], reads=[ok], defer=True)
                for hp in range(2):
                    pf, pfk = fm_block(14 + hp)
                    o, ok, dst = out_f(10 + hp)
                    self.act(o[:], pf[:], AF.Silu, [pfk], [ok])
                    S.dma('sp', dst, o[:], reads=[ok], defer=True)
                for hp in range(2):
                    pf, pfk = fm_block(16 + hp)
                    o, ok, dst = out_f(12 + hp)
                    self.act(o[:], pf[:], AF.Sigmoid, [pfk], [ok], scale=-1.0)
                    self.ts('dve', o[:], o[:], lbc[:, 2 + hp:3 + hp], None, ALU.mult, None, [ok, 'lbc'], [ok])
                    S.dma('sp', dst, o[:], reads=[ok], defer=True)
                    o2, ok2, dst2 = out_f(14 + hp)
                    self.act(o2[:], pf[:], AF.Sigmoid, [pfk], [ok2])
                    self.ts('dve', o2[:], o2[:], lbc[:, 2 + hp:3 + hp], lbc[:, 4 + hp:5 + hp], ALU.mult, ALU.add,
                            [ok2, 'lbc'], [ok2])
                    self.act(o2[:], o2[:], AF.Ln, [ok2], [ok2])
                    S.dma('sp', dst2, o2[:], reads=[ok2], defer=True)
            S.dma('sp', self.iw_d, iw_all[:], reads=['iw_all'], defer=True)
            S.barrier()

    def build(self, phases=None):
        self.es = ExitStack()
        self.alloc_scratch()
        self.consts(self.es)
        self.S.barrier()
        x_src = self.x_in
        for l in range(self.depth):
            last = l == self.depth - 1
            def on(p):
                return phases is None or p in phases
            if on('p1'):
                self.phase1(l, x_src)
            if on('dsa'):
                self.phase_dsa(l)
            wh = tuple(b_ for b_, n_ in ((1, 'gla'), (3, 'hgrn')) if on(n_))
            if wh or on('lru'):
                self.phase_gla2(l, wh, on('lru'))
            if on('merge'):
                self.phase_merge(l, x_src)
            if on('ffn'):
                self.phase_ffn(l, self.out if last else self.xres_d, last)
            x_src = self.xres_d
        self.S.finish('sp')
        self.S.emit()
        self.es.close()
        return self.nc


def const_inputs():
    ident = np.eye(128, dtype=np.float32)
    j = np.arange(128)[:, None]
    i = np.arange(128)[None, :]
    cmask = (j <= i).astype(np.float32)
    bdmask = ((j <= i) & (j // 64 == i // 64)).astype(np.float32)
    scanm = np.ones((128, 128), np.float32)
    scanm[:, 0] = 0
    scanm[:, 64] = 0
    cneg = np.where(i <= j, 0.0, -1e30).astype(np.float32)
    return {'ident': ident, 'cmask': cmask, 'bdmask': bdmask, 'scanm': scanm, 'cneg': cneg}


def t5_bucket_np(dist):
    n = np.maximum(dist, 0)
    nf = np.maximum(n, 16).astype(np.float32)
    large = 16 + (np.log(nf / np.float32(16)) / np.float32(np.log(128 / 16)) * np.float32(16)).astype(np.int32)
    large = np.minimum(large, 31)
    return np.where(n < 16, n, large)


def bias_table(rel_bias):
    kk = np.arange(128)[:, None]
    qq = np.arange(128)[None, :]
    bp = t5_bucket_np(qq - kk + 128)
    bd = t5_bucket_np(qq - kk)
    idx = np.concatenate([bp, bd], axis=1)
    rb = np.asarray(rel_bias, np.float32)
    return np.ascontiguousarray(rb[idx].transpose(0, 2, 1))


def make_in_map(inp, b, depth=DEPTH):
    m = {'x': np.ascontiguousarray(inp['x'][b], dtype=np.float32)}
    for l in range(depth):
        for name in ('w_in', 'w_gate', 'w_uk', 'w_uv', 'w_gk2', 'w_rg_a', 'w_rg_x', 'w_br_a', 'w_br_b', 'w_br_c',
                     'w_br_d', 'w_out', 'w_ff1', 'w_ff2'):
            m[f"{name}{l}"] = np.ascontiguousarray(inp[name][l], dtype=np.float32)
        m[f"pvec{l}"] = pack_pvec(inp, l)
        m[f"bvec{l}"] = pack_bvec(inp, l)
    m['biasT'] = bias_table(inp['rel_bias'])
    m['relb'] = np.ascontiguousarray(inp['rel_bias'], dtype=np.float32)
    m.update(const_inputs())
    return m


def _phase_tools(self, es):
    nc = self.nc

    def sb(name, shape, dt):
        return es.enter_context(nc.sbuf_tensor(self.u(name), shape, dt))

    def pt(name, shape, dt):
        return es.enter_context(nc.psum_tensor(self.u(name), shape, dt))

    def ring(name, n, shape, dt, psum=False):
        tiles = []
        for i in range(n):
            nm = self.u(name)
            t = pt(nm, shape, dt) if psum else sb(nm, shape, dt)
            tiles.append((t, nm))
        r = Ring.__new__(Ring)
        r.tiles, r.i = tiles, 0
        return r
    return sb, pt, ring


NBIS = 14


def phase_dsa(self, l):
    nc, S, T, NT = self.nc, self.S, self.T, self.NT
    W = self.w[l]
    topk = min(256, T // 4)
    with ExitStack() as es:
        sb, pt, ring = _phase_tools(self, es)
        ckvT = sb('ckvT', [128, T], BF16)
        ckv1 = sb('ckv1', [128, NT, 129], BF16)
        ikT = sb('ikT', [128, T], BF16)
        iw = sb('iw', [128, NT, 4], F32)
        wuv = sb('wuv', [128, 4, 64], BF16)
        I4 = sb('I4', [128, 4, 128], BF16)
        cneg = sb('cneg', [128, 128], F32)
        bT = sb('bT', [128, 4, 256], F32)
        bhi = sb('bhi', [128, 4, 256], BF16)
        blo = sb('blo', [128, 4, 256], BF16)
        bhf = sb('bhf', [128, 4, 256], F32)
        b31 = sb('b31', [128, 4], F32)
        stage = ring('stg', 2, [128, 256], F32)
        S.dma('sp', ckvT[:], self.ckvT_d, writes=['ckvT'])
        S.dma('sp', ckv1[:], self.tmb_d[:, 0:129].rearrange("(t p) c -> p t c", p=128), writes=['ckv1'])
        S.dma('sp', ikT[:], self.fmb_d[:, 2, :], writes=['ikT'])
        S.dma('sp', iw[:], self.iw_d, writes=['iw'])
        S.dma('sp', cneg[:], self.cneg_in, writes=['cneg'])
        S.dma('sp', bT[:], self.biasT, writes=['bT'])
        S.dma('sp', b31[:], self.relb_in[31:32, :].partition_broadcast(128), writes=['b31'])
        self.load_cast(stage, wuv[:], 'wuv', W['w_uv'].rearrange("h c d -> c h d"), [4, 64])
        for h in range(4):
            self.cp('pool', I4[:, h, :], self.identb[:], ['identb'], ['I4'])
            self.ts('dve', bT[:, h, :], bT[:, h, :], b31[:, h:h + 1], None, ALU.subtract, None, ['bT', 'b31'], ['bT'])
        self.cp('dve', bhi[:], bT[:], ['bT'], ['bhi'])
        self.cp('dve', bhf[:], bhi[:], ['bhi'], ['bhf'])
        self.tt('dve', bhf[:], bT[:], bhf[:], ALU.subtract, ['bT', 'bhf'], ['bhf'])
        self.cp('dve', blo[:], bhf[:], ['bhf'], ['blo'])

        G = 2
        iqr = ring('iq', 3, [128, 2, 128], BF16)
        qlr = ring('ql', 3 * G, [128, 4, 128], BF16)
        scr = ring('sc', 2 * G, [128, T], F32)
        mnr = ring('mn', 2 * G, [128, T], BF16)
        junk = sb('junkb', [128, T], BF16)
        rlr = ring('rl', 3, [128, 512], F32)
        bs = ring('bs', 3, [128, 8, G], F32)
        pss = ring('pss', 2, [128, 512], F32, psum=True)
        psl = ring('psl', 2, [128, 512], F32, psum=True)
        poA = ring('poA', 1, [128, 2, 129], F32, psum=True)
        poB = ring('poB', 1, [128, 2, 129], F32, psum=True)
        ptr = ring('ptr', 1, [128, 4, 128], BF16, psum=True)
        pyr = ring('py', 1, [128, 2, 128], F32, psum=True)
        pr_ = ring('p', 3, [128, 512], BF16)
        olr = ring('ol', 2, [128, 4, 128], BF16)
        olTr = ring('olT', 2, [128, 4, 128], BF16)
        yr = ring('ya', 2, [128, 2, 128], BF16)
        rdr = ring('rd', 2, [128, 4], F32)
        NG = (NT + G - 1) // G
        info = {}

        def gen_scores(g):
            tiles = list(range(g * G, min(NT, g * G + G)))
            b, bk = bs.next()
            grp = []
            info[g] = (b, bk, grp)
            for gi, qi in enumerate(tiles):
                q0 = qi * 128
                nk = (qi + 1) * 128
                iq, iqk = iqr.next()
                ql, qlk = qlr.next()
                S.dma('sp', iq[:], self.fmb_d[:, 0:2, q0:q0 + 128], writes=[iqk])
                S.dma('sp', ql[:], self.fmb_d[:, 3:7, q0:q0 + 128], writes=[qlk])
                sc, sck = scr.next()
                mn, mnk = mnr.next()
                grp.append((qi, q0, nk, ql, qlk, sc, sck, mn, mnk))
                for k0 in range(0, nk, 512):
                    n = min(512, nk - k0)
                    for h in range(4):
                        hp, e_ = h // 2, h % 2
                        ps, psk = pss.next()
                        self.mm(ps[:, 0:n], iq[e_ * 64:(e_ + 1) * 64, hp, :], ikT[e_ * 64:(e_ + 1) * 64, k0:k0 + n],
                                True, True, [iqk, 'ikT'], [psk])
                        if h == 0:
                            self.ts('dve', sc[:, k0:k0 + n], ps[:, 0:n], 0.0, iw[:, qi, 0:1], ALU.max, ALU.mult,
                                    [psk, 'iw'], [sck])
                        else:
                            rl, rlk = rlr.next()
                            self.act(rl[:, 0:n], ps[:, 0:n], AF.Relu, [psk], [rlk])
                            self.stt(sc[:, k0:k0 + n], rl[:, 0:n], iw[:, qi, h:h + 1], sc[:, k0:k0 + n], ALU.mult, ALU.add,
                                     [rlk, 'iw', sck], [sck])
                    yield
                S.op('dve', lambda e, b=b, sc=sc, nk=nk, gi=gi: e.tensor_reduce(
                    out=b[:, 0, gi:gi + 1], in_=sc[:, 0:nk], axis=AX.X, op=ALU.min), [sck], [(bk, 'mn', gi)])
                S.op('dve', lambda e, b=b, sc=sc, nk=nk, gi=gi: e.tensor_reduce(
                    out=b[:, 1, gi:gi + 1], in_=sc[:, 0:nk], axis=AX.X, op=ALU.max), [sck], [(bk, 'mx', gi)])
                self.tt('dve', sc[:, q0:q0 + 128], sc[:, q0:q0 + 128], cneg[:], ALU.add, [sck, 'cneg'], [sck])
                yield

        def n_scores(g):
            return sum(((qi + 1) * 128 + 511) // 512 + 1 for qi in range(g * G, min(NT, g * G + G)))

        def gen_attend(g):
            for (qi, q0, nk, ql, qlk, sc, sck, mn, mnk) in info[g][2]:
                yield from self.dsa_attend(qi, q0, ql, qlk, mn, mnk, ckvT, ckv1, I4, bhi, blo, wuv,
                                           psl, poA, poB, ptr, pyr, pr_, olr, olTr, yr, rdr)

        def n_attend(g):
            return sum(qi + 2 for qi in range(g * G, min(NT, g * G + G)))

        def advance(gen, n):
            if gen is None:
                return None
            for _ in range(n):
                try:
                    next(gen)
                except StopIteration:
                    return None
            return gen

        advance(gen_scores(0), 10 ** 9)
        for g in range(NG):
            b, bk, grp = info[g]
            ng = len(grp)
            A = gen_scores(g + 1) if g + 1 < NG else None
            C = gen_attend(g - 1) if g >= 1 else None
            stepA = (n_scores(g + 1) + NBIS - 1) // NBIS if A is not None else 0
            stepC = (n_attend(g - 1) + NBIS - 1) // NBIS if C is not None else 0
            gs = slice(0, ng)
            kmm, kR, kC, kU, kT, kH = (bk, 'mm'), (bk, 'R'), (bk, 'cand'), (bk, 'u'), (bk, 'tmp'), (bk, 'thr')
            kin = [(bk, 'mn', gi) for gi in range(ng)] + [(bk, 'mx', gi) for gi in range(ng)]
            self.ts('dve', b[:, 0, gs], b[:, 0, gs], -1.0, None, ALU.add, None, kin, [kmm])
            self.tt('dve', b[:, 2, gs], b[:, 1, gs], b[:, 0, gs], ALU.subtract, kin + [kmm], [kR])
            self.stt(b[:, 3, gs], b[:, 2, gs], 0.5, b[:, 0, gs], ALU.mult, ALU.add, [kR, kmm], [kC])
            for it in range(NBIS):
                ci = 0.5 ** (it + 1)
                for gi, (qi, q0, nk, ql, qlk, sc, sck, mn, mnk) in enumerate(grp):
                    self.ts('dve', junk[:, 0:nk], sc[:, 0:nk], b[:, 3, gi:gi + 1], 0.0, ALU.is_ge, ALU.add,
                            [sck, kC], [(bk, 'cnt', gi)], accum=b[:, 4, gi:gi + 1])
                self.ts('dve', b[:, 5, gs], b[:, 4, gs], float(topk), -0.5, ALU.is_ge, ALU.add,
                        [(bk, 'cnt', gi) for gi in range(ng)], [kU])
                self.tt('dve', b[:, 6, gs], b[:, 5, gs], b[:, 2, gs], ALU.mult, [kU, kR], [kT])
                self.stt(b[:, 3, gs], b[:, 6, gs], ci, b[:, 3, gs], ALU.mult, ALU.add, [kT, kC], [kC])
                A = advance(A, stepA)
                C = advance(C, stepC)
            advance(A, 10 ** 9)
            advance(C, 10 ** 9)
            self.stt(b[:, 7, gs], b[:, 2, gs], -(0.5 ** (NBIS + 1)), b[:, 3, gs], ALU.mult, ALU.add, [kR, kC], [kH])
            for gi, (qi, q0, nk, ql, qlk, sc, sck, mn, mnk) in enumerate(grp):
                self.ts('dve', mn[:, 0:nk], sc[:, 0:nk], b[:, 7, gi:gi + 1], -30000.0, ALU.is_lt, ALU.mult,
                        [sck, kH], [mnk])
        advance(gen_attend(NG - 1), 10 ** 9)
        S.barrier()


def dsa_attend(self, qi, q0, ql, qlk, mn, mnk, ckvT, ckv1, I4, bhi, blo, wuv,
               psl, poA, poB, ptr, pyr, pr_, olr, olTr, yr, rdr):
    S = self.S
    if True:
        if True:
            pa, pak = poA.next()
            pb, pbk = poB.next()
            qlf = ql[:].rearrange("p h q -> p (h q)")
            for kb in range(qi + 1):
                pl, plk = psl.next()
                near = kb >= qi - 1
                self.mm(pl[:], ckvT[:, kb * 128:(kb + 1) * 128], qlf, True, False, ['ckvT', qlk], [plk])
                self.mm(pl[:], mn[:, kb * 128:(kb + 1) * 128], I4[:].rearrange("p h q -> p (h q)"), False, not near,
                        [mnk, 'I4'], [plk])
                if near:
                    o_ = 128 if kb == qi else 0
                    for bt_, bkey, last in ((bhi, 'bhi', False), (blo, 'blo', True)):
                        for h in range(4):
                            self.mm(pl[:, h * 128:(h + 1) * 128], self.identb[:], bt_[:, h, o_:o_ + 128], False,
                                    last and h == 3, ['identb', bkey], [plk])
                p, pk = pr_.next()
                self.act(p[:], pl[:], AF.Exp, [plk], [pk])
                for h in range(4):
                    po, pok = (pa, pak) if h < 2 else (pb, pbk)
                    self.mm(po[:, h % 2, :], p[:, h * 128:(h + 1) * 128], ckv1[:, kb, :], kb == 0 and h % 2 == 0, kb == qi,
                            [pk, 'ckv1'], [pok])
                yield
            rd, rdk = rdr.next()
            ol, olk = olr.next()
            for h in range(4):
                po, pok = (pa, pak) if h < 2 else (pb, pbk)
                S.op('dve', lambda e, rd=rd, po=po, h=h: e.reciprocal(out=rd[:, h:h + 1], in_=po[:, h % 2, 128:129]),
                     [pok], [rdk])
                self.ts('dve', ol[:, h, :], po[:, h % 2, 0:128], rd[:, h:h + 1], None, ALU.mult, None, [pok, rdk], [olk])
            ptt, pttk = ptr.next()
            for h in range(4):
                self.tr(ptt[:, h, :], ol[:, h, :], self.identb[:], [olk, 'identb'], [pttk])
            olT, olTk = olTr.next()
            self.cp('act', olT[:], ptt[:], [pttk], [olTk])
            py, pyk = pyr.next()
            for h in range(4):
                hp, e_ = h // 2, h % 2
                self.mm(py[e_ * 64:(e_ + 1) * 64, hp, :], wuv[:, h, :], olT[:, h, :], True, True, ['wuv', olTk], [pyk])
            ya, yak = yr.next()
            self.cp('act', ya[:], py[:], [pyk], [yak])
            S.dma('sp', self.yT_d[:, 0:2, q0:q0 + 128], ya[:], reads=[yak], defer=True)
            yield


Prog.dsa_attend = dsa_attend
Prog.phase_dsa = phase_dsa


def phase_gla(self, l, br, shared):
    nc, S, T, NT = self.nc, self.S, self.T, self.NT
    W = self.w[l]
    if br == 1:
        sq_, sk_, sg_, vcol, gcol, bvo = 0, 2, 4, 129, 0, BV_GLA
    else:
        sq_, sk_, sg_, vcol, gcol, bvo = 10, 12, 14, 385, 256, BV_HG
    with ExitStack() as es:
        sb, pt, ring = _phase_tools(self, es)
        scanm = sb('scanm', [128, 128], F32)
        bdm = sb('bdm', [128, 2, 128], F32)
        gnb = sb('gnb', [128, 256], F32)
        Sf = sb('Sf', [128, 2, 64], F32)
        S.dma('sp', scanm[:], self.scanm_in, writes=['scanm'])
        S.dma('sp', bdm[:, 0, :], self.bdmask_in, writes=['bdm'])
        S.dma('sp', bdm[:, 1, :], self.bdmask_in, writes=['bdm'])
        S.dma('sp', gnb[:], W['bvec'][:, bvo:bvo + 256].partition_broadcast(128), writes=['gnb'])
        S.op('pool', lambda e: e.memset(Sf[:], 0.0), [], ['Sf'])
        Sbr = ring('Sb', 4, [128, 2, 64], BF16)
        Sb, Sbk = Sbr.next()
        S.op('pool', lambda e, Sb=Sb: e.memset(Sb[:], 0.0), [], [Sbk])
        qr = ring('q', 2, [128, 2, 128], F32)
        kr = ring('k', 2, [128, 2, 128], F32)
        gr = ring('g', 2, [128, 2, 128], F32)
        vr = ring('v', 2, [128, 256], BF16)
        gtr = ring('gt', 2, [128, 256], F32)
        br_ = ring('b', 2, [128, 128], F32)
        bmr = ring('bm', 2, [128, 128], F32)
        e3r = ring('e3', 2, [128, 3, 128], F32)
        qer = ring('qe', 2, [128, 128], BF16)
        ker = ring('ke', 2, [128, 128], BF16)
        q1r = ring('q1', 2, [128, 128], BF16)
        ebr = ring('eb', 2, [128, 4], F32)
        atr = ring('at', 2, [128, 2, 128], BF16)
        ktr = ring('kt', 2, [128, 128], BF16)
        tmr = ring('tm', 2, [128, 64], F32)
        jk = sb('jk', [128, 256], F32)
        ssr = ring('ss', 2, [128, 12], F32)
        yr = ring('y', 2, [128, 256], F32)
        ybr = ring('yb', 2, [128, 256], BF16)
        yTr = ring('yT', 2, [128, 2, 128], BF16)
        patE, pkt, pdsC, porE, pyt = shared

        for ti in range(NT):
            t0 = ti * 128
            q, qk = qr.next()
            k, kk = kr.next()
            g, gk = gr.next()
            v, vk = vr.next()
            gt, gtk = gtr.next()
            S.dma('sp', g[:], self.fmf_d[:, sg_:sg_ + 2, t0:t0 + 128], writes=[gk])
            S.dma('sp', q[:], self.fmf_d[:, sq_:sq_ + 2, t0:t0 + 128], writes=[qk])
            S.dma('sp', k[:], self.fmf_d[:, sk_:sk_ + 2, t0:t0 + 128], writes=[kk])
            S.dma('sp', v[:], self.tmb_d[t0:t0 + 128, vcol:vcol + 256], writes=[vk])
            S.dma('sp', gt[:], self.tmf_d[t0:t0 + 128, gcol:gcol + 256], writes=[gtk])
            poE = [porE[0].next(), porE[1].next()]
            Sb0, Sb0k = Sb, Sbk
            Sb1, Sb1k = Sbr.next()
            Sb2, Sb2k = Sbr.next()
            for hp in range(2):
                b, bk = br_.next()
                bm, bmk = bmr.next()
                S.op('dve', lambda e, b=b, g=g, hp=hp: e.tensor_tensor_scan(
                    out=b[:], data0=scanm[:], data1=g[:, hp, :], initial=0.0, op0=ALU.mult, op1=ALU.add),
                    ['scanm', gk], [bk])
                for c in range(2):
                    self.ts('dve', bm[:, c * 64:(c + 1) * 64], b[:, c * 64:(c + 1) * 64], b[:, c * 64 + 31:c * 64 + 32],
                            None, ALU.subtract, None, [bk], [bmk])
                e3, e3k = e3r.next()
                self.act(e3[:, 0, :], bm[:], AF.Exp, [bmk], [e3k])
                self.act(e3[:, 1, :], bm[:], AF.Exp, [bmk], [e3k], scale=-1.0)
                self.act(e3[:, 2, :], b[:], AF.Exp, [bk], [e3k])
                eb, ebk = ebr.next()
                for c in range(2):
                    self.act(eb[:, c:c + 1], b[:, c * 64 + 63:c * 64 + 64], AF.Exp, [bk], [ebk])
                    self.act(eb[:, 2 + c:3 + c], bm[:, c * 64 + 63:c * 64 + 64], AF.Exp, [bmk], [ebk])
                qe, qek = qer.next()
                ke, kek = ker.next()
                q1, q1k = q1r.next()
                self.tt('dve', qe[:], q[:, hp, :], e3[:, 0, :], ALU.mult, [qk, e3k], [qek])
                self.tt('pool', ke[:], k[:, hp, :], e3[:, 1, :], ALU.mult, [kk, e3k], [kek])
                self.tt('pool', q1[:], q[:, hp, :], e3[:, 2, :], ALU.mult, [qk, e3k], [q1k])
                at, atk = atr.next()
                for e_ in range(2):
                    sl = slice(e_ * 64, (e_ + 1) * 64)
                    pa, pak = patE[e_].next()
                    self.mm(pa[:, 0:128], ke[sl, :], qe[sl, :], True, True, [kek, qek], [pak])
                    self.tt('dve', at[:, e_, :], pa[:, 0:128], bdm[:, 0, :], ALU.mult, [pak, 'bdm'], [(atk, e_)])
                pk_, pkk = pkt.next()
                self.tr(pk_[:, 0:128], ke[:], self.identb[:], [kek, 'identb'], [pkk])
                kt, ktk = ktr.next()
                self.cp('act', kt[:], pk_[:, 0:128], [pkk], [ktk])
                pdc = [pdsC[0].next(), pdsC[1].next()]
                for c in range(2):
                    cs = slice(c * 64, (c + 1) * 64)
                    pd, pdk = pdc[c]
                    for e_ in range(2):
                        sl = slice(e_ * 64, (e_ + 1) * 64)
                        h = hp * 2 + e_
                        self.mm(pd[sl, 0:64], kt[cs, sl], v[cs, h * 64:(h + 1) * 64], True, True, [ktk, vk], [pdk])
                for c, (Sn, Snk) in enumerate(((Sb1, Sb1k), (Sb2, Sb2k))):
                    tm, tmk = tmr.next()
                    pd, pdk = pdc[c]
                    self.ts('dve', tm[:], pd[:, 0:64], eb[:, 2 + c:3 + c], None, ALU.mult, None, [pdk, ebk], [tmk])
                    self.stt(Sf[:, hp, :], Sf[:, hp, :], eb[:, c:c + 1], tm[:], ALU.mult, ALU.add, ['Sf', ebk, tmk], ['Sf'])
                    self.cp('act', Sn[:, hp, :], Sf[:, hp, :], ['Sf'], [Snk])
                for e_ in range(2):
                    sl = slice(e_ * 64, (e_ + 1) * 64)
                    h = hp * 2 + e_
                    hs = slice(h * 64, (h + 1) * 64)
                    po, pok = poE[e_]
                    ps_ = slice(hp * 64, (hp + 1) * 64)
                    self.mm(po[:, ps_], at[:, e_, :], v[:, hs], True, False, [(atk, e_), vk], [pok])
                    self.mm(po[0:64, ps_], q1[sl, 0:64], Sb0[sl, hp, :], False, False, [q1k, Sb0k], [pok])
                    self.mm(po[64:128, ps_], q1[sl, 64:128], Sb1[sl, hp, :], False, True, [q1k, Sb1k], [pok])
            Sb, Sbk = Sb2, Sb2k
            ss, ssk = ssr.next()
            for e_ in range(2):
                self.act(jk[:, e_ * 128:(e_ + 1) * 128], poE[e_][0][:, 0:128], AF.Square, [poE[e_][1]], ['jk'])
            S.op('dve', lambda e, ss=ss: e.tensor_reduce(out=ss[:, 0:4], in_=jk[:].rearrange("p (h v) -> p h v", h=4),
                                                         axis=AX.X, op=ALU.add), ['jk'], [ssk])
            self.act(ss[:, 4:8], ss[:, 0:4], AF.Sqrt, [ssk], [ssk], bias=EPS, scale=1.0 / 64)
            S.op('dve', lambda e, ss=ss: e.reciprocal(out=ss[:, 8:12], in_=ss[:, 4:8]), [ssk], [ssk])
            y, yk = yr.next()
            for h in range(4):
                hs = slice(h * 64, (h + 1) * 64)
                hp, e_ = h // 2, h % 2
                si = 8 + e_ * 2 + hp
                self.stt(y[:, hs], poE[e_][0][:, hp * 64:(hp + 1) * 64], ss[:, si:si + 1], gnb[:, hs], ALU.mult, ALU.mult,
                         [poE[e_][1], ssk, 'gnb'], [yk])
            yb, ybk = ybr.next()
            self.tt('pool', yb[:], y[:], gt[:], ALU.mult, [yk, gtk], [ybk])
            py, pyk = pyt.next()
            for hp in range(2):
                self.tr(py[:, hp, :], yb[:, hp * 128:(hp + 1) * 128], self.identb[:], [ybk, 'identb'], [pyk])
            yT, yTk = yTr.next()
            self.cp('act', yT[:], py[:, 0:2, :], [pyk], [yTk])
            S.dma('sp', self.yT_d[:, br * 2:br * 2 + 2, t0:t0 + 128], yT[:], reads=[yTk], defer=True)
            yield
        yield


def phase_gla2(self, l, which=(1, 3), with_lru=True):
    S, NT, T = self.S, self.NT, self.T
    with ExitStack() as es:
        sb, pt, ring = _phase_tools(self, es)
        pkb = ring('pkb', 1, [128, 8, 128], BF16, psum=True)
        pkb_t, pkb_k = pkb.tiles[0]
        r_pkt = Ring.__new__(Ring)
        r_pkt.tiles, r_pkt.i = [(pkb_t[:, 4, :], pkb_k + '_kt')], 0
        r_pyt = Ring.__new__(Ring)
        r_pyt.tiles, r_pyt.i = [(pkb_t, pkb_k + '_yt')], 0
        shared = ([ring('pat', 1, [128, 512], F32, psum=True) for _ in range(2)],
                  r_pkt,
                  [ring('pds', 1, [128, 512], F32, psum=True) for _ in range(2)],
                  [ring('po', 1, [128, 512], F32, psum=True) for _ in range(2)],
                  r_pyt)
        plru = ring('plru', 1, [128, 512], F32, psum=True)
        gens = [self.phase_gla(l, br, shared) for br in which]
        L = self.phase_lru(l, plru) if with_lru else None
        nL = (T // 512) * 2
        doneL = 0
        for ti in range(NT):
            for g_ in gens:
                next(g_)
            if L is not None:
                want = ((ti + 1) * nL) // NT
                while doneL < want:
                    next(L)
                    doneL += 1
        for g_ in gens:
            next(g_)
        if L is not None:
            while doneL < nL:
                next(L)
                doneL += 1
            next(L)
            for _ in L:
                pass
        for g_ in reversed(gens):
            for _ in g_:
                pass
        S.barrier()


Prog.phase_gla2 = phase_gla2
Prog.phase_gla = phase_gla


def phase_lru(self, l, psr):
    nc, S, T = self.nc, self.S, self.T
    W = self.w[l]
    with ExitStack() as es:
        sb, pt, ring = _phase_tools(self, es)
        pv = sb('pv', [128, NPV], F32)
        cc_ = sb('cc', [128, 8], F32)
        wa = sb('wa', [128, 2, 128], BF16)
        wx = sb('wx', [128, 2, 128], BF16)
        stage = ring('stg', 2, [128, 64], F32)
        S.dma('sp', pv[:], W['pvec'], writes=['pv'])
        self.act(cc_[:, 0:2], pv[:, 20:22], AF.Exp, ['pv'], ['cc'], scale=-1.0)
        self.act(cc_[:, 0:2], cc_[:, 0:2], AF.Ln, ['cc'], ['cc'], bias=1.0)
        self.ts('dve', cc_[:, 2:4], cc_[:, 0:2], -8.0, None, ALU.mult, None, ['cc'], ['cc'])
        self.ts('dve', cc_[:, 4:6], cc_[:, 0:2], -16.0, None, ALU.mult, None, ['cc'], ['cc'])
        S.op('pool', lambda e: e.memset(wa[:], 0.0), [], ['wa'])
        S.op('pool', lambda e: e.memset(wx[:], 0.0), [], ['wx'])
        for n in range(4):
            cc, e_ = n // 2, n % 2
            sl = slice(e_ * 64, (e_ + 1) * 64)
            self.load_cast(stage, wa[sl, cc, e_ * 64:(e_ + 1) * 64], 'wa', W['w_rg_a'][n], [64], pbase=e_ * 64)
            self.load_cast(stage, wx[sl, cc, e_ * 64:(e_ + 1) * 64], 'wx', W['w_rg_x'][n], [64], pbase=e_ * 64)
        LV = int(os.environ.get('DBG_LRU', '9'))
        cxr = [ring('cx', 2, [128, 515], F32) for _ in range(2)]
        hhr = [ring('hh', 2, [128, 512], F32) for _ in range(2)]
        cyr = ring('cy', 2, [128, 512], F32)
        xcr = ring('xc', 2, [128, 512], F32)
        xbr = ring('xb', 2, [128, 512], BF16)
        rr = ring('r', 2, [128, 512], F32)
        ir = ring('i', 2, [128, 512], F32)
        ar = ring('a', 2, [128, 512], F32)
        a2r = ring('a2', 2, [128, 512], F32)
        ur = ring('u', 2, [128, 512], F32)
        gr = ring('gg', 2, [128, 512], F32)
        g2r = ring('g2', 2, [128, 512], F32)
        ycr = ring('yc', 2, [128, 512], BF16)
        prev = [None, None]
        prevh = [None, None]
        for bi in range(T // 512 if LV > 0 else 0):
            tok0 = bi * 512
            for cc in range(2):
                cx, cxk = cxr[cc].next()
                cy, cyk = cyr.next()
                S.dma('sp', cx[:, 3:515], self.fmf_d[:, 6 + cc, tok0:tok0 + 512], writes=[cxk])
                S.dma('sp', cy[:], self.fmf_d[:, 8 + cc, tok0:tok0 + 512], writes=[cyk])
                if prev[cc] is None:
                    S.op('pool', lambda e, cx=cx: e.memset(cx[:, 0:3], 0.0), [], [cxk])
                else:
                    self.cp('pool', cx[:, 0:3], prev[cc][0][:, 512:515], [prev[cc][1]], [cxk])
                prev[cc] = (cx, cxk)
                xc, xck = xcr.next()
                self.ts('dve', xc[:], cx[:, 3:515], pv[:, 14 + cc:15 + cc], pv[:, 6 + cc:7 + cc], ALU.mult, ALU.add,
                        [cxk, 'pv'], [xck])
                for j in range(3):
                    self.stt(xc[:], cx[:, j:j + 512], pv[:, 8 + 2 * j + cc:9 + 2 * j + cc], xc[:], ALU.mult, ALU.add,
                             [cxk, 'pv', xck], [xck])
                if LV < 2:
                    continue
                xb, xbk = xbr.next()
                self.cp('act', xb[:], xc[:], [xck], [xbk])
                r, rk = rr.next()
                i_, ik = ir.next()
                p1, p1k = psr.next()
                self.mm(p1[:], wa[:, cc, :], xb[:], True, True, ['wa', xbk], [p1k])
                self.act(r[:], p1[:], AF.Sigmoid, [p1k, 'pv'], [rk], bias=pv[:, 16 + cc:17 + cc])
                p2, p2k = psr.next()
                self.mm(p2[:], wx[:, cc, :], xb[:], True, True, ['wx', xbk], [p2k])
                self.act(i_[:], p2[:], AF.Sigmoid, [p2k, 'pv'], [ik], bias=pv[:, 18 + cc:19 + cc])
                a, ak = ar.next()
                a2, a2k = a2r.next()
                self.act(a[:], r[:], AF.Exp, [rk, 'cc'], [ak], scale=cc_[:, 2 + cc:3 + cc])
                self.act(a2[:], r[:], AF.Exp, [rk, 'cc'], [a2k], scale=cc_[:, 4 + cc:5 + cc])
                self.act(a2[:], a2[:], AF.Sqrt, [a2k], [a2k], bias=1.0, scale=-1.0)
                u, uk = ur.next()
                self.tt('dve', u[:], a2[:], i_[:], ALU.mult, [a2k, ik], [uk])
                self.tt('pool', u[:], u[:], xc[:], ALU.mult, [uk, xck], [uk])
                if LV < 3:
                    continue
                hh, hhk = hhr[cc].next()
                init = 0.0 if prevh[cc] is None else prevh[cc][0][:, 511:512]
                rd = [ak, uk] + ([] if prevh[cc] is None else [prevh[cc][1]])
                S.op('dve', lambda e, hh=hh, a=a, u=u, init=init: e.tensor_tensor_scan(
                    out=hh[:], data0=a[:], data1=u[:], initial=init, op0=ALU.mult, op1=ALU.add), rd, [hhk])
                prevh[cc] = (hh, hhk)
                if LV < 4:
                    continue
                g, gk = gr.next()
                g2, g2k = g2r.next()
                self.tt('pool', g[:], cy[:], cy[:], ALU.mult, [cyk], [gk])
                self.ts('dve', g[:], g[:], 0.044715, 1.0, ALU.mult, ALU.add, [gk], [gk])
                self.tt('pool', g[:], g[:], cy[:], ALU.mult, [gk, cyk], [gk])
                self.act(g2[:], g[:], AF.Sigmoid, [gk], [g2k], scale=1.5957691216057308)
                self.tt('dve', g2[:], g2[:], cy[:], ALU.mult, [g2k, cyk], [g2k])
                yc, yck = ycr.next()
                self.tt('pool', yc[:], g2[:], hh[:], ALU.mult, [g2k, hhk], [yck])
                S.dma('sp', self.yT_d[:, 4 + cc, tok0:tok0 + 512], yc[:], reads=[yck], defer=True)
                yield
        yield


Prog.phase_lru = phase_lru


def phase_merge(self, l, x_src):
    nc, S, T, NT = self.nc, self.S, self.T, self.NT
    W = self.w[l]
    with ExitStack() as es:
        sb, pt, ring = _phase_tools(self, es)
        wg = sb('wg', [128, 8, 4096], BF16)
        wbr = sb('wbr', [128, 8, 1024], BF16)
        wo = sb('wo', [128, 8, 1024], BF16)
        bgb = sb('bgb', [128, 4096], F32)
        stage = ring('stg', 2, [128, 2048], F32)
        S.dma('sp', bgb[:], W['bvec'][:, BV_BG:BV_BG + 4096].partition_broadcast(128), writes=['bgb'])
        for k in range(8):
            for c0 in (0, 2048):
                self.load_cast(stage, wg[:, k, c0:c0 + 2048], ('wg', k), W['w_gate'][k * 128:(k + 1) * 128, c0:c0 + 2048], [2048])
            self.load_cast(stage, wo[:, k, :], ('wo', k), W['w_out'][k * 128:(k + 1) * 128, :], [1024])
        for b_, nm in enumerate(('w_br_a', 'w_br_b', 'w_br_c', 'w_br_d')):
            for kk in range(2):
                self.load_cast(stage, wbr[:, b_ * 2 + kk, :], ('wbr', b_), W[nm][kk * 128:(kk + 1) * 128, :], [1024])
        hr = ring('hT', 2, [128, 8, 128], BF16)
        yr = ring('yT', 2, [128, 8, 128], BF16)
        xr = ring('x', 2, [128, 1024], F32)
        mr = ring('m', 2, [128, 1024], F32)
        mbr = ring('mb', 2, [128, 1024], BF16)
        mTr = ring('mT', 2, [128, 8, 128], BF16)
        gsr = ring('gs', 3, [128, 512], F32)
        tpr = ring('tp', 2, [128, 512], F32)
        xor_ = ring('xo', 2, [128, 1024], F32)
        pgr = ring('pg', 3, [128, 512], F32, psum=True)
        pyr = ring('py', 2, [128, 512], F32, psum=True)
        ptr = ring('pt', 1, [128, 8, 128], BF16, psum=True)
        for ti in range(NT):
            t0 = ti * 128
            hT, hTk = hr.next()
            yT, yTk = yr.next()
            x, xk = xr.next()
            S.dma('sp', hT[:], self.hT_d[:, :, t0:t0 + 128], writes=[hTk])
            S.dma('sp', yT[:], self.yT_d[:, :, t0:t0 + 128], writes=[yTk])
            S.dma('sp', x[:], x_src[t0:t0 + 128, :], writes=[xk])
            m, mk = mr.next()
            for b_ in range(4):
                for cb in range(2):
                    c0 = b_ * 1024 + cb * 512
                    pg, pgk = pgr.next()
                    for k in range(8):
                        self.mm(pg[:], hT[:, k, :], wg[:, k, c0:c0 + 512], k == 0, k == 7, [hTk, ('wg', k)], [pgk])
                    py, pyk = pyr.next()
                    for kk in range(2):
                        self.mm(py[:], yT[:, b_ * 2 + kk, :], wbr[:, b_ * 2 + kk, cb * 512:(cb + 1) * 512], kk == 0, kk == 1,
                                [yTk, ('wbr', b_)], [pyk])
                    gs, gsk = gsr.next()
                    self.tt('dve', gs[:], pg[:], bgb[:, c0:c0 + 512], ALU.add, [pgk, 'bgb'], [gsk])
                    self.act(gs[:], gs[:], AF.Sigmoid, [gsk], [gsk])
                    ms = m[:, cb * 512:(cb + 1) * 512]
                    if b_ == 0:
                        self.tt('dve', ms, gs[:], py[:], ALU.mult, [gsk, pyk], [(mk, cb)])
                    else:
                        tp, tpk = tpr.next()
                        self.tt('dve', tp[:], gs[:], py[:], ALU.mult, [gsk, pyk], [tpk])
                        self.tt('pool', ms, ms, tp[:], ALU.add, [(mk, cb), tpk], [(mk, cb)])
            mb, mbk = mbr.next()
            self.cp('act', mb[:], m[:], [(mk, 0), (mk, 1)], [mbk])
            p, pk = ptr.next()
            for k in range(8):
                self.tr(p[:, k, :], mb[:, k * 128:(k + 1) * 128], self.identb[:], [mbk, 'identb'], [pk])
            mT, mTk = mTr.next()
            self.cp('act', mT[:], p[:], [pk], [mTk])
            xo, xok = xor_.next()
            for cb in range(2):
                po, pok = pgr.next()
                for k in range(8):
                    self.mm(po[:], mT[:, k, :], wo[:, k, cb * 512:(cb + 1) * 512], k == 0, k == 7, [mTk, ('wo', k)], [pok])
                self.tt('dve', xo[:, cb * 512:(cb + 1) * 512], x[:, cb * 512:(cb + 1) * 512], po[:], ALU.add,
                        [xk, pok], [xok])
            S.dma('sp', self.xmid_d[t0:t0 + 128, :], xo[:], reads=[xok], defer=True)
        S.barrier()


Prog.phase_merge = phase_merge


def phase_ffn(self, l, x_dst, final):
    nc, S, T = self.nc, self.S, self.T
    W = self.w[l]
    NS = 256
    with ExitStack() as es:
        sb, pt, ring = _phase_tools(self, es)
        wf1 = sb('wf1', [128, 8, 4096], BF16)
        wf2 = sb('wf2', [128, 32, 1024], BF16)
        g2b = sb('g2b', [128, 1024], F32)
        gfb = sb('gfb', [128, 1024], F32)
        stage = ring('stg', 2, [128, 1024], F32)
        S.dma('sp', g2b[:], W['bvec'][:, BV_N2:BV_N2 + 1024].partition_broadcast(128), writes=['g2b'])
        S.dma('sp', gfb[:], W['bvec'][:, BV_FN:BV_FN + 1024].partition_broadcast(128), writes=['gfb'])
        for k in range(8):
            for c0 in range(0, 4096, 1024):
                self.load_cast(stage, wf1[:, k, c0:c0 + 1024], ('wf1', k), W['w_ff1'][k * 128:(k + 1) * 128, c0:c0 + 1024], [1024])
        for f in range(32):
            self.load_cast(stage, wf2[:, f, :], ('wf2', f), W['w_ff2'][f * 128:(f + 1) * 128, :], [1024])
        xr = ring('xm', 3, [128, 1024], F32)
        junk = sb('junk', [128, 1024], F32)
        hr = ring('h2', 2, [128, 1024], BF16)
        hTr = ring('h2T', 1, [128, 8, NS], BF16)
        uTr = ring('uT', 1, [128, 32, NS], BF16)
        sqr = ring('sq', 2, [128, NS], F32)
        st1 = ring('st', 4, [128, 4], F32)
        xor_ = ring('xo', 2, [128, 1024], F32)
        ptr = ring('pt', 2, [128, 8, 128], BF16, psum=True)
        pur = ring('pu', 3, [128, 512], F32, psum=True)
        por = ring('po', 3, [128, 512], F32, psum=True)
        for si in range(T // NS):
            tok0 = si * NS
            hT, hTk = hTr.next()
            xs = []
            for j in range(NS // 128):
                t0 = tok0 + j * 128
                x, xk = xr.next()
                xs.append((x, xk))
                S.dma('sp', x[:], self.xmid_d[t0:t0 + 128, :], writes=[xk])
                s1, s1k = st1.next()
                self.act(junk[:], x[:], AF.Square, [xk], ['junk', s1k], accum=s1[:, 0:1])
                self.act(s1[:, 1:2], s1[:, 0:1], AF.Sqrt, [s1k], [s1k], bias=EPS, scale=1.0 / 1024)
                S.op('dve', lambda e, s1=s1: e.reciprocal(out=s1[:, 2:3], in_=s1[:, 1:2]), [s1k], [s1k])
                h, hk = hr.next()
                self.stt(h[:], x[:], s1[:, 2:3], g2b[:], ALU.mult, ALU.mult, [xk, s1k, 'g2b'], [hk])
                p, pk = ptr.next()
                for k in range(8):
                    self.tr(p[:, k, :], h[:, k * 128:(k + 1) * 128], self.identb[:], [hk, 'identb'], [pk])
                self.cp('act', hT[:, :, j * 128:(j + 1) * 128], p[:], [pk], [hTk])
            uT, uTk = uTr.next()
            for f in range(32):
                pu, puk = pur.next()
                for k in range(8):
                    self.mm(pu[:, 0:NS], wf1[:, k, f * 128:(f + 1) * 128], hT[:, k, :], k == 0, k == 7,
                            [hTk, ('wf1', k)], [puk])
                sq, sqk = sqr.next()
                self.act(sq[:], pu[:, 0:NS], AF.Square, [puk], [sqk])
                self.stt(uT[:, f, :], pu[:, 0:NS], 0.0, sq[:], ALU.is_gt, ALU.mult, [puk, sqk], [(uTk, f)])
            for j in range(NS // 128):
                t0 = tok0 + j * 128
                x, xk = xs[j]
                xo, xok = xor_.next()
                for cb in range(2):
                    po, pok = por.next()
                    for f in range(32):
                        self.mm(po[:], uT[:, f, j * 128:(j + 1) * 128], wf2[:, f, cb * 512:(cb + 1) * 512], f == 0, f == 31,
                                [(uTk, f), ('wf2', f)], [pok])
                    self.tt('dve', xo[:, cb * 512:(cb + 1) * 512], x[:, cb * 512:(cb + 1) * 512], po[:], ALU.add,
                            [xk, pok], [xok])
                if final:
                    s1, s1k = st1.next()
                    self.act(junk[:], xo[:], AF.Square, [xok], ['junk', s1k], accum=s1[:, 0:1])
                    self.act(s1[:, 1:2], s1[:, 0:1], AF.Sqrt, [s1k], [s1k], bias=EPS, scale=1.0 / 1024)
                    S.op('dve', lambda e, s1=s1: e.reciprocal(out=s1[:, 2:3], in_=s1[:, 1:2]), [s1k], [s1k])
                    self.stt(xo[:], xo[:], s1[:, 2:3], gfb[:], ALU.mult, ALU.mult, [xok, s1k, 'gfb'], [xok])
                S.dma('sp', x_dst[t0:t0 + 128, :], xo[:], reads=[xok], defer=True)
        S.barrier()


Prog.phase_ffn = phase_ffn


_CACHE = {}


def kernel(**inputs):
    inp = {k: np.asarray(v) for k, v in inputs.items()}
    B, T, D = inp['x'].shape
    P = Prog(T, depth=DEPTH)
    nc = P.build()
    in_maps = [make_in_map(inp, b) for b in range(B)]
    res = run_bass_kernel_spmd(nc, in_maps, core_ids=list(range(B)))
    out = np.stack([np.asarray(r['out'], dtype=np.float32) for r in res.results], axis=0)
    return out
```

```python
import numpy as np
import concourse.bass as bass
import concourse.mybir as mybir
import os
from contextlib import ExitStack
from concourse.bass_utils import run_bass_kernel_spmd

F32 = mybir.dt.float32
BF16 = mybir.dt.bfloat16
AF = mybir.ActivationFunctionType
ALU = mybir.AluOpType
AX = mybir.AxisListType

D_MODEL = 1024
DEPTH = 2
EPS = 1e-6
D_FF = 4096
IN_COLS = 3284
O_AQ, O_CKV, O_IQ, O_IK, O_IW = 0, 256, 384, 640, 704
O_BQ, O_BK, O_BV, O_BLR, O_BR = 708, 964, 1220, 1476, 1492
O_CX, O_CY = 1748, 2004
O_DQ, O_DF, O_DI, O_DG = 2260, 2516, 2772, 3028


class Sched:
    ENG = ('pe', 'dve', 'act', 'pool', 'sp')
    LOOKBACK = 3
    SEM_MAX = 30000

    def __init__(self, nc):
        self.nc = nc
        self.q = {e: [] for e in self.ENG}
        self.sem = {}
        self.cnt = {}
        self.old = {}
        self.all_sems = []
        self.nsem = 0
        for e in self.ENG:
            if e != 'sp':
                self._new_sem(e)
        self.known = {e: {} for e in self.ENG}
        self.res = {}
        self.dma_ring = []
        self.dma_i = 0
        self.NDMA = 40
        self.pend = []
        self.pend_sems = set()
        self.pend_age = 0
        self.DEFER_LOADS = 3
        self.n_ops = 0

    def _alloc(self, name):
        self.nsem += 1
        h = self.nc.alloc_semaphore(f"{name}_{self.nsem}")
        self.all_sems.append(h)
        return h

    def _new_sem(self, e):
        if e in self.sem:
            self.old.setdefault(e, []).append((self.sem[e], self.cnt[e]))
        self.sem[e] = self._alloc('s' + e)
        self.cnt[e] = 0

    def _need(self, eng, ev, waits, use_known=True):
        if ev is None:
            return
        sem, val, src = ev
        if sem.name in self.pend_sems:
            self.flush()
        if src == eng:
            if eng == 'pe':
                return
            if sem is self.sem[eng] and val <= self.cnt[eng] - self.LOOKBACK:
                return
            if sem is not self.sem[eng]:
                return
        k = self.known[eng]
        if use_known and k.get(sem.name, 0) >= val:
            return
        cur = waits.get(sem.name)
        if cur is None or cur[1] < val:
            waits[sem.name] = (sem, val)

    def _deps(self, eng, reads, writes, use_known=True):
        waits = {}
        for r in reads:
            st = self.res.get(r)
            if st is not None:
                self._need(eng, st[0], waits, use_known)
        for w in writes:
            st = self.res.get(w)
            if st is not None:
                self._need(eng, st[0], waits, use_known)
                for ev in st[1].values():
                    self._need(eng, ev, waits, use_known)
        if use_known:
            for name, (sem, val) in waits.items():
                self.known[eng][name] = val
        return list(waits.values())

    def _commit(self, eng, ev, reads, writes):
        for r in reads:
            st = self.res.setdefault(r, [None, {}])
            st[1][eng if ev[2] != 'dma' else ('dma', ev[0].name)] = ev
        for w in writes:
            self.res[w] = [ev, {}]

    def op(self, eng, fn, reads=(), writes=()):
        waits = self._deps(eng, reads, writes)
        if self.cnt[eng] >= self.SEM_MAX:
            self._new_sem(eng)
        sem = self.sem[eng]
        self.cnt[eng] += 1
        ev = (sem, self.cnt[eng], eng)
        self._commit(eng, ev, reads, writes)
        self.q[eng].append((waits, fn, sem, 1))
        self.n_ops += 1

    def flush(self):
        k = self.known['sp']
        for (waits, fn, sem, inc) in self.pend:
            w2 = []
            for (s_, v_) in waits:
                if k.get(s_.name, 0) < v_:
                    k[s_.name] = v_
                    w2.append((s_, v_))
            self.q['sp'].append((w2, fn, sem, inc))
        self.pend = []
        self.pend_sems = set()
        self.pend_age = 0

    def dma(self, eng, out, in_, reads=(), writes=(), defer=False):
        eng = 'sp'
        if len(self.dma_ring) < self.NDMA:
            self.dma_ring.append([self._alloc('d'), 0, None])
            slot = self.dma_ring[-1]
        else:
            slot = self.dma_ring[self.dma_i % self.NDMA]
        self.dma_i += 1
        if slot[0].name in self.pend_sems:
            self.flush()
        waits = self._deps(eng, reads, writes, use_known=not defer)
        if slot[2] is not None:
            w2 = {}
            self._need(eng, slot[2], w2, use_known=not defer)
            for name, (sem, val) in w2.items():
                if not defer:
                    self.known[eng][name] = val
                waits = [w for w in waits if w[0].name != name] + [(sem, val)]
        slot[1] += 16
        ev = (slot[0], slot[1], 'dma')
        slot[2] = ev
        self._commit(eng, ev, reads, writes)
        ent = (waits, lambda e, o=out, i=in_: e.dma_start(out=o, in_=i), slot[0], 16)
        if defer:
            self.pend.append(ent)
            self.pend_sems.add(slot[0].name)
        else:
            self.q[eng].append(ent)
            if self.pend:
                self.pend_age += 1
                if self.pend_age >= self.DEFER_LOADS:
                    self.flush()
        self.n_ops += 1

    def barrier(self):
        self.flush()
        for e in self.ENG:
            waits = []
            k = self.known[e]
            for f in self.ENG:
                if f == 'sp' or (f == e and e == 'pe'):
                    continue
                for (sem, c) in self.old.get(f, []) + [(self.sem[f], self.cnt[f])]:
                    if c > 0 and k.get(sem.name, 0) < c:
                        waits.append((sem, c))
                        k[sem.name] = c
            for slot in self.dma_ring:
                if slot[2] is not None and k.get(slot[0].name, 0) < slot[1]:
                    waits.append((slot[0], slot[1]))
                    k[slot[0].name] = slot[1]
            self.q[e].append((waits, None, None, 0))
        self.res = {}

    def finish(self, eng='sp'):
        self.flush()
        waits = {}
        for slot in self.dma_ring:
            if slot[2] is not None:
                self._need(eng, slot[2], waits)
        self.q[eng].append((list(waits.values()), None, None, 0))

    def emit(self):
        nc = self.nc
        q = self.q

        def run(e, lst):
            for waits, fn, sem, inc in lst:
                for (s, v) in waits:
                    e.wait_ge(s, v)
                if fn is not None:
                    ins = fn(e)
                    ins.then_inc(sem, inc)

        for h in self.all_sems:
            nc.sync.sem_clear(h)
        with nc.Block() as block:
            @block.tensor
            def _(e):
                run(e, q['pe'])

            @block.vector
            def _(e):
                run(e, q['dve'])

            @block.scalar
            def _(e):
                run(e, q['act'])

            @block.gpsimd
            def _(e):
                run(e, q['pool'])

            @block.sync
            def _(e):
                run(e, q['sp'])


class Ring:
    def __init__(self, nc, name, n, shape, dtype, psum=False):
        self.tiles = []
        for i in range(n):
            if psum:
                t = nc.alloc_psum_tensor(f"{name}{i}", shape, dtype)
            else:
                t = nc.alloc_sbuf_tensor(f"{name}{i}", shape, dtype)
            self.tiles.append((t, f"{name}{i}"))
        self.i = 0

    def next(self):
        t = self.tiles[self.i % len(self.tiles)]
        self.i += 1
        return t


NPV = 24


def pack_pvec(inp, l):
    def c2(v):
        return np.ascontiguousarray(np.asarray(v, np.float32).reshape(2, 128).T)
    cols = [c2(inp['b_gk'][l]), c2(inp['lb_param'][0]), c2(inp['lb_param'][1]), c2(inp['conv_b'][l])]
    for j in range(4):
        cols.append(c2(inp['conv_w'][l][j]))
    cols += [c2(inp['b_rg_a'][l]), c2(inp['b_rg_x'][l]), c2(inp['lru_lambda'][l])]
    pv = np.concatenate(cols, axis=1)
    out = np.zeros((128, NPV), np.float32)
    out[:, :pv.shape[1]] = pv
    return out


BV_N1, BV_KV, BV_GLA, BV_HG, BV_BG, BV_N2, BV_FN = 0, 1024, 1152, 1408, 1664, 5760, 6784
NBV = 7808


def pack_bvec(inp, l):
    v = np.concatenate([
        inp['norm1_g'][l], inp['kv_norm_g'][l], np.tile(inp['gla_norm_g'][l], 4),
        np.tile(inp['hgrn_norm_g'][l], 4), inp['b_gate'][l], inp['norm2_g'][l], inp['final_norm_g']
    ]).astype(np.float32)
    assert v.shape[0] == NBV
    return v.reshape(1, NBV)


class Prog:
    def __init__(self, T, depth=DEPTH, debug=()):
        self.T = T
        self.NT = T // 128
        self.depth = depth
        self.debug = set(debug)
        nc = self.nc = bass.Bass("TRN2", target_bir_lowering=False)
        self.S = Sched(nc)
        self.uid = 0
        dt = nc.dram_tensor
        self.x_in = dt("x", [T, D_MODEL], F32, kind="ExternalInput").ap()
        self.out = dt("out", [T, D_MODEL], F32, kind="ExternalOutput").ap()
        self.w = []
        for l in range(depth):
            d = {}
            for name, shape in [('w_in', [D_MODEL, IN_COLS]), ('w_gate', [D_MODEL, 4096]),
                                ('w_uk', [4, 64, 128]), ('w_uv', [4, 128, 64]), ('w_gk2', [16, 256]),
                                ('w_rg_a', [4, 64, 64]), ('w_rg_x', [4, 64, 64]),
                                ('w_br_a', [256, 1024]), ('w_br_b', [256, 1024]), ('w_br_c', [256, 1024]),
                                ('w_br_d', [256, 1024]), ('w_out', [1024, 1024]), ('w_ff1', [1024, 4096]),
                                ('w_ff2', [4096, 1024]), ('pvec', [128, NPV]), ('bvec', [1, NBV])]:
                d[name] = dt(f"{name}{l}", shape, F32, kind="ExternalInput").ap()
            self.w.append(d)
        self.biasT = dt("biasT", [128, 4, 256], F32, kind="ExternalInput").ap()
        self.scr = {}

    def scratch(self, name, shape, dtype):
        kind = "ExternalOutput" if name in self.debug else "Internal"
        t = self.nc.dram_tensor(name, shape, dtype, kind=kind).ap()
        self.scr[name] = t
        return t

    def u(self, p):
        self.uid += 1
        return f"{p}{self.uid}"

    def act(self, out, in_, func, reads, writes, bias=0.0, scale=1.0, accum=None):
        kw = {}
        if accum is not None:
            kw['accum_out'] = accum
        self.S.op('act', lambda e: e.activation(out=out, in_=in_, func=func, bias=bias, scale=scale, **kw),
                  reads, writes)

    def mm(self, out, lhsT, rhs, start, stop, reads, writes):
        self.S.op('pe', lambda e: e.matmul(out, lhsT, rhs, start=start, stop=stop), reads, writes)

    def tr(self, out, in_, ident, reads, writes):
        self.S.op('pe', lambda e: e.transpose(out, in_, ident), reads, writes)

    def tt(self, eng, out, in0, in1, op, reads, writes):
        self.S.op(eng, lambda e: e.tensor_tensor(out=out, in0=in0, in1=in1, op=op), reads, writes)

    def ts(self, eng, out, in0, s1, s2, op0, op1, reads, writes, accum=None):
        if op1 is None:
            self.S.op(eng, lambda e: e.tensor_scalar(out=out, in0=in0, scalar1=s1, scalar2=None, op0=op0),
                      reads, writes)
        elif accum is not None:
            self.S.op(eng, lambda e: e.tensor_scalar(out=out, in0=in0, scalar1=s1, scalar2=s2, op0=op0, op1=op1,
                                                     accum_out=accum), reads, writes)
        else:
            self.S.op(eng, lambda e: e.tensor_scalar(out=out, in0=in0, scalar1=s1, scalar2=s2, op0=op0, op1=op1),
                      reads, writes)

    def stt(self, out, in0, scalar, in1, op0, op1, reads, writes):
        self.S.op('dve', lambda e: e.scalar_tensor_tensor(out=out, in0=in0, scalar=scalar, in1=in1, op0=op0, op1=op1),
                  reads, writes)

    def cp(self, eng, out, in_, reads, writes):
        if eng == 'act':
            self.S.op('act', lambda e: e.copy(out=out, in_=in_), reads, writes)
        else:
            self.S.op(eng, lambda e: e.tensor_copy(out=out, in_=in_), reads, writes)

    def load_cast(self, ring, dst, dkey, src, shape, engs=('pool', 'dve'), pbase=0):
        st, sk = ring.next()
        if len(shape) == 1:
            sv = st[:, 0:shape[0]]
        else:
            n = shape[0] * shape[1]
            sv = st[:, 0:n].rearrange("p (a b) -> p a b", a=shape[0])
        np_ = dst.shape[0]
        self.S.dma('sp', sv[pbase:pbase + np_], src, writes=[sk])
        self.cast_i = getattr(self, 'cast_i', 0) + 1
        self.cp(engs[self.cast_i % len(engs)], dst, sv[pbase:pbase + np_], [sk], [dkey])

    def alloc_scratch(self):
        T = self.T
        self.hT_d = self.scratch("hT_d", [128, 8, T], BF16)
        self.fmf_d = self.scratch("fmf_d", [128, 16, T], F32)
        self.fmb_d = self.scratch("fmb_d", [128, 7, T], BF16)
        self.tmb_d = self.scratch("tmb_d", [T, 641], BF16)
        self.tmf_d = self.scratch("tmf_d", [T, 512], F32)
        self.ckvT_d = self.scratch("ckvT_d", [128, T], BF16)
        self.iw_d = self.scratch("iw_d", [128, self.NT, 4], F32)
        self.yT_d = self.scratch("yT_d", [128, 8, T], BF16)
        self.xmid_d = self.scratch("xmid_d", [T, D_MODEL], F32)
        self.xres_d = self.scratch("xres_d", [T, D_MODEL], F32)

    def consts(self, es):
        nc, S = self.nc, self.S
        self.ident_in = nc.dram_tensor("ident", [128, 128], F32, kind="ExternalInput").ap()
        self.cmask_in = nc.dram_tensor("cmask", [128, 128], F32, kind="ExternalInput").ap()
        self.bdmask_in = nc.dram_tensor("bdmask", [128, 128], F32, kind="ExternalInput").ap()
        self.scanm_in = nc.dram_tensor("scanm", [128, 128], F32, kind="ExternalInput").ap()
        self.cneg_in = nc.dram_tensor("cneg", [128, 128], F32, kind="ExternalInput").ap()
        self.relb_in = nc.dram_tensor("relb", [32, 4], F32, kind="ExternalInput").ap()
        self.identb = es.enter_context(nc.sbuf_tensor("identb", [128, 128], BF16))
        self.identf = es.enter_context(nc.sbuf_tensor("identf", [128, 128], F32))
        S.dma('sp', self.identf[:], self.ident_in, writes=['identf'])
        self.cp('dve', self.identb[:], self.identf[:], ['identf'], ['identb'])

    def phase1(self, l, x_src):
        nc, S, T = self.nc, self.S, self.T
        W = self.w[l]
        with ExitStack() as es:
            def sb(name, shape, dt):
                return es.enter_context(nc.sbuf_tensor(self.u(name), shape, dt))

            def pt(name, shape, dt):
                return es.enter_context(nc.psum_tensor(self.u(name), shape, dt))

            def ring(name, n, shape, dt, psum=False):
                tiles = []
                for i in range(n):
                    nm = self.u(name)
                    t = pt(nm, shape, dt) if psum else sb(nm, shape, dt)
                    tiles.append((t, nm))
                r = Ring.__new__(Ring)
                r.tiles, r.i = tiles, 0
                return r

            wfm = sb('wfm', [128, 8, 2304], BF16)
            wtm = sb('wtm', [128, 8, 1156], BF16)
            wuk = sb('wuk', [128, 2, 128], BF16)
            wgk = sb('wgk', [16, 256], BF16)
            pv = sb('pv', [128, NPV], F32)
            lbc = sb('lbc', [128, 8], F32)
            nbgk = sb('nbgk', [128, 2], F32)
            g1b = sb('g1b', [128, 1024], F32)
            gkvb = sb('gkvb', [128, 128], F32)
            iw_all = sb('iw_all', [128, self.NT, 4], F32)
            stage = ring('stg', 3, [128, IN_COLS], F32)

            S.dma('sp', pv[:], W['pvec'], writes=['pv'])
            S.dma('sp', g1b[:], W['bvec'][:, BV_N1:BV_N1 + 1024].partition_broadcast(128), writes=['g1b'])
            S.dma('sp', gkvb[:], W['bvec'][:, BV_KV:BV_KV + 128].partition_broadcast(128), writes=['gkvb'])
            if l == 0:
                S.op('dve', lambda e: e.memset(lbc[:, 0:2], 0.0), [], ['lbc'])
            else:
                self.tt('dve', lbc[:, 6:8], pv[:, 4:6], pv[:, 2:4], ALU.subtract, ['pv'], ['lbc'])
                self.act(lbc[:, 0:2], lbc[:, 6:8], AF.Sigmoid, ['lbc'], ['lbc'])
            self.ts('dve', lbc[:, 2:4], lbc[:, 0:2], -1.0, 1.0, ALU.mult, ALU.add, ['lbc'], ['lbc'])
            self.ts('dve', lbc[:, 4:6], lbc[:, 0:2], 1e-20, None, ALU.max, None, ['lbc'], ['lbc'])
            self.ts('dve', nbgk[:], pv[:, 0:2], -1.0, None, ALU.mult, None, ['pv'], ['nbgk'])

            fm_src = [(O_AQ, 256), (O_IQ, 256), (O_IK, 64), (O_IK, 64), (O_BQ, 256), (O_BK, 256), (O_BLR, 16),
                      (O_CX, 256), (O_CY, 256), (O_DQ, 256), (O_DF, 256)]
            fm_dst = [0, 256, 512, 576, 640, 896, 1152, 1280, 1536, 1792, 2048]
            tm_src = [(O_CKV, 128), (O_IW, 4), (O_BV, 256), (O_BR, 256), (O_DI, 256), (O_DG, 256)]
            tm_dst = [0, 128, 132, 388, 644, 900]
            for k in range(8):
                st, sk = stage.next()
                S.dma('sp', st[:, 0:IN_COLS], W['w_in'][k * 128:(k + 1) * 128, :], writes=[sk])
                ci_ = 0
                for dst_t, key, srcs, dsts in ((wfm, ('wfm', k), fm_src, fm_dst), (wtm, ('wtm', k), tm_src, tm_dst)):
                    for (so, n), do in zip(srcs, dsts):
                        ci_ += 1
                        self.cp(('pool', 'dve', 'act')[ci_ % 3], dst_t[:, k, do:do + n], st[:, so:so + n], [sk], [key])
            self.load_cast(stage, wuk[:], 'wuk', W['w_uk'].rearrange("(hp e) d c -> (e d) hp c", e=2), [2, 128])
            self.load_cast(stage, wgk[:], 'wgk', W['w_gk2'], [256])

            xr = ring('xt', 3, [128, 1024], F32)
            junk = sb('junk', [128, 1024], F32)
            hr = ring('h', 2, [128, 1024], BF16)
            hTr = ring('hT', 2, [128, 8, 512], BF16)
            st1 = ring('st1', 4, [128, 4], F32)
            ptr = ring('ptr', 2, [128, 8, 128], BF16, psum=True)
            psr = ring('ps', 4, [128, 512], F32, psum=True)
            ptc = ring('ptc', 1, [128, 128], BF16, psum=True)
            tmbr = ring('tmb', 3, [128, 641], BF16)
            tmfr = ring('tmf', 3, [128, 512], F32)
            ckvTr = ring('ckvT', 2, [128, 512], BF16)
            qTr = ring('qT', 2, [128, 2, 512], BF16)
            blrr = ring('blr', 2, [16, 512], BF16)
            ofr = ring('of', 6, [128, 512], F32)
            obr = ring('ob', 4, [128, 512], BF16)
            for t_, _k in tmbr.tiles:
                S.op('pool', lambda e, t_=t_: e.memset(t_[:, 128:129], 1.0), [], [_k])

            for st_i in range(T // 512):
                tok0 = st_i * 512
                hT, hTk = hTr.next()
                ckvT, ckvTk = ckvTr.next()
                for j in range(4):
                    t0 = tok0 + j * 128
                    tt_i = t0 // 128
                    xt, xk = xr.next()
                    S.dma('sp', xt[:], x_src[t0:t0 + 128, :], writes=[xk])
                    s1, s1k = st1.next()
                    self.act(junk[:], xt[:], AF.Square, [xk], ['junk', s1k], accum=s1[:, 0:1])
                    self.act(s1[:, 1:2], s1[:, 0:1], AF.Sqrt, [s1k], [s1k], bias=EPS, scale=1.0 / 1024)
                    S.op('dve', lambda e, s1=s1: e.reciprocal(out=s1[:, 2:3], in_=s1[:, 1:2]), [s1k], [s1k])
                    h, hk = hr.next()
                    self.stt(h[:], xt[:], s1[:, 2:3], g1b[:], ALU.mult, ALU.mult, [xk, s1k, 'g1b'], [hk])
                    p, pk = ptr.next()
                    for k in range(8):
                        self.tr(p[:, k, :], h[:, k * 128:(k + 1) * 128], self.identb[:], [hk, 'identb'], [pk])
                    self.cp('act', hT[:, :, j * 128:(j + 1) * 128], p[:], [pk], [hTk])
                    tmb, tmbk = tmbr.next()
                    tmf, tmfk = tmfr.next()
                    pa, pak = psr.next()
                    for k in range(8):
                        self.mm(pa[:, 0:388], hT[:, k, j * 128:(j + 1) * 128], wtm[:, k, 0:388], k == 0, k == 7,
                                [hTk, ('wtm', k)], [pak])
                    s2, s2k = st1.next()
                    self.act(junk[:, 0:128], pa[:, 0:128], AF.Square, [pak], ['junk', s2k], accum=s2[:, 0:1])
                    self.act(s2[:, 1:2], s2[:, 0:1], AF.Sqrt, [s2k], [s2k], bias=EPS, scale=1.0 / 128)
                    S.op('dve', lambda e, s2=s2: e.reciprocal(out=s2[:, 2:3], in_=s2[:, 1:2]), [s2k], [s2k])
                    self.stt(tmb[:, 0:128], pa[:, 0:128], s2[:, 2:3], gkvb[:], ALU.mult, ALU.mult,
                             [pak, s2k, 'gkvb'], [tmbk])
                    self.ts('dve', iw_all[:, tt_i, :], pa[:, 128:132], 1.0 / 16, None, ALU.mult, None, [pak], ['iw_all'])
                    self.cp('act', tmb[:, 129:385], pa[:, 132:388], [pak], [tmbk])
                    pc, pck = ptc.next()
                    self.tr(pc[:], tmb[:, 0:128], self.identb[:], [tmbk, 'identb'], [pck])
                    self.cp('dve', ckvT[:, j * 128:(j + 1) * 128], pc[:], [pck], [ckvTk])
                    pb, pbk = psr.next()
                    for k in range(8):
                        self.mm(pb[:, 0:512], hT[:, k, j * 128:(j + 1) * 128], wtm[:, k, 388:900], k == 0, k == 7,
                                [hTk, ('wtm', k)], [pbk])
                    self.act(tmf[:, 0:256], pb[:, 0:256], AF.Silu, [pbk], [tmfk])
                    self.cp('dve', tmb[:, 385:641], pb[:, 256:512], [pbk], [tmbk])
                    pcc, pcck = psr.next()
                    for k in range(8):
                        self.mm(pcc[:, 0:256], hT[:, k, j * 128:(j + 1) * 128], wtm[:, k, 900:1156], k == 0, k == 7,
                                [hTk, ('wtm', k)], [pcck])
                    self.act(tmf[:, 256:512], pcc[:, 0:256], AF.Silu, [pcck], [tmfk])
                    S.dma('sp', self.tmb_d[t0:t0 + 128, :], tmb[:], reads=[tmbk], defer=True)
                    S.dma('sp', self.tmf_d[t0:t0 + 128, :], tmf[:], reads=[tmfk], defer=True)
                S.dma('sp', self.hT_d[:, :, tok0:tok0 + 512], hT[:], reads=[hTk], defer=True)
                S.dma('sp', self.ckvT_d[:, tok0:tok0 + 512], ckvT[:], reads=[ckvTk], defer=True)

                def fm_block(blk, M=128):
                    pf, pfk = psr.next()
                    for k in range(8):
                        self.mm(pf[0:M, :], wfm[:, k, blk * 128:blk * 128 + M], hT[:, k, :], k == 0, k == 7,
                                [hTk, ('wfm', k)], [pfk])
                    return pf, pfk

                def out_f(slot):
                    o, ok = ofr.next()
                    return o, ok, self.fmf_d[:, slot, tok0:tok0 + 512]

                def out_b(slot):
                    o, ok = obr.next()
                    return o, ok, self.fmb_d[:, slot, tok0:tok0 + 512]

                qT, qTk = qTr.next()
                for hp in range(2):
                    pf, pfk = fm_block(0 + hp)
                    self.cp('act', qT[:, hp, :], pf[:], [pfk], [qTk])
                for h in range(4):
                    hp, e_ = h // 2, h % 2
                    pf, pfk = psr.next()
                    self.mm(pf[:], wuk[e_ * 64:(e_ + 1) * 64, hp, :], qT[e_ * 64:(e_ + 1) * 64, hp, :], True, True,
                            ['wuk', qTk], [pfk])
                    o, ok, dst = out_b(3 + h)
                    self.ts('dve', o[:], pf[:], 0.125, None, ALU.mult, None, [pfk], [ok])
                    S.dma('sp', dst, o[:], reads=[ok], defer=True)
                for hp in range(2):
                    pf, pfk = fm_block(2 + hp)
                    o, ok, dst = out_b(0 + hp)
                    self.cp('act', o[:], pf[:], [pfk], [ok])
                    S.dma('sp', dst, o[:], reads=[ok], defer=True)
                pf, pfk = fm_block(4)
                o, ok, dst = out_b(2)
                self.cp('dve', o[:], pf[:], [pfk], [ok])
                S.dma('sp', dst, o[:], reads=[ok], defer=True)
                for hp in range(2):
                    pf, pfk = fm_block(5 + hp)
                    o, ok, dst = out_f(0 + hp)
                    self.ts('dve', o[:], pf[:], 0.125, None, ALU.mult, None, [pfk], [ok])
                    S.dma('sp', dst, o[:], reads=[ok], defer=True)
                for hp in range(2):
                    pf, pfk = fm_block(7 + hp)
                    o, ok, dst = out_f(2 + hp)
                    self.cp('act', o[:], pf[:], [pfk], [ok])
                    S.dma('sp', dst, o[:], reads=[ok], defer=True)
                pf, pfk = fm_block(9, M=16)
                blr, blrk = blrr.next()
                self.cp('dve', blr[:], pf[0:16, :], [pfk], [blrk])
                for hp in range(2):
                    pg, pgk = psr.next()
                    self.mm(pg[:], wgk[0:16, hp * 128:(hp + 1) * 128], blr[0:16, :], True, True, ['wgk', blrk], [pgk])
                    o, ok, dst = out_f(4 + hp)
                    self.act(o[:], pg[:], AF.Exp, [pgk, 'nbgk'], [ok], bias=nbgk[:, hp:hp + 1], scale=-1.0)
                    self.act(o[:], o[:], AF.Ln, [ok], [ok], bias=1.0)
                    self.ts('dve', o[:], o[:], -1.0 / 16, None, ALU.mult, None, [ok], [ok])
                    S.dma('sp', dst, o[:], reads=[ok], defer=True)
                for i_, slot0 in ((10, 6), (12, 8)):
                    for hp in range(2):
                        pf, pfk = fm_block(i_ + hp)
                        o, ok, dst = out_f(slot0 + hp)
                        self.cp('act' if hp else 'dve', o[:], pf[:], [pfk], [ok])
                        S.dma('sp', dst, o[:], reads=[ok], defer=True)
                for hp in range(2):
                    pf, pfk = fm_block(14 + hp)
                    o, ok, dst = out_f(10 + hp)
                    self.act(o[:], pf[:], AF.Silu, [pfk], [ok])
                    S.dma('sp', dst, o[:], reads=[ok], defer=True)
                for hp in range(2):
                    pf, pfk = fm_block(16 + hp)
                    o, ok, dst = out_f(12 + hp)
                    self.act(o[:], pf[:], AF.Sigmoid, [pfk], [ok], scale=-1.0)
                    self.ts('dve', o[:], o[:], lbc[:, 2 + hp:3 + hp], None, ALU.mult, None, [ok, 'lbc'], [ok])
                    S.dma('sp', dst, o[:], reads=[ok], defer=True)
                    o2, ok2, dst2 = out_f(14 + hp)
                    self.act(o2[:], pf[:], AF.Sigmoid, [pfk], [ok2])
                    self.ts('dve', o2[:], o2[:], lbc[:, 2 + hp:3 + hp], lbc[:, 4 + hp:5 + hp], ALU.mult, ALU.add,
                            [ok2, 'lbc'], [ok2])
                    self.act(o2[:], o2[:], AF.Ln, [ok2], [ok2])
                    S.dma('sp', dst2, o2[:], reads=[ok2], defer=True)
            S.dma('sp', self.iw_d, iw_all[:], reads=['iw_all'], defer=True)
            S.barrier()

    def build(self, phases=None):
        self.es = ExitStack()
        self.alloc_scratch()
        self.consts(self.es)
        self.S.barrier()
        x_src = self.x_in
        for l in range(self.depth):
            last = l == self.depth - 1
            def on(p):
                return phases is None or p in phases
            if on('p1'):
                self.phase1(l, x_src)
            if on('dsa'):
                self.phase_dsa(l)
            wh = tuple(b_ for b_, n_ in ((1, 'gla'), (3, 'hgrn')) if on(n_))
            if wh or on('lru'):
                self.phase_gla2(l, wh, on('lru'))
            if on('merge'):
                self.phase_merge(l, x_src)
            if on('ffn'):
                self.phase_ffn(l, self.out if last else self.xres_d, last)
            x_src = self.xres_d
        self.S.finish('sp')
        self.S.emit()
        self.es.close()
        return self.nc


def const_inputs():
    ident = np.eye(128, dtype=np.float32)
    j = np.arange(128)[:, None]
    i = np.arange(128)[None, :]
    cmask = (j <= i).astype(np.float32)
    bdmask = ((j <= i) & (j // 64 == i // 64)).astype(np.float32)
    scanm = np.ones((128, 128), np.float32)
    scanm[:, 0] = 0
    scanm[:, 64] = 0
    cneg = np.where(i <= j, 0.0, -1e30).astype(np.float32)
    return {'ident': ident, 'cmask': cmask, 'bdmask': bdmask, 'scanm': scanm, 'cneg': cneg}


def t5_bucket_np(dist):
    n = np.maximum(dist, 0)
    nf = np.maximum(n, 16).astype(np.float32)
    large = 16 + (np.log(nf / np.float32(16)) / np.float32(np.log(128 / 16)) * np.float32(16)).astype(np.int32)
    large = np.minimum(large, 31)
    return np.where(n < 16, n, large)


def bias_table(rel_bias):
    kk = np.arange(128)[:, None]
    qq = np.arange(128)[None, :]
    bp = t5_bucket_np(qq - kk + 128)
    bd = t5_bucket_np(qq - kk)
    idx = np.concatenate([bp, bd], axis=1)
    rb = np.asarray(rel_bias, np.float32)
    return np.ascontiguousarray(rb[idx].transpose(0, 2, 1))


def make_in_map(inp, b, depth=DEPTH):
    m = {'x': np.ascontiguousarray(inp['x'][b], dtype=np.float32)}
    for l in range(depth):
        for name in ('w_in', 'w_gate', 'w_uk', 'w_uv', 'w_gk2', 'w_rg_a', 'w_rg_x', 'w_br_a', 'w_br_b', 'w_br_c',
                     'w_br_d', 'w_out', 'w_ff1', 'w_ff2'):
            m[f"{name}{l}"] = np.ascontiguousarray(inp[name][l], dtype=np.float32)
        m[f"pvec{l}"] = pack_pvec(inp, l)
        m[f"bvec{l}"] = pack_bvec(inp, l)
    m['biasT'] = bias_table(inp['rel_bias'])
    m['relb'] = np.ascontiguousarray(inp['rel_bias'], dtype=np.float32)
    m.update(const_inputs())
    return m


def _phase_tools(self, es):
    nc = self.nc

    def sb(name, shape, dt):
        return es.enter_context(nc.sbuf_tensor(self.u(name), shape, dt))

    def pt(name, shape, dt):
        return es.enter_context(nc.psum_tensor(self.u(name), shape, dt))

    def ring(name, n, shape, dt, psum=False):
        tiles = []
        for i in range(n):
            nm = self.u(name)
            t = pt(nm, shape, dt) if psum else sb(nm, shape, dt)
            tiles.append((t, nm))
        r = Ring.__new__(Ring)
        r.tiles, r.i = tiles, 0
        return r
    return sb, pt, ring


NBIS = 14


def phase_dsa(self, l):
    nc, S, T, NT = self.nc, self.S, self.T, self.NT
    W = self.w[l]
    topk = min(256, T // 4)
    with ExitStack() as es:
        sb, pt, ring = _phase_tools(self, es)
        ckvT = sb('ckvT', [128, T], BF16)
        ckv1 = sb('ckv1', [128, NT, 129], BF16)
        ikT = sb('ikT', [128, T], BF16)
        iw = sb('iw', [128, NT, 4], F32)
        wuv = sb('wuv', [128, 4, 64], BF16)
        I4 = sb('I4', [128, 4, 128], BF16)
        cneg = sb('cneg', [128, 128], F32)
        bT = sb('bT', [128, 4, 256], F32)
        bhi = sb('bhi', [128, 4, 256], BF16)
        blo = sb('blo', [128, 4, 256], BF16)
        bhf = sb('bhf', [128, 4, 256], F32)
        b31 = sb('b31', [128, 4], F32)
        stage = ring('stg', 2, [128, 256], F32)
        S.dma('sp', ckvT[:], self.ckvT_d, writes=['ckvT'])
        S.dma('sp', ckv1[:], self.tmb_d[:, 0:129].rearrange("(t p) c -> p t c", p=128), writes=['ckv1'])
        S.dma('sp', ikT[:], self.fmb_d[:, 2, :], writes=['ikT'])
        S.dma('sp', iw[:], self.iw_d, writes=['iw'])
        S.dma('sp', cneg[:], self.cneg_in, writes=['cneg'])
        S.dma('sp', bT[:], self.biasT, writes=['bT'])
        S.dma('sp', b31[:], self.relb_in[31:32, :].partition_broadcast(128), writes=['b31'])
        self.load_cast(stage, wuv[:], 'wuv', W['w_uv'].rearrange("h c d -> c h d"), [4, 64])
        for h in range(4):
            self.cp('pool', I4[:, h, :], self.identb[:], ['identb'], ['I4'])
            self.ts('dve', bT[:, h, :], bT[:, h, :], b31[:, h:h + 1], None, ALU.subtract, None, ['bT', 'b31'], ['bT'])
        self.cp('dve', bhi[:], bT[:], ['bT'], ['bhi'])
        self.cp('dve', bhf[:], bhi[:], ['bhi'], ['bhf'])
        self.tt('dve', bhf[:], bT[:], bhf[:], ALU.subtract, ['bT', 'bhf'], ['bhf'])
        self.cp('dve', blo[:], bhf[:], ['bhf'], ['blo'])

        G = 2
        iqr = ring('iq', 3, [128, 2, 128], BF16)
        qlr = ring('ql', 3 * G, [128, 4, 128], BF16)
        scr = ring('sc', 2 * G, [128, T], F32)
        mnr = ring('mn', 2 * G, [128, T], BF16)
        junk = sb('junkb', [128, T], BF16)
        rlr = ring('rl', 3, [128, 512], F32)
        bs = ring('bs', 3, [128, 8, G], F32)
        pss = ring('pss', 2, [128, 512], F32, psum=True)
        psl = ring('psl', 2, [128, 512], F32, psum=True)
        poA = ring('poA', 1, [128, 2, 129], F32, psum=True)
        poB = ring('poB', 1, [128, 2, 129], F32, psum=True)
        ptr = ring('ptr', 1, [128, 4, 128], BF16, psum=True)
        pyr = ring('py', 1, [128, 2, 128], F32, psum=True)
        pr_ = ring('p', 3, [128, 512], BF16)
        olr = ring('ol', 2, [128, 4, 128], BF16)
        olTr = ring('olT', 2, [128, 4, 128], BF16)
        yr = ring('ya', 2, [128, 2, 128], BF16)
        rdr = ring('rd', 2, [128, 4], F32)
        NG = (NT + G - 1) // G
        info = {}

        def gen_scores(g):
            tiles = list(range(g * G, min(NT, g * G + G)))
            b, bk = bs.next()
            grp = []
            info[g] = (b, bk, grp)
            for gi, qi in enumerate(tiles):
                q0 = qi * 128
                nk = (qi + 1) * 128
                iq, iqk = iqr.next()
                ql, qlk = qlr.next()
                S.dma('sp', iq[:], self.fmb_d[:, 0:2, q0:q0 + 128], writes=[iqk])
                S.dma('sp', ql[:], self.fmb_d[:, 3:7, q0:q0 + 128], writes=[qlk])
                sc, sck = scr.next()
                mn, mnk = mnr.next()
                grp.append((qi, q0, nk, ql, qlk, sc, sck, mn, mnk))
                for k0 in range(0, nk, 512):
                    n = min(512, nk - k0)
                    for h in range(4):
                        hp, e_ = h // 2, h % 2
                        ps, psk = pss.next()
                        self.mm(ps[:, 0:n], iq[e_ * 64:(e_ + 1) * 64, hp, :], ikT[e_ * 64:(e_ + 1) * 64, k0:k0 + n],
                                True, True, [iqk, 'ikT'], [psk])
                        if h == 0:
                            self.ts('dve', sc[:, k0:k0 + n], ps[:, 0:n], 0.0, iw[:, qi, 0:1], ALU.max, ALU.mult,
                                    [psk, 'iw'], [sck])
                        else:
                            rl, rlk = rlr.next()
                            self.act(rl[:, 0:n], ps[:, 0:n], AF.Relu, [psk], [rlk])
                            self.stt(sc[:, k0:k0 + n], rl[:, 0:n], iw[:, qi, h:h + 1], sc[:, k0:k0 + n], ALU.mult, ALU.add,
                                     [rlk, 'iw', sck], [sck])
                    yield
                S.op('dve', lambda e, b=b, sc=sc, nk=nk, gi=gi: e.tensor_reduce(
                    out=b[:, 0, gi:gi + 1], in_=sc[:, 0:nk], axis=AX.X, op=ALU.min), [sck], [(bk, 'mn', gi)])
                S.op('dve', lambda e, b=b, sc=sc, nk=nk, gi=gi: e.tensor_reduce(
                    out=b[:, 1, gi:gi + 1], in_=sc[:, 0:nk], axis=AX.X, op=ALU.max), [sck], [(bk, 'mx', gi)])
                self.tt('dve', sc[:, q0:q0 + 128], sc[:, q0:q0 + 128], cneg[:], ALU.add, [sck, 'cneg'], [sck])
                yield

        def n_scores(g):
            return sum(((qi + 1) * 128 + 511) // 512 + 1 for qi in range(g * G, min(NT, g * G + G)))

        def gen_attend(g):
            for (qi, q0, nk, ql, qlk, sc, sck, mn, mnk) in info[g][2]:
                yield from self.dsa_attend(qi, q0, ql, qlk, mn, mnk, ckvT, ckv1, I4, bhi, blo, wuv,
                                           psl, poA, poB, ptr, pyr, pr_, olr, olTr, yr, rdr)

        def n_attend(g):
            return sum(qi + 2 for qi in range(g * G, min(NT, g * G + G)))

        def advance(gen, n):
            if gen is None:
                return None
            for _ in range(n):
                try:
                    next(gen)
                except StopIteration:
                    return None
            return gen

        advance(gen_scores(0), 10 ** 9)
        for g in range(NG):
            b, bk, grp = info[g]
            ng = len(grp)
            A = gen_scores(g + 1) if g + 1 < NG else None
            C = gen_attend(g - 1) if g >= 1 else None
            stepA = (n_scores(g + 1) + NBIS - 1) // NBIS if A is not None else 0
            stepC = (n_attend(g - 1) + NBIS - 1) // NBIS if C is not None else 0
            gs = slice(0, ng)
            kmm, kR, kC, kU, kT, kH = (bk, 'mm'), (bk, 'R'), (bk, 'cand'), (bk, 'u'), (bk, 'tmp'), (bk, 'thr')
            kin = [(bk, 'mn', gi) for gi in range(ng)] + [(bk, 'mx', gi) for gi in range(ng)]
            self.ts('dve', b[:, 0, gs], b[:, 0, gs], -1.0, None, ALU.add, None, kin, [kmm])
            self.tt('dve', b[:, 2, gs], b[:, 1, gs], b[:, 0, gs], ALU.subtract, kin + [kmm], [kR])
            self.stt(b[:, 3, gs], b[:, 2, gs], 0.5, b[:, 0, gs], ALU.mult, ALU.add, [kR, kmm], [kC])
            for it in range(NBIS):
                ci = 0.5 ** (it + 1)
                for gi, (qi, q0, nk, ql, qlk, sc, sck, mn, mnk) in enumerate(grp):
                    self.ts('dve', junk[:, 0:nk], sc[:, 0:nk], b[:, 3, gi:gi + 1], 0.0, ALU.is_ge, ALU.add,
                            [sck, kC], [(bk, 'cnt', gi)], accum=b[:, 4, gi:gi + 1])
                self.ts('dve', b[:, 5, gs], b[:, 4, gs], float(topk), -0.5, ALU.is_ge, ALU.add,
                        [(bk, 'cnt', gi) for gi in range(ng)], [kU])
                self.tt('dve', b[:, 6, gs], b[:, 5, gs], b[:, 2, gs], ALU.mult, [kU, kR], [kT])
                self.stt(b[:, 3, gs], b[:, 6, gs], ci, b[:, 3, gs], ALU.mult, ALU.add, [kT, kC], [kC])
                A = advance(A, stepA)
                C = advance(C, stepC)
            advance(A, 10 ** 9)
            advance(C, 10 ** 9)
            self.stt(b[:, 7, gs], b[:, 2, gs], -(0.5 ** (NBIS + 1)), b[:, 3, gs], ALU.mult, ALU.add, [kR, kC], [kH])
            for gi, (qi, q0, nk, ql, qlk, sc, sck, mn, mnk) in enumerate(grp):
                self.ts('dve', mn[:, 0:nk], sc[:, 0:nk], b[:, 7, gi:gi + 1], -30000.0, ALU.is_lt, ALU.mult,
                        [sck, kH], [mnk])
        advance(gen_attend(NG - 1), 10 ** 9)
        S.barrier()


def dsa_attend(self, qi, q0, ql, qlk, mn, mnk, ckvT, ckv1, I4, bhi, blo, wuv,
               psl, poA, poB, ptr, pyr, pr_, olr, olTr, yr, rdr):
    S = self.S
    if True:
        if True:
            pa, pak = poA.next()
            pb, pbk = poB.next()
            qlf = ql[:].rearrange("p h q -> p (h q)")
            for kb in range(qi + 1):
                pl, plk = psl.next()
                near = kb >= qi - 1
                self.mm(pl[:], ckvT[:, kb * 128:(kb + 1) * 128], qlf, True, False, ['ckvT', qlk], [plk])
                self.mm(pl[:], mn[:, kb * 128:(kb + 1) * 128], I4[:].rearrange("p h q -> p (h q)"), False, not near,
                        [mnk, 'I4'], [plk])
                if near:
                    o_ = 128 if kb == qi else 0
                    for bt_, bkey, last in ((bhi, 'bhi', False), (blo, 'blo', True)):
                        for h in range(4):
                            self.mm(pl[:, h * 128:(h + 1) * 128], self.identb[:], bt_[:, h, o_:o_ + 128], False,
                                    last and h == 3, ['identb', bkey], [plk])
                p, pk = pr_.next()
                self.act(p[:], pl[:], AF.Exp, [plk], [pk])
                for h in range(4):
                    po, pok = (pa, pak) if h < 2 else (pb, pbk)
                    self.mm(po[:, h % 2, :], p[:, h * 128:(h + 1) * 128], ckv1[:, kb, :], kb == 0 and h % 2 == 0, kb == qi,
                            [pk, 'ckv1'], [pok])
                yield
            rd, rdk = rdr.next()
            ol, olk = olr.next()
            for h in range(4):
                po, pok = (pa, pak) if h < 2 else (pb, pbk)
                S.op('dve', lambda e, rd=rd, po=po, h=h: e.reciprocal(out=rd[:, h:h + 1], in_=po[:, h % 2, 128:129]),
                     [pok], [rdk])
                self.ts('dve', ol[:, h, :], po[:, h % 2, 0:128], rd[:, h:h + 1], None, ALU.mult, None, [pok, rdk], [olk])
            ptt, pttk = ptr.next()
            for h in range(4):
                self.tr(ptt[:, h, :], ol[:, h, :], self.identb[:], [olk, 'identb'], [pttk])
            olT, olTk = olTr.next()
            self.cp('act', olT[:], ptt[:], [pttk], [olTk])
            py, pyk = pyr.next()
            for h in range(4):
                hp, e_ = h // 2, h % 2
                self.mm(py[e_ * 64:(e_ + 1) * 64, hp, :], wuv[:, h, :], olT[:, h, :], True, True, ['wuv', olTk], [pyk])
            ya, yak = yr.next()
            self.cp('act', ya[:], py[:], [pyk], [yak])
            S.dma('sp', self.yT_d[:, 0:2, q0:q0 + 128], ya[:], reads=[yak], defer=True)
            yield


Prog.dsa_attend = dsa_attend
Prog.phase_dsa = phase_dsa


def phase_gla(self, l, br, shared):
    nc, S, T, NT = self.nc, self.S, self.T, self.NT
    W = self.w[l]
    if br == 1:
        sq_, sk_, sg_, vcol, gcol, bvo = 0, 2, 4, 129, 0, BV_GLA
    else:
        sq_, sk_, sg_, vcol, gcol, bvo = 10, 12, 14, 385, 256, BV_HG
    with ExitStack() as es:
        sb, pt, ring = _phase_tools(self, es)
        scanm = sb('scanm', [128, 128], F32)
        bdm = sb('bdm', [128, 2, 128], F32)
        gnb = sb('gnb', [128, 256], F32)
        Sf = sb('Sf', [128, 2, 64], F32)
        S.dma('sp', scanm[:], self.scanm_in, writes=['scanm'])
        S.dma('sp', bdm[:, 0, :], self.bdmask_in, writes=['bdm'])
        S.dma('sp', bdm[:, 1, :], self.bdmask_in, writes=['bdm'])
        S.dma('sp', gnb[:], W['bvec'][:, bvo:bvo + 256].partition_broadcast(128), writes=['gnb'])
        S.op('pool', lambda e: e.memset(Sf[:], 0.0), [], ['Sf'])
        Sbr = ring('Sb', 4, [128, 2, 64], BF16)
        Sb, Sbk = Sbr.next()
        S.op('pool', lambda e, Sb=Sb: e.memset(Sb[:], 0.0), [], [Sbk])
        qr = ring('q', 2, [128, 2, 128], F32)
        kr = ring('k', 2, [128, 2, 128], F32)
        gr = ring('g', 2, [128, 2, 128], F32)
        vr = ring('v', 2, [128, 256], BF16)
        gtr = ring('gt', 2, [128, 256], F32)
        br_ = ring('b', 2, [128, 128], F32)
        bmr = ring('bm', 2, [128, 128], F32)
        e3r = ring('e3', 2, [128, 3, 128], F32)
        qer = ring('qe', 2, [128, 128], BF16)
        ker = ring('ke', 2, [128, 128], BF16)
        q1r = ring('q1', 2, [128, 128], BF16)
        ebr = ring('eb', 2, [128, 4], F32)
        atr = ring('at', 2, [128, 2, 128], BF16)
        ktr = ring('kt', 2, [128, 128], BF16)
        tmr = ring('tm', 2, [128, 64], F32)
        jk = sb('jk', [128, 256], F32)
        ssr = ring('ss', 2, [128, 12], F32)
        yr = ring('y', 2, [128, 256], F32)
        ybr = ring('yb', 2, [128, 256], BF16)
        yTr = ring('yT', 2, [128, 2, 128], BF16)
        patE, pkt, pdsC, porE, pyt = shared

        for ti in range(NT):
            t0 = ti * 128
            q, qk = qr.next()
            k, kk = kr.next()
            g, gk = gr.next()
            v, vk = vr.next()
            gt, gtk = gtr.next()
            S.dma('sp', g[:], self.fmf_d[:, sg_:sg_ + 2, t0:t0 + 128], writes=[gk])
            S.dma('sp', q[:], self.fmf_d[:, sq_:sq_ + 2, t0:t0 + 128], writes=[qk])
            S.dma('sp', k[:], self.fmf_d[:, sk_:sk_ + 2, t0:t0 + 128], writes=[kk])
            S.dma('sp', v[:], self.tmb_d[t0:t0 + 128, vcol:vcol + 256], writes=[vk])
            S.dma('sp', gt[:], self.tmf_d[t0:t0 + 128, gcol:gcol + 256], writes=[gtk])
            poE = [porE[0].next(), porE[1].next()]
            Sb0, Sb0k = Sb, Sbk
            Sb1, Sb1k = Sbr.next()
            Sb2, Sb2k = Sbr.next()
            for hp in range(2):
                b, bk = br_.next()
                bm, bmk = bmr.next()
                S.op('dve', lambda e, b=b, g=g, hp=hp: e.tensor_tensor_scan(
                    out=b[:], data0=scanm[:], data1=g[:, hp, :], initial=0.0, op0=ALU.mult, op1=ALU.add),
                    ['scanm', gk], [bk])
                for c in range(2):
                    self.ts('dve', bm[:, c * 64:(c + 1) * 64], b[:, c * 64:(c + 1) * 64], b[:, c * 64 + 31:c * 64 + 32],
                            None, ALU.subtract, None, [bk], [bmk])
                e3, e3k = e3r.next()
                self.act(e3[:, 0, :], bm[:], AF.Exp, [bmk], [e3k])
                self.act(e3[:, 1, :], bm[:], AF.Exp, [bmk], [e3k], scale=-1.0)
                self.act(e3[:, 2, :], b[:], AF.Exp, [bk], [e3k])
                eb, ebk = ebr.next()
                for c in range(2):
                    self.act(eb[:, c:c + 1], b[:, c * 64 + 63:c * 64 + 64], AF.Exp, [bk], [ebk])
                    self.act(eb[:, 2 + c:3 + c], bm[:, c * 64 + 63:c * 64 + 64], AF.Exp, [bmk], [ebk])
                qe, qek = qer.next()
                ke, kek = ker.next()
                q1, q1k = q1r.next()
                self.tt('dve', qe[:], q[:, hp, :], e3[:, 0, :], ALU.mult, [qk, e3k], [qek])
                self.tt('pool', ke[:], k[:, hp, :], e3[:, 1, :], ALU.mult, [kk, e3k], [kek])
                self.tt('pool', q1[:], q[:, hp, :], e3[:, 2, :], ALU.mult, [qk, e3k], [q1k])
                at, atk = atr.next()
                for e_ in range(2):
                    sl = slice(e_ * 64, (e_ + 1) * 64)
                    pa, pak = patE[e_].next()
                    self.mm(pa[:, 0:128], ke[sl, :], qe[sl, :], True, True, [kek, qek], [pak])
                    self.tt('dve', at[:, e_, :], pa[:, 0:128], bdm[:, 0, :], ALU.mult, [pak, 'bdm'], [(atk, e_)])
                pk_, pkk = pkt.next()
                self.tr(pk_[:, 0:128], ke[:], self.identb[:], [kek, 'identb'], [pkk])
                kt, ktk = ktr.next()
                self.cp('act', kt[:], pk_[:, 0:128], [pkk], [ktk])
                pdc = [pdsC[0].next(), pdsC[1].next()]
                for c in range(2):
                    cs = slice(c * 64, (c + 1) * 64)
                    pd, pdk = pdc[c]
                    for e_ in range(2):
                        sl = slice(e_ * 64, (e_ + 1) * 64)
                        h = hp * 2 + e_
                        self.mm(pd[sl, 0:64], kt[cs, sl], v[cs, h * 64:(h + 1) * 64], True, True, [ktk, vk], [pdk])
                for c, (Sn, Snk) in enumerate(((Sb1, Sb1k), (Sb2, Sb2k))):
                    tm, tmk = tmr.next()
                    pd, pdk = pdc[c]
                    self.ts('dve', tm[:], pd[:, 0:64], eb[:, 2 + c:3 + c], None, ALU.mult, None, [pdk, ebk], [tmk])
                    self.stt(Sf[:, hp, :], Sf[:, hp, :], eb[:, c:c + 1], tm[:], ALU.mult, ALU.add, ['Sf', ebk, tmk], ['Sf'])
                    self.cp('act', Sn[:, hp, :], Sf[:, hp, :], ['Sf'], [Snk])
                for e_ in range(2):
                    sl = slice(e_ * 64, (e_ + 1) * 64)
                    h = hp * 2 + e_
                    hs = slice(h * 64, (h + 1) * 64)
                    po, pok = poE[e_]
                    ps_ = slice(hp * 64, (hp + 1) * 64)
                    self.mm(po[:, ps_], at[:, e_, :], v[:, hs], True, False, [(atk, e_), vk], [pok])
                    self.mm(po[0:64, ps_], q1[sl, 0:64], Sb0[sl, hp, :], False, False, [q1k, Sb0k], [pok])
                    self.mm(po[64:128, ps_], q1[sl, 64:128], Sb1[sl, hp, :], False, True, [q1k, Sb1k], [pok])
            Sb, Sbk = Sb2, Sb2k
            ss, ssk = ssr.next()
            for e_ in range(2):
                self.act(jk[:, e_ * 128:(e_ + 1) * 128], poE[e_][0][:, 0:128], AF.Square, [poE[e_][1]], ['jk'])
            S.op('dve', lambda e, ss=ss: e.tensor_reduce(out=ss[:, 0:4], in_=jk[:].rearrange("p (h v) -> p h v", h=4),
                                                         axis=AX.X, op=ALU.add), ['jk'], [ssk])
            self.act(ss[:, 4:8], ss[:, 0:4], AF.Sqrt, [ssk], [ssk], bias=EPS, scale=1.0 / 64)
            S.op('dve', lambda e, ss=ss: e.reciprocal(out=ss[:, 8:12], in_=ss[:, 4:8]), [ssk], [ssk])
            y, yk = yr.next()
            for h in range(4):
                hs = slice(h * 64, (h + 1) * 64)
                hp, e_ = h // 2, h % 2
                si = 8 + e_ * 2 + hp
                self.stt(y[:, hs], poE[e_][0][:, hp * 64:(hp + 1) * 64], ss[:, si:si + 1], gnb[:, hs], ALU.mult, ALU.mult,
                         [poE[e_][1], ssk, 'gnb'], [yk])
            yb, ybk = ybr.next()
            self.tt('pool', yb[:], y[:], gt[:], ALU.mult, [yk, gtk], [ybk])
            py, pyk = pyt.next()
            for hp in range(2):
                self.tr(py[:, hp, :], yb[:, hp * 128:(hp + 1) * 128], self.identb[:], [ybk, 'identb'], [pyk])
            yT, yTk = yTr.next()
            self.cp('act', yT[:], py[:, 0:2, :], [pyk], [yTk])
            S.dma('sp', self.yT_d[:, br * 2:br * 2 + 2, t0:t0 + 128], yT[:], reads=[yTk], defer=True)
            yield
        yield


def phase_gla2(self, l, which=(1, 3), with_lru=True):
    S, NT, T = self.S, self.NT, self.T
    with ExitStack() as es:
        sb, pt, ring = _phase_tools(self, es)
        pkb = ring('pkb', 1, [128, 8, 128], BF16, psum=True)
        pkb_t, pkb_k = pkb.tiles[0]
        r_pkt = Ring.__new__(Ring)
        r_pkt.tiles, r_pkt.i = [(pkb_t[:, 4, :], pkb_k + '_kt')], 0
        r_pyt = Ring.__new__(Ring)
        r_pyt.tiles, r_pyt.i = [(pkb_t, pkb_k + '_yt')], 0
        shared = ([ring('pat', 1, [128, 512], F32, psum=True) for _ in range(2)],
                  r_pkt,
                  [ring('pds', 1, [128, 512], F32, psum=True) for _ in range(2)],
                  [ring('po', 1, [128, 512], F32, psum=True) for _ in range(2)],
                  r_pyt)
        plru = ring('plru', 1, [128, 512], F32, psum=True)
        gens = [self.phase_gla(l, br, shared) for br in which]
        L = self.phase_lru(l, plru) if with_lru else None
        nL = (T // 512) * 2
        doneL = 0
        for ti in range(NT):
            for g_ in gens:
                next(g_)
            if L is not None:
                want = ((ti + 1) * nL) // NT
                while doneL < want:
                    next(L)
                    doneL += 1
        for g_ in gens:
            next(g_)
        if L is not None:
            while doneL < nL:
                next(L)
                doneL += 1
            next(L)
            for _ in L:
                pass
        for g_ in reversed(gens):
            for _ in g_:
                pass
        S.barrier()


Prog.phase_gla2 = phase_gla2
Prog.phase_gla = phase_gla


def phase_lru(self, l, psr):
    nc, S, T = self.nc, self.S, self.T
    W = self.w[l]
    with ExitStack() as es:
        sb, pt, ring = _phase_tools(self, es)
        pv = sb('pv', [128, NPV], F32)
        cc_ = sb('cc', [128, 8], F32)
        wa = sb('wa', [128, 2, 128], BF16)
        wx = sb('wx', [128, 2, 128], BF16)
        stage = ring('stg', 2, [128, 64], F32)
        S.dma('sp', pv[:], W['pvec'], writes=['pv'])
        self.act(cc_[:, 0:2], pv[:, 20:22], AF.Exp, ['pv'], ['cc'], scale=-1.0)
        self.act(cc_[:, 0:2], cc_[:, 0:2], AF.Ln, ['cc'], ['cc'], bias=1.0)
        self.ts('dve', cc_[:, 2:4], cc_[:, 0:2], -8.0, None, ALU.mult, None, ['cc'], ['cc'])
        self.ts('dve', cc_[:, 4:6], cc_[:, 0:2], -16.0, None, ALU.mult, None, ['cc'], ['cc'])
        S.op('pool', lambda e: e.memset(wa[:], 0.0), [], ['wa'])
        S.op('pool', lambda e: e.memset(wx[:], 0.0), [], ['wx'])
        for n in range(4):
            cc, e_ = n // 2, n % 2
            sl = slice(e_ * 64, (e_ + 1) * 64)
            self.load_cast(stage, wa[sl, cc, e_ * 64:(e_ + 1) * 64], 'wa', W['w_rg_a'][n], [64], pbase=e_ * 64)
            self.load_cast(stage, wx[sl, cc, e_ * 64:(e_ + 1) * 64], 'wx', W['w_rg_x'][n], [64], pbase=e_ * 64)
        LV = int(os.environ.get('DBG_LRU', '9'))
        cxr = [ring('cx', 2, [128, 515], F32) for _ in range(2)]
        hhr = [ring('hh', 2, [128, 512], F32) for _ in range(2)]
        cyr = ring('cy', 2, [128, 512], F32)
        xcr = ring('xc', 2, [128, 512], F32)
        xbr = ring('xb', 2, [128, 512], BF16)
        rr = ring('r', 2, [128, 512], F32)
        ir = ring('i', 2, [128, 512], F32)
        ar = ring('a', 2, [128, 512], F32)
        a2r = ring('a2', 2, [128, 512], F32)
        ur = ring('u', 2, [128, 512], F32)
        gr = ring('gg', 2, [128, 512], F32)
        g2r = ring('g2', 2, [128, 512], F32)
        ycr = ring('yc', 2, [128, 512], BF16)
        prev = [None, None]
        prevh = [None, None]
        for bi in range(T // 512 if LV > 0 else 0):
            tok0 = bi * 512
            for cc in range(2):
                cx, cxk = cxr[cc].next()
                cy, cyk = cyr.next()
                S.dma('sp', cx[:, 3:515], self.fmf_d[:, 6 + cc, tok0:tok0 + 512], writes=[cxk])
                S.dma('sp', cy[:], self.fmf_d[:, 8 + cc, tok0:tok0 + 512], writes=[cyk])
                if prev[cc] is None:
                    S.op('pool', lambda e, cx=cx: e.memset(cx[:, 0:3], 0.0), [], [cxk])
                else:
                    self.cp('pool', cx[:, 0:3], prev[cc][0][:, 512:515], [prev[cc][1]], [cxk])
                prev[cc] = (cx, cxk)
                xc, xck = xcr.next()
                self.ts('dve', xc[:], cx[:, 3:515], pv[:, 14 + cc:15 + cc], pv[:, 6 + cc:7 + cc], ALU.mult, ALU.add,
                        [cxk, 'pv'], [xck])
                for j in range(3):
                    self.stt(xc[:], cx[:, j:j + 512], pv[:, 8 + 2 * j + cc:9 + 2 * j + cc], xc[:], ALU.mult, ALU.add,
                             [cxk, 'pv', xck], [xck])
                if LV < 2:
                    continue
                xb, xbk = xbr.next()
                self.cp('act', xb[:], xc[:], [xck], [xbk])
                r, rk = rr.next()
                i_, ik = ir.next()
                p1, p1k = psr.next()
                self.mm(p1[:], wa[:, cc, :], xb[:], True, True, ['wa', xbk], [p1k])
                self.act(r[:], p1[:], AF.Sigmoid, [p1k, 'pv'], [rk], bias=pv[:, 16 + cc:17 + cc])
                p2, p2k = psr.next()
                self.mm(p2[:], wx[:, cc, :], xb[:], True, True, ['wx', xbk], [p2k])
                self.act(i_[:], p2[:], AF.Sigmoid, [p2k, 'pv'], [ik], bias=pv[:, 18 + cc:19 + cc])
                a, ak = ar.next()
                a2, a2k = a2r.next()
                self.act(a[:], r[:], AF.Exp, [rk, 'cc'], [ak], scale=cc_[:, 2 + cc:3 + cc])
                self.act(a2[:], r[:], AF.Exp, [rk, 'cc'], [a2k], scale=cc_[:, 4 + cc:5 + cc])
                self.act(a2[:], a2[:], AF.Sqrt, [a2k], [a2k], bias=1.0, scale=-1.0)
                u, uk = ur.next()
                self.tt('dve', u[:], a2[:], i_[:], ALU.mult, [a2k, ik], [uk])
                self.tt('pool', u[:], u[:], xc[:], ALU.mult, [uk, xck], [uk])
                if LV < 3:
                    continue
                hh, hhk = hhr[cc].next()
                init = 0.0 if prevh[cc] is None else prevh[cc][0][:, 511:512]
                rd = [ak, uk] + ([] if prevh[cc] is None else [prevh[cc][1]])
                S.op('dve', lambda e, hh=hh, a=a, u=u, init=init: e.tensor_tensor_scan(
                    out=hh[:], data0=a[:], data1=u[:], initial=init, op0=ALU.mult, op1=ALU.add), rd, [hhk])
                prevh[cc] = (hh, hhk)
                if LV < 4:
                    continue
                g, gk = gr.next()
                g2, g2k = g2r.next()
                self.tt('pool', g[:], cy[:], cy[:], ALU.mult, [cyk], [gk])
                self.ts('dve', g[:], g[:], 0.044715, 1.0, ALU.mult, ALU.add, [gk], [gk])
                self.tt('pool', g[:], g[:], cy[:], ALU.mult, [gk, cyk], [gk])
                self.act(g2[:], g[:], AF.Sigmoid, [gk], [g2k], scale=1.5957691216057308)
                self.tt('dve', g2[:], g2[:], cy[:], ALU.mult, [g2k, cyk], [g2k])
                yc, yck = ycr.next()
                self.tt('pool', yc[:], g2[:], hh[:], ALU.mult, [g2k, hhk], [yck])
                S.dma('sp', self.yT_d[:, 4 + cc, tok0:tok0 + 512], yc[:], reads=[yck], defer=True)
                yield
        yield


Prog.phase_lru = phase_lru


def phase_merge(self, l, x_src):
    nc, S, T, NT = self.nc, self.S, self.T, self.NT
    W = self.w[l]
    with ExitStack() as es:
        sb, pt, ring = _phase_tools(self, es)
        wg = sb('wg', [128, 8, 4096], BF16)
        wbr = sb('wbr', [128, 8, 1024], BF16)
        wo = sb('wo', [128, 8, 1024], BF16)
        bgb = sb('bgb', [128, 4096], F32)
        stage = ring('stg', 2, [128, 2048], F32)
        S.dma('sp', bgb[:], W['bvec'][:, BV_BG:BV_BG + 4096].partition_broadcast(128), writes=['bgb'])
        for k in range(8):
            for c0 in (0, 2048):
                self.load_cast(stage, wg[:, k, c0:c0 + 2048], ('wg', k), W['w_gate'][k * 128:(k + 1) * 128, c0:c0 + 2048], [2048])
            self.load_cast(stage, wo[:, k, :], ('wo', k), W['w_out'][k * 128:(k + 1) * 128, :], [1024])
        for b_, nm in enumerate(('w_br_a', 'w_br_b', 'w_br_c', 'w_br_d')):
            for kk in range(2):
                self.load_cast(stage, wbr[:, b_ * 2 + kk, :], ('wbr', b_), W[nm][kk * 128:(kk + 1) * 128, :], [1024])
        hr = ring('hT', 2, [128, 8, 128], BF16)
        yr = ring('yT', 2, [128, 8, 128], BF16)
        xr = ring('x', 3, [128, 1024], F32)
        mr = ring('m', 2, [128, 1024], F32)
        mbr = ring('mb', 3, [128, 1024], BF16)
        mTr = ring('mT', 2, [128, 8, 128], BF16)
        gsr = ring('gs', 3, [128, 512], F32)
        tpr = ring('tp', 2, [128, 512], F32)
        xor_ = ring('xo', 2, [128, 1024], F32)
        pgr = ring('pg', 3, [128, 512], F32, psum=True)
        pyr = ring('py', 2, [128, 512], F32, psum=True)
        ptr = ring('pt', 1, [128, 8, 128], BF16, psum=True)
        def stage_a(ti):
            t0 = ti * 128
            hT, hTk = hr.next()
            yT, yTk = yr.next()
            x, xk = xr.next()
            S.dma('sp', hT[:], self.hT_d[:, :, t0:t0 + 128], writes=[hTk])
            S.dma('sp', yT[:], self.yT_d[:, :, t0:t0 + 128], writes=[yTk])
            S.dma('sp', x[:], x_src[t0:t0 + 128, :], writes=[xk])
            m, mk = mr.next()
            for b_ in range(4):
                for cb in range(2):
                    c0 = b_ * 1024 + cb * 512
                    pg, pgk = pgr.next()
                    for k in range(8):
                        self.mm(pg[:], hT[:, k, :], wg[:, k, c0:c0 + 512], k == 0, k == 7, [hTk, ('wg', k)], [pgk])
                    py, pyk = pyr.next()
                    for kk in range(2):
                        self.mm(py[:], yT[:, b_ * 2 + kk, :], wbr[:, b_ * 2 + kk, cb * 512:(cb + 1) * 512], kk == 0, kk == 1,
                                [yTk, ('wbr', b_)], [pyk])
                    gs, gsk = gsr.next()
                    self.tt('dve', gs[:], pg[:], bgb[:, c0:c0 + 512], ALU.add, [pgk, 'bgb'], [gsk])
                    self.act(gs[:], gs[:], AF.Sigmoid, [gsk], [gsk])
                    ms = m[:, cb * 512:(cb + 1) * 512]
                    if b_ == 0:
                        self.tt('dve', ms, gs[:], py[:], ALU.mult, [gsk, pyk], [(mk, cb)])
                    else:
                        tp, tpk = tpr.next()
                        self.tt('dve', tp[:], gs[:], py[:], ALU.mult, [gsk, pyk], [tpk])
                        self.tt('pool', ms, ms, tp[:], ALU.add, [(mk, cb), tpk], [(mk, cb)])
            mb, mbk = mbr.next()
            self.cp('act', mb[:], m[:], [(mk, 0), (mk, 1)], [mbk])
            return (t0, x, xk, mb, mbk)

        def stage_b(st):
            t0, x, xk, mb, mbk = st
            p, pk = ptr.next()
            for k in range(8):
                self.tr(p[:, k, :], mb[:, k * 128:(k + 1) * 128], self.identb[:], [mbk, 'identb'], [pk])
            mT, mTk = mTr.next()
            self.cp('act', mT[:], p[:], [pk], [mTk])
            xo, xok = xor_.next()
            for cb in range(2):
                po, pok = pgr.next()
                for k in range(8):
                    self.mm(po[:], mT[:, k, :], wo[:, k, cb * 512:(cb + 1) * 512], k == 0, k == 7, [mTk, ('wo', k)], [pok])
                self.tt('dve', xo[:, cb * 512:(cb + 1) * 512], x[:, cb * 512:(cb + 1) * 512], po[:], ALU.add,
                        [xk, pok], [xok])
            S.dma('sp', self.xmid_d[t0:t0 + 128, :], xo[:], reads=[xok], defer=True)

        prev = None
        for ti in range(NT):
            cur = stage_a(ti)
            if prev is not None:
                stage_b(prev)
            prev = cur
        stage_b(prev)
        S.barrier()


Prog.phase_merge = phase_merge


def phase_ffn(self, l, x_dst, final):
    nc, S, T = self.nc, self.S, self.T
    W = self.w[l]
    NS = 256
    with ExitStack() as es:
        sb, pt, ring = _phase_tools(self, es)
        wf1 = sb('wf1', [128, 8, 4096], BF16)
        wf2 = sb('wf2', [128, 32, 1024], BF16)
        g2b = sb('g2b', [128, 1024], F32)
        gfb = sb('gfb', [128, 1024], F32)
        stage = ring('stg', 2, [128, 1024], F32)
        S.dma('sp', g2b[:], W['bvec'][:, BV_N2:BV_N2 + 1024].partition_broadcast(128), writes=['g2b'])
        S.dma('sp', gfb[:], W['bvec'][:, BV_FN:BV_FN + 1024].partition_broadcast(128), writes=['gfb'])
        for k in range(8):
            for c0 in range(0, 4096, 1024):
                self.load_cast(stage, wf1[:, k, c0:c0 + 1024], ('wf1', k), W['w_ff1'][k * 128:(k + 1) * 128, c0:c0 + 1024], [1024])
        for f in range(32):
            self.load_cast(stage, wf2[:, f, :], ('wf2', f), W['w_ff2'][f * 128:(f + 1) * 128, :], [1024])
        xr = ring('xm', 4, [128, 1024], F32)
        junk = sb('junk', [128, 1024], F32)
        hr = ring('h2', 2, [128, 1024], BF16)
        hTr = ring('h2T', 2, [128, 8, NS], BF16)
        uTr = ring('uT', 1, [128, 32, NS], BF16)
        sqr = ring('sq', 2, [128, NS], F32)
        st1 = ring('st', 4, [128, 4], F32)
        xor_ = ring('xo', 2, [128, 1024], F32)
        ptr = ring('pt', 2, [128, 8, 128], BF16, psum=True)
        pur = ring('pu', 3, [128, 512], F32, psum=True)
        por = ring('po', 3, [128, 512], F32, psum=True)
        def stage_a(si):
            tok0 = si * NS
            hT, hTk = hTr.next()
            xs = []
            for j in range(NS // 128):
                t0 = tok0 + j * 128
                x, xk = xr.next()
                xs.append((x, xk))
                S.dma('sp', x[:], self.xmid_d[t0:t0 + 128, :], writes=[xk])
                s1, s1k = st1.next()
                self.act(junk[:], x[:], AF.Square, [xk], ['junk', s1k], accum=s1[:, 0:1])
                self.act(s1[:, 1:2], s1[:, 0:1], AF.Sqrt, [s1k], [s1k], bias=EPS, scale=1.0 / 1024)
                S.op('dve', lambda e, s1=s1: e.reciprocal(out=s1[:, 2:3], in_=s1[:, 1:2]), [s1k], [s1k])
                h, hk = hr.next()
                self.stt(h[:], x[:], s1[:, 2:3], g2b[:], ALU.mult, ALU.mult, [xk, s1k, 'g2b'], [hk])
                p, pk = ptr.next()
                for k in range(8):
                    self.tr(p[:, k, :], h[:, k * 128:(k + 1) * 128], self.identb[:], [hk, 'identb'], [pk])
                self.cp('act', hT[:, :, j * 128:(j + 1) * 128], p[:], [pk], [hTk])
            return (tok0, hT, hTk, xs)

        def ff1(st):
            tok0, hT, hTk, xs = st
            uT, uTk = uTr.next()
            for f in range(32):
                pu, puk = pur.next()
                for k in range(8):
                    self.mm(pu[:, 0:NS], wf1[:, k, f * 128:(f + 1) * 128], hT[:, k, :], k == 0, k == 7,
                            [hTk, ('wf1', k)], [puk])
                sq, sqk = sqr.next()
                self.act(sq[:], pu[:, 0:NS], AF.Square, [puk], [sqk])
                self.stt(uT[:, f, :], pu[:, 0:NS], 0.0, sq[:], ALU.is_gt, ALU.mult, [puk, sqk], [(uTk, f)])
            return uT, uTk

        def ff2(st, uT, uTk):
            tok0, hT, hTk, xs = st
            for j in range(NS // 128):
                t0 = tok0 + j * 128
                x, xk = xs[j]
                xo, xok = xor_.next()
                for cb in range(2):
                    po, pok = por.next()
                    for f in range(32):
                        self.mm(po[:], uT[:, f, j * 128:(j + 1) * 128], wf2[:, f, cb * 512:(cb + 1) * 512], f == 0, f == 31,
                                [(uTk, f), ('wf2', f)], [pok])
                    self.tt('dve', xo[:, cb * 512:(cb + 1) * 512], x[:, cb * 512:(cb + 1) * 512], po[:], ALU.add,
                            [xk, pok], [xok])
                if final:
                    s1, s1k = st1.next()
                    self.act(junk[:], xo[:], AF.Square, [xok], ['junk', s1k], accum=s1[:, 0:1])
                    self.act(s1[:, 1:2], s1[:, 0:1], AF.Sqrt, [s1k], [s1k], bias=EPS, scale=1.0 / 1024)
                    S.op('dve', lambda e, s1=s1: e.reciprocal(out=s1[:, 2:3], in_=s1[:, 1:2]), [s1k], [s1k])
                    self.stt(xo[:], xo[:], s1[:, 2:3], gfb[:], ALU.mult, ALU.mult, [xok, s1k, 'gfb'], [xok])
                S.dma('sp', x_dst[t0:t0 + 128, :], xo[:], reads=[xok], defer=True)

        nS = T // NS
        cur = stage_a(0)
        for si in range(nS):
            uT, uTk = ff1(cur)
            nxt = stage_a(si + 1) if si + 1 < nS else None
            ff2(cur, uT, uTk)
            cur = nxt
        S.barrier()


Prog.phase_ffn = phase_ffn


_CACHE = {}


def kernel(**inputs):
    inp = {k: np.asarray(v) for k, v in inputs.items()}
    B, T, D = inp['x'].shape
    P = Prog(T, depth=DEPTH)
    nc = P.build()
    in_maps = [make_in_map(inp, b) for b in range(B)]
    res = run_bass_kernel_spmd(nc, in_maps, core_ids=list(range(B)))
    out = np.stack([np.asarray(r['out'], dtype=np.float32) for r in res.results], axis=0)
    return out
```

```python
import numpy as np
import concourse.bass as bass
import concourse.mybir as mybir
import os
from contextlib import ExitStack
from concourse.bass_utils import run_bass_kernel_spmd

F32 = mybir.dt.float32
BF16 = mybir.dt.bfloat16
AF = mybir.ActivationFunctionType
ALU = mybir.AluOpType
AX = mybir.AxisListType

D_MODEL = 1024
DEPTH = 2
EPS = 1e-6
D_FF = 4096
IN_COLS = 3284
O_AQ, O_CKV, O_IQ, O_IK, O_IW = 0, 256, 384, 640, 704
O_BQ, O_BK, O_BV, O_BLR, O_BR = 708, 964, 1220, 1476, 1492
O_CX, O_CY = 1748, 2004
O_DQ, O_DF, O_DI, O_DG = 2260, 2516, 2772, 3028


class Sched:
    ENG = ('pe', 'dve', 'act', 'pool', 'sp')
    LOOKBACK = 3
    SEM_MAX = 30000

    def __init__(self, nc):
        self.nc = nc
        self.q = {e: [] for e in self.ENG}
        self.sem = {}
        self.cnt = {}
        self.old = {}
        self.all_sems = []
        self.nsem = 0
        for e in self.ENG:
            if e != 'sp':
                self._new_sem(e)
        self.known = {e: {} for e in self.ENG}
        self.res = {}
        self.dma_ring = []
        self.dma_i = 0
        self.NDMA = 40
        self.pend = []
        self.pend_sems = set()
        self.pend_age = 0
        self.DEFER_LOADS = 3
        self.n_ops = 0

    def _alloc(self, name):
        self.nsem += 1
        h = self.nc.alloc_semaphore(f"{name}_{self.nsem}")
        self.all_sems.append(h)
        return h

    def _new_sem(self, e):
        if e in self.sem:
            self.old.setdefault(e, []).append((self.sem[e], self.cnt[e]))
        self.sem[e] = self._alloc('s' + e)
        self.cnt[e] = 0

    def _need(self, eng, ev, waits, use_known=True):
        if ev is None:
            return
        sem, val, src = ev
        if sem.name in self.pend_sems:
            self.flush()
        if src == eng:
            if eng == 'pe':
                return
            if sem is self.sem[eng] and val <= self.cnt[eng] - self.LOOKBACK:
                return
            if sem is not self.sem[eng]:
                return
        k = self.known[eng]
        if use_known and k.get(sem.name, 0) >= val:
            return
        cur = waits.get(sem.name)
        if cur is None or cur[1] < val:
            waits[sem.name] = (sem, val)

    def _deps(self, eng, reads, writes, use_known=True):
        waits = {}
        for r in reads:
            st = self.res.get(r)
            if st is not None:
                self._need(eng, st[0], waits, use_known)
        for w in writes:
            st = self.res.get(w)
            if st is not None:
                self._need(eng, st[0], waits, use_known)
                for ev in st[1].values():
                    self._need(eng, ev, waits, use_known)
        if use_known:
            for name, (sem, val) in waits.items():
                self.known[eng][name] = val
        return list(waits.values())

    def _commit(self, eng, ev, reads, writes):
        for r in reads:
            st = self.res.setdefault(r, [None, {}])
            st[1][eng if ev[2] != 'dma' else ('dma', ev[0].name)] = ev
        for w in writes:
            self.res[w] = [ev, {}]

    def op(self, eng, fn, reads=(), writes=()):
        waits = self._deps(eng, reads, writes)
        if self.cnt[eng] >= self.SEM_MAX:
            self._new_sem(eng)
        sem = self.sem[eng]
        self.cnt[eng] += 1
        ev = (sem, self.cnt[eng], eng)
        self._commit(eng, ev, reads, writes)
        self.q[eng].append((waits, fn, sem, 1))
        self.n_ops += 1

    def flush(self):
        k = self.known['sp']
        for (waits, fn, sem, inc) in self.pend:
            w2 = []
            for (s_, v_) in waits:
                if k.get(s_.name, 0) < v_:
                    k[s_.name] = v_
                    w2.append((s_, v_))
            self.q['sp'].append((w2, fn, sem, inc))
        self.pend = []
        self.pend_sems = set()
        self.pend_age = 0

    def dma(self, eng, out, in_, reads=(), writes=(), defer=False):
        eng = 'sp'
        if len(self.dma_ring) < self.NDMA:
            self.dma_ring.append([self._alloc('d'), 0, None])
            slot = self.dma_ring[-1]
        else:
            slot = self.dma_ring[self.dma_i % self.NDMA]
        self.dma_i += 1
        if slot[0].name in self.pend_sems:
            self.flush()
        waits = self._deps(eng, reads, writes, use_known=not defer)
        if slot[2] is not None:
            w2 = {}
            self._need(eng, slot[2], w2, use_known=not defer)
            for name, (sem, val) in w2.items():
                if not defer:
                    self.known[eng][name] = val
                waits = [w for w in waits if w[0].name != name] + [(sem, val)]
        slot[1] += 16
        ev = (slot[0], slot[1], 'dma')
        slot[2] = ev
        self._commit(eng, ev, reads, writes)
        ent = (waits, lambda e, o=out, i=in_: e.dma_start(out=o, in_=i), slot[0], 16)
        if defer:
            self.pend.append(ent)
            self.pend_sems.add(slot[0].name)
        else:
            self.q[eng].append(ent)
            if self.pend:
                self.pend_age += 1
                if self.pend_age >= self.DEFER_LOADS:
                    self.flush()
        self.n_ops += 1

    def barrier(self):
        self.flush()
        for e in self.ENG:
            waits = []
            k = self.known[e]
            for f in self.ENG:
                if f == 'sp' or (f == e and e == 'pe'):
                    continue
                for (sem, c) in self.old.get(f, []) + [(self.sem[f], self.cnt[f])]:
                    if c > 0 and k.get(sem.name, 0) < c:
                        waits.append((sem, c))
                        k[sem.name] = c
            for slot in self.dma_ring:
                if slot[2] is not None and k.get(slot[0].name, 0) < slot[1]:
                    waits.append((slot[0], slot[1]))
                    k[slot[0].name] = slot[1]
            self.q[e].append((waits, None, None, 0))
        self.res = {}

    def finish(self, eng='sp'):
        self.flush()
        waits = {}
        for slot in self.dma_ring:
            if slot[2] is not None:
                self._need(eng, slot[2], waits)
        self.q[eng].append((list(waits.values()), None, None, 0))

    def emit(self):
        nc = self.nc
        q = self.q

        def run(e, lst):
            for waits, fn, sem, inc in lst:
                for (s, v) in waits:
                    e.wait_ge(s, v)
                if fn is not None:
                    ins = fn(e)
                    ins.then_inc(sem, inc)

        for h in self.all_sems:
            nc.sync.sem_clear(h)
        with nc.Block() as block:
            @block.tensor
            def _(e):
                run(e, q['pe'])

            @block.vector
            def _(e):
                run(e, q['dve'])

            @block.scalar
            def _(e):
                run(e, q['act'])

            @block.gpsimd
            def _(e):
                run(e, q['pool'])

            @block.sync
            def _(e):
                run(e, q['sp'])


class Ring:
    def __init__(self, nc, name, n, shape, dtype, psum=False):
        self.tiles = []
        for i in range(n):
            if psum:
                t = nc.alloc_psum_tensor(f"{name}{i}", shape, dtype)
            else:
                t = nc.alloc_sbuf_tensor(f"{name}{i}", shape, dtype)
            self.tiles.append((t, f"{name}{i}"))
        self.i = 0

    def next(self):
        t = self.tiles[self.i % len(self.tiles)]
        self.i += 1
        return t


NPV = 24


def pack_pvec(inp, l):
    def c2(v):
        return np.ascontiguousarray(np.asarray(v, np.float32).reshape(2, 128).T)
    cols = [c2(inp['b_gk'][l]), c2(inp['lb_param'][0]), c2(inp['lb_param'][1]), c2(inp['conv_b'][l])]
    for j in range(4):
        cols.append(c2(inp['conv_w'][l][j]))
    cols += [c2(inp['b_rg_a'][l]), c2(inp['b_rg_x'][l]), c2(inp['lru_lambda'][l])]
    pv = np.concatenate(cols, axis=1)
    out = np.zeros((128, NPV), np.float32)
    out[:, :pv.shape[1]] = pv
    return out


BV_N1, BV_KV, BV_GLA, BV_HG, BV_BG, BV_N2, BV_FN = 0, 1024, 1152, 1408, 1664, 5760, 6784
NBV = 7808


def pack_bvec(inp, l):
    v = np.concatenate([
        inp['norm1_g'][l], inp['kv_norm_g'][l], np.tile(inp['gla_norm_g'][l], 4),
        np.tile(inp['hgrn_norm_g'][l], 4), inp['b_gate'][l], inp['norm2_g'][l], inp['final_norm_g']
    ]).astype(np.float32)
    assert v.shape[0] == NBV
    return v.reshape(1, NBV)


class Prog:
    def __init__(self, T, depth=DEPTH, debug=()):
        self.T = T
        self.NT = T // 128
        self.depth = depth
        self.debug = set(debug)
        nc = self.nc = bass.Bass("TRN2", target_bir_lowering=False)
        self.S = Sched(nc)
        self.uid = 0
        dt = nc.dram_tensor
        self.x_in = dt("x", [T, D_MODEL], F32, kind="ExternalInput").ap()
        self.out = dt("out", [T, D_MODEL], F32, kind="ExternalOutput").ap()
        self.w = []
        for l in range(depth):
            d = {}
            for name, shape in [('w_in', [D_MODEL, IN_COLS]), ('w_gate', [D_MODEL, 4096]),
                                ('w_uk', [4, 64, 128]), ('w_uv', [4, 128, 64]), ('w_gk2', [16, 256]),
                                ('w_rg_a', [4, 64, 64]), ('w_rg_x', [4, 64, 64]),
                                ('w_br_a', [256, 1024]), ('w_br_b', [256, 1024]), ('w_br_c', [256, 1024]),
                                ('w_br_d', [256, 1024]), ('w_out', [1024, 1024]), ('w_ff1', [1024, 4096]),
                                ('w_ff2', [4096, 1024]), ('pvec', [128, NPV]), ('bvec', [1, NBV])]:
                d[name] = dt(f"{name}{l}", shape, F32, kind="ExternalInput").ap()
            self.w.append(d)
        self.biasT = dt("biasT", [128, 4, 256], F32, kind="ExternalInput").ap()
        self.scr = {}

    def scratch(self, name, shape, dtype):
        kind = "ExternalOutput" if name in self.debug else "Internal"
        t = self.nc.dram_tensor(name, shape, dtype, kind=kind).ap()
        self.scr[name] = t
        return t

    def u(self, p):
        self.uid += 1
        return f"{p}{self.uid}"

    def act(self, out, in_, func, reads, writes, bias=0.0, scale=1.0, accum=None):
        kw = {}
        if accum is not None:
            kw['accum_out'] = accum
        self.S.op('act', lambda e: e.activation(out=out, in_=in_, func=func, bias=bias, scale=scale, **kw),
                  reads, writes)

    def mm(self, out, lhsT, rhs, start, stop, reads, writes):
        self.S.op('pe', lambda e: e.matmul(out, lhsT, rhs, start=start, stop=stop), reads, writes)

    def tr(self, out, in_, ident, reads, writes):
        self.S.op('pe', lambda e: e.transpose(out, in_, ident), reads, writes)

    def tt(self, eng, out, in0, in1, op, reads, writes):
        self.S.op(eng, lambda e: e.tensor_tensor(out=out, in0=in0, in1=in1, op=op), reads, writes)

    def ts(self, eng, out, in0, s1, s2, op0, op1, reads, writes, accum=None):
        if op1 is None:
            self.S.op(eng, lambda e: e.tensor_scalar(out=out, in0=in0, scalar1=s1, scalar2=None, op0=op0),
                      reads, writes)
        elif accum is not None:
            self.S.op(eng, lambda e: e.tensor_scalar(out=out, in0=in0, scalar1=s1, scalar2=s2, op0=op0, op1=op1,
                                                     accum_out=accum), reads, writes)
        else:
            self.S.op(eng, lambda e: e.tensor_scalar(out=out, in0=in0, scalar1=s1, scalar2=s2, op0=op0, op1=op1),
                      reads, writes)

    def stt(self, out, in0, scalar, in1, op0, op1, reads, writes):
        self.S.op('dve', lambda e: e.scalar_tensor_tensor(out=out, in0=in0, scalar=scalar, in1=in1, op0=op0, op1=op1),
                  reads, writes)

    def cp(self, eng, out, in_, reads, writes):
        if eng == 'act':
            self.S.op('act', lambda e: e.copy(out=out, in_=in_), reads, writes)
        else:
            self.S.op(eng, lambda e: e.tensor_copy(out=out, in_=in_), reads, writes)

    def load_cast(self, ring, dst, dkey, src, shape, engs=('pool', 'dve'), pbase=0):
        st, sk = ring.next()
        if len(shape) == 1:
            sv = st[:, 0:shape[0]]
        else:
            n = shape[0] * shape[1]
            sv = st[:, 0:n].rearrange("p (a b) -> p a b", a=shape[0])
        np_ = dst.shape[0]
        self.S.dma('sp', sv[pbase:pbase + np_], src, writes=[sk])
        self.cast_i = getattr(self, 'cast_i', 0) + 1
        self.cp(engs[self.cast_i % len(engs)], dst, sv[pbase:pbase + np_], [sk], [dkey])

    def alloc_scratch(self):
        T = self.T
        self.hT_d = self.scratch("hT_d", [128, 8, T], BF16)
        self.fmf_d = self.scratch("fmf_d", [128, 16, T], F32)
        self.fmb_d = self.scratch("fmb_d", [128, 7, T], BF16)
        self.tmb_d = self.scratch("tmb_d", [T, 641], BF16)
        self.tmf_d = self.scratch("tmf_d", [T, 512], F32)
        self.ckvT_d = self.scratch("ckvT_d", [128, T], BF16)
        self.iw_d = self.scratch("iw_d", [128, self.NT, 4], F32)
        self.yT_d = self.scratch("yT_d", [128, 8, T], BF16)
        self.xmid_d = self.scratch("xmid_d", [T, D_MODEL], F32)
        self.xres_d = self.scratch("xres_d", [T, D_MODEL], F32)

    def consts(self, es):
        nc, S = self.nc, self.S
        self.ident_in = nc.dram_tensor("ident", [128, 128], F32, kind="ExternalInput").ap()
        self.cmask_in = nc.dram_tensor("cmask", [128, 128], F32, kind="ExternalInput").ap()
        self.bdmask_in = nc.dram_tensor("bdmask", [128, 128], F32, kind="ExternalInput").ap()
        self.scanm_in = nc.dram_tensor("scanm", [128, 128], F32, kind="ExternalInput").ap()
        self.cneg_in = nc.dram_tensor("cneg", [128, 128], F32, kind="ExternalInput").ap()
        self.relb_in = nc.dram_tensor("relb", [32, 4], F32, kind="ExternalInput").ap()
        self.identb = es.enter_context(nc.sbuf_tensor("identb", [128, 128], BF16))
        self.identf = es.enter_context(nc.sbuf_tensor("identf", [128, 128], F32))
        S.dma('sp', self.identf[:], self.ident_in, writes=['identf'])
        self.cp('dve', self.identb[:], self.identf[:], ['identf'], ['identb'])

    def phase1(self, l, x_src):
        nc, S, T = self.nc, self.S, self.T
        W = self.w[l]
        with ExitStack() as es:
            def sb(name, shape, dt):
                return es.enter_context(nc.sbuf_tensor(self.u(name), shape, dt))

            def pt(name, shape, dt):
                return es.enter_context(nc.psum_tensor(self.u(name), shape, dt))

            def ring(name, n, shape, dt, psum=False):
                tiles = []
                for i in range(n):
                    nm = self.u(name)
                    t = pt(nm, shape, dt) if psum else sb(nm, shape, dt)
                    tiles.append((t, nm))
                r = Ring.__new__(Ring)
                r.tiles, r.i = tiles, 0
                return r

            wfm = sb('wfm', [128, 8, 2304], BF16)
            wtm = sb('wtm', [128, 8, 1156], BF16)
            wuk = sb('wuk', [128, 2, 128], BF16)
            wgk = sb('wgk', [16, 256], BF16)
            pv = sb('pv', [128, NPV], F32)
            lbc = sb('lbc', [128, 8], F32)
            nbgk = sb('nbgk', [128, 2], F32)
            g1b = sb('g1b', [128, 1024], F32)
            gkvb = sb('gkvb', [128, 128], F32)
            iw_all = sb('iw_all', [128, self.NT, 4], F32)
            stage = ring('stg', 3, [128, IN_COLS], F32)

            S.dma('sp', pv[:], W['pvec'], writes=['pv'])
            S.dma('sp', g1b[:], W['bvec'][:, BV_N1:BV_N1 + 1024].partition_broadcast(128), writes=['g1b'])
            S.dma('sp', gkvb[:], W['bvec'][:, BV_KV:BV_KV + 128].partition_broadcast(128), writes=['gkvb'])
            if l == 0:
                S.op('dve', lambda e: e.memset(lbc[:, 0:2], 0.0), [], ['lbc'])
            else:
                self.tt('dve', lbc[:, 6:8], pv[:, 4:6], pv[:, 2:4], ALU.subtract, ['pv'], ['lbc'])
                self.act(lbc[:, 0:2], lbc[:, 6:8], AF.Sigmoid, ['lbc'], ['lbc'])
            self.ts('dve', lbc[:, 2:4], lbc[:, 0:2], -1.0, 1.0, ALU.mult, ALU.add, ['lbc'], ['lbc'])
            self.ts('dve', lbc[:, 4:6], lbc[:, 0:2], 1e-20, None, ALU.max, None, ['lbc'], ['lbc'])
            self.ts('dve', nbgk[:], pv[:, 0:2], -1.0, None, ALU.mult, None, ['pv'], ['nbgk'])

            fm_src = [(O_AQ, 256), (O_IQ, 256), (O_IK, 64), (O_IK, 64), (O_BQ, 256), (O_BK, 256), (O_BLR, 16),
                      (O_CX, 256), (O_CY, 256), (O_DQ, 256), (O_DF, 256)]
            fm_dst = [0, 256, 512, 576, 640, 896, 1152, 1280, 1536, 1792, 2048]
            tm_src = [(O_CKV, 128), (O_IW, 4), (O_BV, 256), (O_BR, 256), (O_DI, 256), (O_DG, 256)]
            tm_dst = [0, 128, 132, 388, 644, 900]
            for k in range(8):
                st, sk = stage.next()
                S.dma('sp', st[:, 0:IN_COLS], W['w_in'][k * 128:(k + 1) * 128, :], writes=[sk])
                ci_ = 0
                for dst_t, key, srcs, dsts in ((wfm, ('wfm', k), fm_src, fm_dst), (wtm, ('wtm', k), tm_src, tm_dst)):
                    for (so, n), do in zip(srcs, dsts):
                        ci_ += 1
                        self.cp(('pool', 'dve', 'act')[ci_ % 3], dst_t[:, k, do:do + n], st[:, so:so + n], [sk], [key])
            self.load_cast(stage, wuk[:], 'wuk', W['w_uk'].rearrange("(hp e) d c -> (e d) hp c", e=2), [2, 128])
            self.load_cast(stage, wgk[:], 'wgk', W['w_gk2'], [256])

            xr = ring('xt', 3, [128, 1024], F32)
            junk = sb('junk', [128, 1024], F32)
            hr = ring('h', 2, [128, 1024], BF16)
            hTr = ring('hT', 2, [128, 8, 512], BF16)
            st1 = ring('st1', 4, [128, 4], F32)
            ptr = ring('ptr', 2, [128, 8, 128], BF16, psum=True)
            psr = ring('ps', 4, [128, 512], F32, psum=True)
            ptc = ring('ptc', 1, [128, 128], BF16, psum=True)
            tmbr = ring('tmb', 3, [128, 641], BF16)
            tmfr = ring('tmf', 3, [128, 512], F32)
            ckvTr = ring('ckvT', 2, [128, 512], BF16)
            qTr = ring('qT', 2, [128, 2, 512], BF16)
            blrr = ring('blr', 2, [16, 512], BF16)
            ofr = ring('of', 6, [128, 512], F32)
            obr = ring('ob', 4, [128, 512], BF16)
            for t_, _k in tmbr.tiles:
                S.op('pool', lambda e, t_=t_: e.memset(t_[:, 128:129], 1.0), [], [_k])

            def stage_n(st_i):
                tok0 = st_i * 512
                hT, hTk = hTr.next()
                for j in range(4):
                    t0 = tok0 + j * 128
                    xt, xk = xr.next()
                    S.dma('sp', xt[:], x_src[t0:t0 + 128, :], writes=[xk])
                    s1, s1k = st1.next()
                    self.act(junk[:], xt[:], AF.Square, [xk], ['junk', s1k], accum=s1[:, 0:1])
                    self.act(s1[:, 1:2], s1[:, 0:1], AF.Sqrt, [s1k], [s1k], bias=EPS, scale=1.0 / 1024)
                    S.op('dve', lambda e, s1=s1: e.reciprocal(out=s1[:, 2:3], in_=s1[:, 1:2]), [s1k], [s1k])
                    h, hk = hr.next()
                    self.stt(h[:], xt[:], s1[:, 2:3], g1b[:], ALU.mult, ALU.mult, [xk, s1k, 'g1b'], [hk])
                    p, pk = ptr.next()
                    for k in range(8):
                        self.tr(p[:, k, :], h[:, k * 128:(k + 1) * 128], self.identb[:], [hk, 'identb'], [pk])
                    self.cp('act', hT[:, :, j * 128:(j + 1) * 128], p[:], [pk], [hTk])
                return tok0, hT, hTk

            def stage_rest(tok0, hT, hTk):
                ckvT, ckvTk = ckvTr.next()
                for j in range(4):
                    t0 = tok0 + j * 128
                    tt_i = t0 // 128
                    tmb, tmbk = tmbr.next()
                    tmf, tmfk = tmfr.next()
                    pa, pak = psr.next()
                    for k in range(8):
                        self.mm(pa[:, 0:388], hT[:, k, j * 128:(j + 1) * 128], wtm[:, k, 0:388], k == 0, k == 7,
                                [hTk, ('wtm', k)], [pak])
                    s2, s2k = st1.next()
                    self.act(junk[:, 0:128], pa[:, 0:128], AF.Square, [pak], ['junk', s2k], accum=s2[:, 0:1])
                    self.act(s2[:, 1:2], s2[:, 0:1], AF.Sqrt, [s2k], [s2k], bias=EPS, scale=1.0 / 128)
                    S.op('dve', lambda e, s2=s2: e.reciprocal(out=s2[:, 2:3], in_=s2[:, 1:2]), [s2k], [s2k])
                    self.stt(tmb[:, 0:128], pa[:, 0:128], s2[:, 2:3], gkvb[:], ALU.mult, ALU.mult,
                             [pak, s2k, 'gkvb'], [tmbk])
                    self.ts('dve', iw_all[:, tt_i, :], pa[:, 128:132], 1.0 / 16, None, ALU.mult, None, [pak], ['iw_all'])
                    self.cp('act', tmb[:, 129:385], pa[:, 132:388], [pak], [tmbk])
                    pc, pck = ptc.next()
                    self.tr(pc[:], tmb[:, 0:128], self.identb[:], [tmbk, 'identb'], [pck])
                    self.cp('dve', ckvT[:, j * 128:(j + 1) * 128], pc[:], [pck], [ckvTk])
                    pb, pbk = psr.next()
                    for k in range(8):
                        self.mm(pb[:, 0:512], hT[:, k, j * 128:(j + 1) * 128], wtm[:, k, 388:900], k == 0, k == 7,
                                [hTk, ('wtm', k)], [pbk])
                    self.act(tmf[:, 0:256], pb[:, 0:256], AF.Silu, [pbk], [tmfk])
                    self.cp('dve', tmb[:, 385:641], pb[:, 256:512], [pbk], [tmbk])
                    pcc, pcck = psr.next()
                    for k in range(8):
                        self.mm(pcc[:, 0:256], hT[:, k, j * 128:(j + 1) * 128], wtm[:, k, 900:1156], k == 0, k == 7,
                                [hTk, ('wtm', k)], [pcck])
                    self.act(tmf[:, 256:512], pcc[:, 0:256], AF.Silu, [pcck], [tmfk])
                    S.dma('sp', self.tmb_d[t0:t0 + 128, :], tmb[:], reads=[tmbk], defer=True)
                    S.dma('sp', self.tmf_d[t0:t0 + 128, :], tmf[:], reads=[tmfk], defer=True)
                S.dma('sp', self.hT_d[:, :, tok0:tok0 + 512], hT[:], reads=[hTk], defer=True)
                S.dma('sp', self.ckvT_d[:, tok0:tok0 + 512], ckvT[:], reads=[ckvTk], defer=True)

                def fm_block(blk, M=128):
                    pf, pfk = psr.next()
                    for k in range(8):
                        self.mm(pf[0:M, :], wfm[:, k, blk * 128:blk * 128 + M], hT[:, k, :], k == 0, k == 7,
                                [hTk, ('wfm', k)], [pfk])
                    return pf, pfk

                def out_f(slot):
                    o, ok = ofr.next()
                    return o, ok, self.fmf_d[:, slot, tok0:tok0 + 512]

                def out_b(slot):
                    o, ok = obr.next()
                    return o, ok, self.fmb_d[:, slot, tok0:tok0 + 512]

                qT, qTk = qTr.next()
                for hp in range(2):
                    pf, pfk = fm_block(0 + hp)
                    self.cp('act', qT[:, hp, :], pf[:], [pfk], [qTk])
                for h in range(4):
                    hp, e_ = h // 2, h % 2
                    pf, pfk = psr.next()
                    self.mm(pf[:], wuk[e_ * 64:(e_ + 1) * 64, hp, :], qT[e_ * 64:(e_ + 1) * 64, hp, :], True, True,
                            ['wuk', qTk], [pfk])
                    o, ok, dst = out_b(3 + h)
                    self.ts('dve', o[:], pf[:], 0.125, None, ALU.mult, None, [pfk], [ok])
                    S.dma('sp', dst, o[:], reads=[ok], defer=True)
                for hp in range(2):
                    pf, pfk = fm_block(2 + hp)
                    o, ok, dst = out_b(0 + hp)
                    self.cp('act', o[:], pf[:], [pfk], [ok])
                    S.dma('sp', dst, o[:], reads=[ok], defer=True)
                pf, pfk = fm_block(4)
                o, ok, dst = out_b(2)
                self.cp('dve', o[:], pf[:], [pfk], [ok])
                S.dma('sp', dst, o[:], reads=[ok], defer=True)
                for hp in range(2):
                    pf, pfk = fm_block(5 + hp)
                    o, ok, dst = out_f(0 + hp)
                    self.ts('dve', o[:], pf[:], 0.125, None, ALU.mult, None, [pfk], [ok])
                    S.dma('sp', dst, o[:], reads=[ok], defer=True)
                for hp in range(2):
                    pf, pfk = fm_block(7 + hp)
                    o, ok, dst = out_f(2 + hp)
                    self.cp('act', o[:], pf[:], [pfk], [ok])
                    S.dma('sp', dst, o[:], reads=[ok], defer=True)
                yield
                pf, pfk = fm_block(9, M=16)
                blr, blrk = blrr.next()
                self.cp('dve', blr[:], pf[0:16, :], [pfk], [blrk])
                for hp in range(2):
                    pg, pgk = psr.next()
                    self.mm(pg[:], wgk[0:16, hp * 128:(hp + 1) * 128], blr[0:16, :], True, True, ['wgk', blrk], [pgk])
                    o, ok, dst = out_f(4 + hp)
                    self.act(o[:], pg[:], AF.Exp, [pgk, 'nbgk'], [ok], bias=nbgk[:, hp:hp + 1], scale=-1.0)
                    self.act(o[:], o[:], AF.Ln, [ok], [ok], bias=1.0)
                    self.ts('dve', o[:], o[:], -1.0 / 16, None, ALU.mult, None, [ok], [ok])
                    S.dma('sp', dst, o[:], reads=[ok], defer=True)
                for i_, slot0 in ((10, 6), (12, 8)):
                    for hp in range(2):
                        pf, pfk = fm_block(i_ + hp)
                        o, ok, dst = out_f(slot0 + hp)
                        self.cp('act' if hp else 'dve', o[:], pf[:], [pfk], [ok])
                        S.dma('sp', dst, o[:], reads=[ok], defer=True)
                for hp in range(2):
                    pf, pfk = fm_block(14 + hp)
                    o, ok, dst = out_f(10 + hp)
                    self.act(o[:], pf[:], AF.Silu, [pfk], [ok])
                    S.dma('sp', dst, o[:], reads=[ok], defer=True)
                for hp in range(2):
                    pf, pfk = fm_block(16 + hp)
                    o, ok, dst = out_f(12 + hp)
                    self.act(o[:], pf[:], AF.Sigmoid, [pfk], [ok], scale=-1.0)
                    self.ts('dve', o[:], o[:], lbc[:, 2 + hp:3 + hp], None, ALU.mult, None, [ok, 'lbc'], [ok])
                    S.dma('sp', dst, o[:], reads=[ok], defer=True)
                    o2, ok2, dst2 = out_f(14 + hp)
                    self.act(o2[:], pf[:], AF.Sigmoid, [pfk], [ok2])
                    self.ts('dve', o2[:], o2[:], lbc[:, 2 + hp:3 + hp], lbc[:, 4 + hp:5 + hp], ALU.mult, ALU.add,
                            [ok2, 'lbc'], [ok2])
                    self.act(o2[:], o2[:], AF.Ln, [ok2], [ok2])
                    S.dma('sp', dst2, o2[:], reads=[ok2], defer=True)
            ctx = stage_n(0)
            nS_ = T // 512
            for st_i in range(nS_):
                g_ = stage_rest(*ctx)
                next(g_)
                ctx = stage_n(st_i + 1) if st_i + 1 < nS_ else None
                for _ in g_:
                    pass
            S.dma('sp', self.iw_d, iw_all[:], reads=['iw_all'], defer=True)
            S.barrier()

    def build(self, phases=None):
        self.es = ExitStack()
        self.alloc_scratch()
        self.consts(self.es)
        self.S.barrier()
        x_src = self.x_in
        for l in range(self.depth):
            last = l == self.depth - 1
            def on(p):
                return phases is None or p in phases
            if on('p1'):
                self.phase1(l, x_src)
            if on('dsa'):
                self.phase_dsa(l)
            wh = tuple(b_ for b_, n_ in ((1, 'gla'), (3, 'hgrn')) if on(n_))
            if wh or on('lru'):
                self.phase_gla2(l, wh, on('lru'))
            if on('merge'):
                self.phase_merge(l, x_src)
            if on('ffn'):
                self.phase_ffn(l, self.out if last else self.xres_d, last)
            x_src = self.xres_d
        self.S.finish('sp')
        self.S.emit()
        self.es.close()
        return self.nc


def const_inputs():
    ident = np.eye(128, dtype=np.float32)
    j = np.arange(128)[:, None]
    i = np.arange(128)[None, :]
    cmask = (j <= i).astype(np.float32)
    bdmask = ((j <= i) & (j // 64 == i // 64)).astype(np.float32)
    scanm = np.ones((128, 128), np.float32)
    scanm[:, 0] = 0
    scanm[:, 64] = 0
    cneg = np.where(i <= j, 0.0, -1e30).astype(np.float32)
    return {'ident': ident, 'cmask': cmask, 'bdmask': bdmask, 'scanm': scanm, 'cneg': cneg}


def t5_bucket_np(dist):
    n = np.maximum(dist, 0)
    nf = np.maximum(n, 16).astype(np.float32)
    large = 16 + (np.log(nf / np.float32(16)) / np.float32(np.log(128 / 16)) * np.float32(16)).astype(np.int32)
    large = np.minimum(large, 31)
    return np.where(n < 16, n, large)


def bias_table(rel_bias):
    kk = np.arange(128)[:, None]
    qq = np.arange(128)[None, :]
    bp = t5_bucket_np(qq - kk + 128)
    bd = t5_bucket_np(qq - kk)
    idx = np.concatenate([bp, bd], axis=1)
    rb = np.asarray(rel_bias, np.float32)
    return np.ascontiguousarray(rb[idx].transpose(0, 2, 1))


def make_in_map(inp, b, depth=DEPTH):
    m = {'x': np.ascontiguousarray(inp['x'][b], dtype=np.float32)}
    for l in range(depth):
        for name in ('w_in', 'w_gate', 'w_uk', 'w_uv', 'w_gk2', 'w_rg_a', 'w_rg_x', 'w_br_a', 'w_br_b', 'w_br_c',
                     'w_br_d', 'w_out', 'w_ff1', 'w_ff2'):
            m[f"{name}{l}"] = np.ascontiguousarray(inp[name][l], dtype=np.float32)
        m[f"pvec{l}"] = pack_pvec(inp, l)
        m[f"bvec{l}"] = pack_bvec(inp, l)
    m['biasT'] = bias_table(inp['rel_bias'])
    m['relb'] = np.ascontiguousarray(inp['rel_bias'], dtype=np.float32)
    m.update(const_inputs())
    return m


def _phase_tools(self, es):
    nc = self.nc

    def sb(name, shape, dt):
        return es.enter_context(nc.sbuf_tensor(self.u(name), shape, dt))

    def pt(name, shape, dt):
        return es.enter_context(nc.psum_tensor(self.u(name), shape, dt))

    def ring(name, n, shape, dt, psum=False):
        tiles = []
        for i in range(n):
            nm = self.u(name)
            t = pt(nm, shape, dt) if psum else sb(nm, shape, dt)
            tiles.append((t, nm))
        r = Ring.__new__(Ring)
        r.tiles, r.i = tiles, 0
        return r
    return sb, pt, ring


NBIS = 14


def phase_dsa(self, l):
    nc, S, T, NT = self.nc, self.S, self.T, self.NT
    W = self.w[l]
    topk = min(256, T // 4)
    with ExitStack() as es:
        sb, pt, ring = _phase_tools(self, es)
        ckvT = sb('ckvT', [128, T], BF16)
        ckv1 = sb('ckv1', [128, NT, 129], BF16)
        ikT = sb('ikT', [128, T], BF16)
        iw = sb('iw', [128, NT, 4], F32)
        wuv = sb('wuv', [128, 4, 64], BF16)
        I4 = sb('I4', [128, 4, 128], BF16)
        cneg = sb('cneg', [128, 128], F32)
        bT = sb('bT', [128, 4, 256], F32)
        bhi = sb('bhi', [128, 4, 256], BF16)
        blo = sb('blo', [128, 4, 256], BF16)
        bhf = sb('bhf', [128, 4, 256], F32)
        b31 = sb('b31', [128, 4], F32)
        stage = ring('stg', 2, [128, 256], F32)
        S.dma('sp', ckvT[:], self.ckvT_d, writes=['ckvT'])
        S.dma('sp', ckv1[:], self.tmb_d[:, 0:129].rearrange("(t p) c -> p t c", p=128), writes=['ckv1'])
        S.dma('sp', ikT[:], self.fmb_d[:, 2, :], writes=['ikT'])
        S.dma('sp', iw[:], self.iw_d, writes=['iw'])
        S.dma('sp', cneg[:], self.cneg_in, writes=['cneg'])
        S.dma('sp', bT[:], self.biasT, writes=['bT'])
        S.dma('sp', b31[:], self.relb_in[31:32, :].partition_broadcast(128), writes=['b31'])
        self.load_cast(stage, wuv[:], 'wuv', W['w_uv'].rearrange("h c d -> c h d"), [4, 64])
        for h in range(4):
            self.cp('pool', I4[:, h, :], self.identb[:], ['identb'], ['I4'])
            self.ts('dve', bT[:, h, :], bT[:, h, :], b31[:, h:h + 1], None, ALU.subtract, None, ['bT', 'b31'], ['bT'])
        self.cp('dve', bhi[:], bT[:], ['bT'], ['bhi'])
        self.cp('dve', bhf[:], bhi[:], ['bhi'], ['bhf'])
        self.tt('dve', bhf[:], bT[:], bhf[:], ALU.subtract, ['bT', 'bhf'], ['bhf'])
        self.cp('dve', blo[:], bhf[:], ['bhf'], ['blo'])

        G = 2
        iqr = ring('iq', 3, [128, 2, 128], BF16)
        qlr = ring('ql', 3 * G, [128, 4, 128], BF16)
        scr = ring('sc', 2 * G, [128, T], F32)
        mnr = ring('mn', 2 * G, [128, T], BF16)
        junk = sb('junkb', [128, T], BF16)
        rlr = ring('rl', 3, [128, 512], F32)
        bs = ring('bs', 3, [128, 8, G], F32)
        pss = ring('pss', 2, [128, 512], F32, psum=True)
        psl = ring('psl', 2, [128, 512], F32, psum=True)
        poA = ring('poA', 1, [128, 2, 129], F32, psum=True)
        poB = ring('poB', 1, [128, 2, 129], F32, psum=True)
        ptr = ring('ptr', 1, [128, 4, 128], BF16, psum=True)
        pyr = ring('py', 1, [128, 2, 128], F32, psum=True)
        pr_ = ring('p', 3, [128, 512], BF16)
        olr = ring('ol', 2, [128, 4, 128], BF16)
        olTr = ring('olT', 2, [128, 4, 128], BF16)
        yr = ring('ya', 2, [128, 2, 128], BF16)
        rdr = ring('rd', 2, [128, 4], F32)
        NG = (NT + G - 1) // G
        info = {}

        def gen_scores(g):
            tiles = list(range(g * G, min(NT, g * G + G)))
            b, bk = bs.next()
            grp = []
            info[g] = (b, bk, grp)
            for gi, qi in enumerate(tiles):
                q0 = qi * 128
                nk = (qi + 1) * 128
                iq, iqk = iqr.next()
                ql, qlk = qlr.next()
                S.dma('sp', iq[:], self.fmb_d[:, 0:2, q0:q0 + 128], writes=[iqk])
                S.dma('sp', ql[:], self.fmb_d[:, 3:7, q0:q0 + 128], writes=[qlk])
                sc, sck = scr.next()
                mn, mnk = mnr.next()
                grp.append((qi, q0, nk, ql, qlk, sc, sck, mn, mnk))
                for k0 in range(0, nk, 512):
                    n = min(512, nk - k0)
                    for h in range(4):
                        hp, e_ = h // 2, h % 2
                        ps, psk = pss.next()
                        self.mm(ps[:, 0:n], iq[e_ * 64:(e_ + 1) * 64, hp, :], ikT[e_ * 64:(e_ + 1) * 64, k0:k0 + n],
                                True, True, [iqk, 'ikT'], [psk])
                        if h == 0:
                            self.ts('dve', sc[:, k0:k0 + n], ps[:, 0:n], 0.0, iw[:, qi, 0:1], ALU.max, ALU.mult,
                                    [psk, 'iw'], [sck])
                        else:
                            rl, rlk = rlr.next()
                            self.act(rl[:, 0:n], ps[:, 0:n], AF.Relu, [psk], [rlk])
                            self.stt(sc[:, k0:k0 + n], rl[:, 0:n], iw[:, qi, h:h + 1], sc[:, k0:k0 + n], ALU.mult, ALU.add,
                                     [rlk, 'iw', sck], [sck])
                    yield
                S.op('dve', lambda e, b=b, sc=sc, nk=nk, gi=gi: e.tensor_reduce(
                    out=b[:, 0, gi:gi + 1], in_=sc[:, 0:nk], axis=AX.X, op=ALU.min), [sck], [(bk, 'mn', gi)])
                S.op('dve', lambda e, b=b, sc=sc, nk=nk, gi=gi: e.tensor_reduce(
                    out=b[:, 1, gi:gi + 1], in_=sc[:, 0:nk], axis=AX.X, op=ALU.max), [sck], [(bk, 'mx', gi)])
                self.tt('dve', sc[:, q0:q0 + 128], sc[:, q0:q0 + 128], cneg[:], ALU.add, [sck, 'cneg'], [sck])
                yield

        def n_scores(g):
            return sum(((qi + 1) * 128 + 511) // 512 + 1 for qi in range(g * G, min(NT, g * G + G)))

        def gen_attend(g):
            for (qi, q0, nk, ql, qlk, sc, sck, mn, mnk) in info[g][2]:
                yield from self.dsa_attend(qi, q0, ql, qlk, mn, mnk, ckvT, ckv1, I4, bhi, blo, wuv,
                                           psl, poA, poB, ptr, pyr, pr_, olr, olTr, yr, rdr)

        def n_attend(g):
            return sum(qi + 2 for qi in range(g * G, min(NT, g * G + G)))

        def advance(gen, n):
            if gen is None:
                return None
            for _ in range(n):
                try:
                    next(gen)
                except StopIteration:
                    return None
            return gen

        advance(gen_scores(0), 10 ** 9)
        for g in range(NG):
            b, bk, grp = info[g]
            ng = len(grp)
            A = gen_scores(g + 1) if g + 1 < NG else None
            C = gen_attend(g - 1) if g >= 1 else None
            stepA = (n_scores(g + 1) + NBIS - 1) // NBIS if A is not None else 0
            stepC = (n_attend(g - 1) + NBIS - 1) // NBIS if C is not None else 0
            gs = slice(0, ng)
            kmm, kR, kC, kU, kT, kH = (bk, 'mm'), (bk, 'R'), (bk, 'cand'), (bk, 'u'), (bk, 'tmp'), (bk, 'thr')
            kin = [(bk, 'mn', gi) for gi in range(ng)] + [(bk, 'mx', gi) for gi in range(ng)]
            self.ts('dve', b[:, 0, gs], b[:, 0, gs], -1.0, None, ALU.add, None, kin, [kmm])
            self.tt('dve', b[:, 2, gs], b[:, 1, gs], b[:, 0, gs], ALU.subtract, kin + [kmm], [kR])
            self.stt(b[:, 3, gs], b[:, 2, gs], 0.5, b[:, 0, gs], ALU.mult, ALU.add, [kR, kmm], [kC])
            for it in range(NBIS):
                ci = 0.5 ** (it + 1)
                for gi, (qi, q0, nk, ql, qlk, sc, sck, mn, mnk) in enumerate(grp):
                    self.ts('dve', junk[:, 0:nk], sc[:, 0:nk], b[:, 3, gi:gi + 1], 0.0, ALU.is_ge, ALU.add,
                            [sck, kC], [(bk, 'cnt', gi)], accum=b[:, 4, gi:gi + 1])
                self.ts('dve', b[:, 5, gs], b[:, 4, gs], float(topk), -0.5, ALU.is_ge, ALU.add,
                        [(bk, 'cnt', gi) for gi in range(ng)], [kU])
                self.tt('dve', b[:, 6, gs], b[:, 5, gs], b[:, 2, gs], ALU.mult, [kU, kR], [kT])
                self.stt(b[:, 3, gs], b[:, 6, gs], ci, b[:, 3, gs], ALU.mult, ALU.add, [kT, kC], [kC])
                A = advance(A, stepA)
                C = advance(C, stepC)
            advance(A, 10 ** 9)
            advance(C, 10 ** 9)
            self.stt(b[:, 7, gs], b[:, 2, gs], -(0.5 ** (NBIS + 1)), b[:, 3, gs], ALU.mult, ALU.add, [kR, kC], [kH])
            for gi, (qi, q0, nk, ql, qlk, sc, sck, mn, mnk) in enumerate(grp):
                self.ts('dve', mn[:, 0:nk], sc[:, 0:nk], b[:, 7, gi:gi + 1], -30000.0, ALU.is_lt, ALU.mult,
                        [sck, kH], [mnk])
        advance(gen_attend(NG - 1), 10 ** 9)
        S.barrier()


def dsa_attend(self, qi, q0, ql, qlk, mn, mnk, ckvT, ckv1, I4, bhi, blo, wuv,
               psl, poA, poB, ptr, pyr, pr_, olr, olTr, yr, rdr):
    S = self.S
    if True:
        if True:
            pa, pak = poA.next()
            pb, pbk = poB.next()
            qlf = ql[:].rearrange("p h q -> p (h q)")
            for kb in range(qi + 1):
                pl, plk = psl.next()
                near = kb >= qi - 1
                self.mm(pl[:], ckvT[:, kb * 128:(kb + 1) * 128], qlf, True, False, ['ckvT', qlk], [plk])
                self.mm(pl[:], mn[:, kb * 128:(kb + 1) * 128], I4[:].rearrange("p h q -> p (h q)"), False, not near,
                        [mnk, 'I4'], [plk])
                if near:
                    o_ = 128 if kb == qi else 0
                    for bt_, bkey, last in ((bhi, 'bhi', False), (blo, 'blo', True)):
                        for h in range(4):
                            self.mm(pl[:, h * 128:(h + 1) * 128], self.identb[:], bt_[:, h, o_:o_ + 128], False,
                                    last and h == 3, ['identb', bkey], [plk])
                p, pk = pr_.next()
                self.act(p[:], pl[:], AF.Exp, [plk], [pk])
                for h in range(4):
                    po, pok = (pa, pak) if h < 2 else (pb, pbk)
                    self.mm(po[:, h % 2, :], p[:, h * 128:(h + 1) * 128], ckv1[:, kb, :], kb == 0 and h % 2 == 0, kb == qi,
                            [pk, 'ckv1'], [pok])
                yield
            rd, rdk = rdr.next()
            ol, olk = olr.next()
            for h in range(4):
                po, pok = (pa, pak) if h < 2 else (pb, pbk)
                S.op('dve', lambda e, rd=rd, po=po, h=h: e.reciprocal(out=rd[:, h:h + 1], in_=po[:, h % 2, 128:129]),
                     [pok], [rdk])
                self.ts('dve', ol[:, h, :], po[:, h % 2, 0:128], rd[:, h:h + 1], None, ALU.mult, None, [pok, rdk], [olk])
            ptt, pttk = ptr.next()
            for h in range(4):
                self.tr(ptt[:, h, :], ol[:, h, :], self.identb[:], [olk, 'identb'], [pttk])
            olT, olTk = olTr.next()
            self.cp('act', olT[:], ptt[:], [pttk], [olTk])
            py, pyk = pyr.next()
            for h in range(4):
                hp, e_ = h // 2, h % 2
                self.mm(py[e_ * 64:(e_ + 1) * 64, hp, :], wuv[:, h, :], olT[:, h, :], True, True, ['wuv', olTk], [pyk])
            ya, yak = yr.next()
            self.cp('act', ya[:], py[:], [pyk], [yak])
            S.dma('sp', self.yT_d[:, 0:2, q0:q0 + 128], ya[:], reads=[yak], defer=True)
            yield


Prog.dsa_attend = dsa_attend
Prog.phase_dsa = phase_dsa


def phase_gla(self, l, br, shared):
    nc, S, T, NT = self.nc, self.S, self.T, self.NT
    W = self.w[l]
    if br == 1:
        sq_, sk_, sg_, vcol, gcol, bvo = 0, 2, 4, 129, 0, BV_GLA
    else:
        sq_, sk_, sg_, vcol, gcol, bvo = 10, 12, 14, 385, 256, BV_HG
    with ExitStack() as es:
        sb, pt, ring = _phase_tools(self, es)
        scanm = sb('scanm', [128, 128], F32)
        bdm = sb('bdm', [128, 2, 128], F32)
        gnb = sb('gnb', [128, 256], F32)
        Sf = sb('Sf', [128, 2, 64], F32)
        S.dma('sp', scanm[:], self.scanm_in, writes=['scanm'])
        S.dma('sp', bdm[:, 0, :], self.bdmask_in, writes=['bdm'])
        S.dma('sp', bdm[:, 1, :], self.bdmask_in, writes=['bdm'])
        S.dma('sp', gnb[:], W['bvec'][:, bvo:bvo + 256].partition_broadcast(128), writes=['gnb'])
        S.op('pool', lambda e: e.memset(Sf[:], 0.0), [], ['Sf'])
        Sbr = ring('Sb', 4, [128, 2, 64], BF16)
        Sb, Sbk = Sbr.next()
        S.op('pool', lambda e, Sb=Sb: e.memset(Sb[:], 0.0), [], [Sbk])
        qr = ring('q', 2, [128, 2, 128], F32)
        kr = ring('k', 2, [128, 2, 128], F32)
        gr = ring('g', 2, [128, 2, 128], F32)
        vr = ring('v', 2, [128, 256], BF16)
        gtr = ring('gt', 2, [128, 256], F32)
        br_ = ring('b', 2, [128, 128], F32)
        bmr = ring('bm', 2, [128, 128], F32)
        e3r = ring('e3', 2, [128, 3, 128], F32)
        qer = ring('qe', 2, [128, 128], BF16)
        ker = ring('ke', 2, [128, 128], BF16)
        q1r = ring('q1', 2, [128, 128], BF16)
        ebr = ring('eb', 2, [128, 4], F32)
        atr = ring('at', 2, [128, 2, 128], BF16)
        ktr = ring('kt', 2, [128, 128], BF16)
        tmr = ring('tm', 2, [128, 64], F32)
        jk = sb('jk', [128, 256], F32)
        ssr = ring('ss', 2, [128, 12], F32)
        yr = ring('y', 2, [128, 256], F32)
        ybr = ring('yb', 2, [128, 256], BF16)
        yTr = ring('yT', 2, [128, 2, 128], BF16)
        patE, pkt, pdsC, porE, pyt = shared

        for ti in range(NT):
            t0 = ti * 128
            q, qk = qr.next()
            k, kk = kr.next()
            g, gk = gr.next()
            v, vk = vr.next()
            gt, gtk = gtr.next()
            S.dma('sp', g[:], self.fmf_d[:, sg_:sg_ + 2, t0:t0 + 128], writes=[gk])
            S.dma('sp', q[:], self.fmf_d[:, sq_:sq_ + 2, t0:t0 + 128], writes=[qk])
            S.dma('sp', k[:], self.fmf_d[:, sk_:sk_ + 2, t0:t0 + 128], writes=[kk])
            S.dma('sp', v[:], self.tmb_d[t0:t0 + 128, vcol:vcol + 256], writes=[vk])
            S.dma('sp', gt[:], self.tmf_d[t0:t0 + 128, gcol:gcol + 256], writes=[gtk])
            poE = [porE[0].next(), porE[1].next()]
            Sb0, Sb0k = Sb, Sbk
            Sb1, Sb1k = Sbr.next()
            Sb2, Sb2k = Sbr.next()
            for hp in range(2):
                b, bk = br_.next()
                bm, bmk = bmr.next()
                S.op('dve', lambda e, b=b, g=g, hp=hp: e.tensor_tensor_scan(
                    out=b[:], data0=scanm[:], data1=g[:, hp, :], initial=0.0, op0=ALU.mult, op1=ALU.add),
                    ['scanm', gk], [bk])
                for c in range(2):
                    self.ts('dve', bm[:, c * 64:(c + 1) * 64], b[:, c * 64:(c + 1) * 64], b[:, c * 64 + 31:c * 64 + 32],
                            None, ALU.subtract, None, [bk], [bmk])
                e3, e3k = e3r.next()
                self.act(e3[:, 0, :], bm[:], AF.Exp, [bmk], [e3k])
                self.act(e3[:, 1, :], bm[:], AF.Exp, [bmk], [e3k], scale=-1.0)
                self.act(e3[:, 2, :], b[:], AF.Exp, [bk], [e3k])
                eb, ebk = ebr.next()
                for c in range(2):
                    self.act(eb[:, c:c + 1], b[:, c * 64 + 63:c * 64 + 64], AF.Exp, [bk], [ebk])
                    self.act(eb[:, 2 + c:3 + c], bm[:, c * 64 + 63:c * 64 + 64], AF.Exp, [bmk], [ebk])
                qe, qek = qer.next()
                ke, kek = ker.next()
                q1, q1k = q1r.next()
                self.tt('dve', qe[:], q[:, hp, :], e3[:, 0, :], ALU.mult, [qk, e3k], [qek])
                self.tt('pool', ke[:], k[:, hp, :], e3[:, 1, :], ALU.mult, [kk, e3k], [kek])
                self.tt('pool', q1[:], q[:, hp, :], e3[:, 2, :], ALU.mult, [qk, e3k], [q1k])
                at, atk = atr.next()
                for e_ in range(2):
                    sl = slice(e_ * 64, (e_ + 1) * 64)
                    pa, pak = patE[e_].next()
                    self.mm(pa[:, 0:128], ke[sl, :], qe[sl, :], True, True, [kek, qek], [pak])
                    self.tt('dve', at[:, e_, :], pa[:, 0:128], bdm[:, 0, :], ALU.mult, [pak, 'bdm'], [(atk, e_)])
                pk_, pkk = pkt.next()
                self.tr(pk_[:, 0:128], ke[:], self.identb[:], [kek, 'identb'], [pkk])
                kt, ktk = ktr.next()
                self.cp('act', kt[:], pk_[:, 0:128], [pkk], [ktk])
                pdc = [pdsC[0].next(), pdsC[1].next()]
                for c in range(2):
                    cs = slice(c * 64, (c + 1) * 64)
                    pd, pdk = pdc[c]
                    for e_ in range(2):
                        sl = slice(e_ * 64, (e_ + 1) * 64)
                        h = hp * 2 + e_
                        self.mm(pd[sl, 0:64], kt[cs, sl], v[cs, h * 64:(h + 1) * 64], True, True, [ktk, vk], [pdk])
                for c, (Sn, Snk) in enumerate(((Sb1, Sb1k), (Sb2, Sb2k))):
                    tm, tmk = tmr.next()
                    pd, pdk = pdc[c]
                    self.ts('dve', tm[:], pd[:, 0:64], eb[:, 2 + c:3 + c], None, ALU.mult, None, [pdk, ebk], [tmk])
                    self.stt(Sf[:, hp, :], Sf[:, hp, :], eb[:, c:c + 1], tm[:], ALU.mult, ALU.add, ['Sf', ebk, tmk], ['Sf'])
                    self.cp('act', Sn[:, hp, :], Sf[:, hp, :], ['Sf'], [Snk])
                for e_ in range(2):
                    sl = slice(e_ * 64, (e_ + 1) * 64)
                    h = hp * 2 + e_
                    hs = slice(h * 64, (h + 1) * 64)
                    po, pok = poE[e_]
                    ps_ = slice(hp * 64, (hp + 1) * 64)
                    self.mm(po[:, ps_], at[:, e_, :], v[:, hs], True, False, [(atk, e_), vk], [pok])
                    self.mm(po[0:64, ps_], q1[sl, 0:64], Sb0[sl, hp, :], False, False, [q1k, Sb0k], [pok])
                    self.mm(po[64:128, ps_], q1[sl, 64:128], Sb1[sl, hp, :], False, True, [q1k, Sb1k], [pok])
            Sb, Sbk = Sb2, Sb2k
            ss, ssk = ssr.next()
            for e_ in range(2):
                self.act(jk[:, e_ * 128:(e_ + 1) * 128], poE[e_][0][:, 0:128], AF.Square, [poE[e_][1]], ['jk'])
            S.op('dve', lambda e, ss=ss: e.tensor_reduce(out=ss[:, 0:4], in_=jk[:].rearrange("p (h v) -> p h v", h=4),
                                                         axis=AX.X, op=ALU.add), ['jk'], [ssk])
            self.act(ss[:, 4:8], ss[:, 0:4], AF.Sqrt, [ssk], [ssk], bias=EPS, scale=1.0 / 64)
            S.op('dve', lambda e, ss=ss: e.reciprocal(out=ss[:, 8:12], in_=ss[:, 4:8]), [ssk], [ssk])
            y, yk = yr.next()
            for h in range(4):
                hs = slice(h * 64, (h + 1) * 64)
                hp, e_ = h // 2, h % 2
                si = 8 + e_ * 2 + hp
                self.stt(y[:, hs], poE[e_][0][:, hp * 64:(hp + 1) * 64], ss[:, si:si + 1], gnb[:, hs], ALU.mult, ALU.mult,
                         [poE[e_][1], ssk, 'gnb'], [yk])
            yb, ybk = ybr.next()
            self.tt('pool', yb[:], y[:], gt[:], ALU.mult, [yk, gtk], [ybk])
            py, pyk = pyt.next()
            for hp in range(2):
                self.tr(py[:, hp, :], yb[:, hp * 128:(hp + 1) * 128], self.identb[:], [ybk, 'identb'], [pyk])
            yT, yTk = yTr.next()
            self.cp('act', yT[:], py[:, 0:2, :], [pyk], [yTk])
            S.dma('sp', self.yT_d[:, br * 2:br * 2 + 2, t0:t0 + 128], yT[:], reads=[yTk], defer=True)
            yield
        yield


def phase_gla2(self, l, which=(1, 3), with_lru=True):
    S, NT, T = self.S, self.NT, self.T
    with ExitStack() as es:
        sb, pt, ring = _phase_tools(self, es)
        pkb = ring('pkb', 1, [128, 8, 128], BF16, psum=True)
        pkb_t, pkb_k = pkb.tiles[0]
        r_pkt = Ring.__new__(Ring)
        r_pkt.tiles, r_pkt.i = [(pkb_t[:, 4, :], pkb_k + '_kt')], 0
        r_pyt = Ring.__new__(Ring)
        r_pyt.tiles, r_pyt.i = [(pkb_t, pkb_k + '_yt')], 0
        shared = ([ring('pat', 1, [128, 512], F32, psum=True) for _ in range(2)],
                  r_pkt,
                  [ring('pds', 1, [128, 512], F32, psum=True) for _ in range(2)],
                  [ring('po', 1, [128, 512], F32, psum=True) for _ in range(2)],
                  r_pyt)
        plru = ring('plru', 1, [128, 512], F32, psum=True)
        gens = [self.phase_gla(l, br, shared) for br in which]
        L = self.phase_lru(l, plru) if with_lru else None
        nL = (T // 512) * 2
        doneL = 0
        for ti in range(NT):
            for g_ in gens:
                next(g_)
            if L is not None:
                want = ((ti + 1) * nL) // NT
                while doneL < want:
                    next(L)
                    doneL += 1
        for g_ in gens:
            next(g_)
        if L is not None:
            while doneL < nL:
                next(L)
                doneL += 1
            next(L)
            for _ in L:
                pass
        for g_ in reversed(gens):
            for _ in g_:
                pass
        S.barrier()


Prog.phase_gla2 = phase_gla2
Prog.phase_gla = phase_gla


def phase_lru(self, l, psr):
    nc, S, T = self.nc, self.S, self.T
    W = self.w[l]
    with ExitStack() as es:
        sb, pt, ring = _phase_tools(self, es)
        pv = sb('pv', [128, NPV], F32)
        cc_ = sb('cc', [128, 8], F32)
        wa = sb('wa', [128, 2, 128], BF16)
        wx = sb('wx', [128, 2, 128], BF16)
        stage = ring('stg', 2, [128, 64], F32)
        S.dma('sp', pv[:], W['pvec'], writes=['pv'])
        self.act(cc_[:, 0:2], pv[:, 20:22], AF.Exp, ['pv'], ['cc'], scale=-1.0)
        self.act(cc_[:, 0:2], cc_[:, 0:2], AF.Ln, ['cc'], ['cc'], bias=1.0)
        self.ts('dve', cc_[:, 2:4], cc_[:, 0:2], -8.0, None, ALU.mult, None, ['cc'], ['cc'])
        self.ts('dve', cc_[:, 4:6], cc_[:, 0:2], -16.0, None, ALU.mult, None, ['cc'], ['cc'])
        S.op('pool', lambda e: e.memset(wa[:], 0.0), [], ['wa'])
        S.op('pool', lambda e: e.memset(wx[:], 0.0), [], ['wx'])
        for n in range(4):
            cc, e_ = n // 2, n % 2
            sl = slice(e_ * 64, (e_ + 1) * 64)
            self.load_cast(stage, wa[sl, cc, e_ * 64:(e_ + 1) * 64], 'wa', W['w_rg_a'][n], [64], pbase=e_ * 64)
            self.load_cast(stage, wx[sl, cc, e_ * 64:(e_ + 1) * 64], 'wx', W['w_rg_x'][n], [64], pbase=e_ * 64)
        LV = int(os.environ.get('DBG_LRU', '9'))
        cxr = [ring('cx', 2, [128, 515], F32) for _ in range(2)]
        hhr = [ring('hh', 2, [128, 512], F32) for _ in range(2)]
        cyr = ring('cy', 2, [128, 512], F32)
        xcr = ring('xc', 2, [128, 512], F32)
        xbr = ring('xb', 2, [128, 512], BF16)
        rr = ring('r', 2, [128, 512], F32)
        ir = ring('i', 2, [128, 512], F32)
        ar = ring('a', 2, [128, 512], F32)
        a2r = ring('a2', 2, [128, 512], F32)
        ur = ring('u', 2, [128, 512], F32)
        gr = ring('gg', 2, [128, 512], F32)
        g2r = ring('g2', 2, [128, 512], F32)
        ycr = ring('yc', 2, [128, 512], BF16)
        prev = [None, None]
        prevh = [None, None]
        for bi in range(T // 512 if LV > 0 else 0):
            tok0 = bi * 512
            for cc in range(2):
                cx, cxk = cxr[cc].next()
                cy, cyk = cyr.next()
                S.dma('sp', cx[:, 3:515], self.fmf_d[:, 6 + cc, tok0:tok0 + 512], writes=[cxk])
                S.dma('sp', cy[:], self.fmf_d[:, 8 + cc, tok0:tok0 + 512], writes=[cyk])
                if prev[cc] is None:
                    S.op('pool', lambda e, cx=cx: e.memset(cx[:, 0:3], 0.0), [], [cxk])
                else:
                    self.cp('pool', cx[:, 0:3], prev[cc][0][:, 512:515], [prev[cc][1]], [cxk])
                prev[cc] = (cx, cxk)
                xc, xck = xcr.next()
                self.ts('dve', xc[:], cx[:, 3:515], pv[:, 14 + cc:15 + cc], pv[:, 6 + cc:7 + cc], ALU.mult, ALU.add,
                        [cxk, 'pv'], [xck])
                for j in range(3):
                    self.stt(xc[:], cx[:, j:j + 512], pv[:, 8 + 2 * j + cc:9 + 2 * j + cc], xc[:], ALU.mult, ALU.add,
                             [cxk, 'pv', xck], [xck])
                if LV < 2:
                    continue
                xb, xbk = xbr.next()
                self.cp('act', xb[:], xc[:], [xck], [xbk])
                r, rk = rr.next()
                i_, ik = ir.next()
                p1, p1k = psr.next()
                self.mm(p1[:], wa[:, cc, :], xb[:], True, True, ['wa', xbk], [p1k])
                self.act(r[:], p1[:], AF.Sigmoid, [p1k, 'pv'], [rk], bias=pv[:, 16 + cc:17 + cc])
                p2, p2k = psr.next()
                self.mm(p2[:], wx[:, cc, :], xb[:], True, True, ['wx', xbk], [p2k])
                self.act(i_[:], p2[:], AF.Sigmoid, [p2k, 'pv'], [ik], bias=pv[:, 18 + cc:19 + cc])
                a, ak = ar.next()
                a2, a2k = a2r.next()
                self.act(a[:], r[:], AF.Exp, [rk, 'cc'], [ak], scale=cc_[:, 2 + cc:3 + cc])
                self.act(a2[:], r[:], AF.Exp, [rk, 'cc'], [a2k], scale=cc_[:, 4 + cc:5 + cc])
                self.act(a2[:], a2[:], AF.Sqrt, [a2k], [a2k], bias=1.0, scale=-1.0)
                u, uk = ur.next()
                self.tt('dve', u[:], a2[:], i_[:], ALU.mult, [a2k, ik], [uk])
                self.tt('pool', u[:], u[:], xc[:], ALU.mult, [uk, xck], [uk])
                if LV < 3:
                    continue
                hh, hhk = hhr[cc].next()
                init = 0.0 if prevh[cc] is None else prevh[cc][0][:, 511:512]
                rd = [ak, uk] + ([] if prevh[cc] is None else [prevh[cc][1]])
                S.op('dve', lambda e, hh=hh, a=a, u=u, init=init: e.tensor_tensor_scan(
                    out=hh[:], data0=a[:], data1=u[:], initial=init, op0=ALU.mult, op1=ALU.add), rd, [hhk])
                prevh[cc] = (hh, hhk)
                if LV < 4:
                    continue
                g, gk = gr.next()
                g2, g2k = g2r.next()
                self.tt('pool', g[:], cy[:], cy[:], ALU.mult, [cyk], [gk])
                self.ts('dve', g[:], g[:], 0.044715, 1.0, ALU.mult, ALU.add, [gk], [gk])
                self.tt('pool', g[:], g[:], cy[:], ALU.mult, [gk, cyk], [gk])
                self.act(g2[:], g[:], AF.Sigmoid, [gk], [g2k], scale=1.5957691216057308)
                self.tt('dve', g2[:], g2[:], cy[:], ALU.mult, [g2k, cyk], [g2k])
                yc, yck = ycr.next()
                self.tt('pool', yc[:], g2[:], hh[:], ALU.mult, [g2k, hhk], [yck])
                S.dma('sp', self.yT_d[:, 4 + cc, tok0:tok0 + 512], yc[:], reads=[yck], defer=True)
                yield
        yield


Prog.phase_lru = phase_lru


def phase_merge(self, l, x_src):
    nc, S, T, NT = self.nc, self.S, self.T, self.NT
    W = self.w[l]
    with ExitStack() as es:
        sb, pt, ring = _phase_tools(self, es)
        wg = sb('wg', [128, 8, 4096], BF16)
        wbr = sb('wbr', [128, 8, 1024], BF16)
        wo = sb('wo', [128, 8, 1024], BF16)
        bgb = sb('bgb', [128, 4096], F32)
        stage = ring('stg', 2, [128, 2048], F32)
        S.dma('sp', bgb[:], W['bvec'][:, BV_BG:BV_BG + 4096].partition_broadcast(128), writes=['bgb'])
        for k in range(8):
            for c0 in (0, 2048):
                self.load_cast(stage, wg[:, k, c0:c0 + 2048], ('wg', k), W['w_gate'][k * 128:(k + 1) * 128, c0:c0 + 2048], [2048])
            self.load_cast(stage, wo[:, k, :], ('wo', k), W['w_out'][k * 128:(k + 1) * 128, :], [1024])
        for b_, nm in enumerate(('w_br_a', 'w_br_b', 'w_br_c', 'w_br_d')):
            for kk in range(2):
                self.load_cast(stage, wbr[:, b_ * 2 + kk, :], ('wbr', b_), W[nm][kk * 128:(kk + 1) * 128, :], [1024])
        hr = ring('hT', 2, [128, 8, 128], BF16)
        yr = ring('yT', 2, [128, 8, 128], BF16)
        xr = ring('x', 3, [128, 1024], F32)
        mr = ring('m', 2, [128, 1024], F32)
        mbr = ring('mb', 3, [128, 1024], BF16)
        mTr = ring('mT', 2, [128, 8, 128], BF16)
        gsr = ring('gs', 3, [128, 512], F32)
        tpr = ring('tp', 2, [128, 512], F32)
        xor_ = ring('xo', 2, [128, 1024], F32)
        pgr = ring('pg', 3, [128, 512], F32, psum=True)
        pyr = ring('py', 2, [128, 512], F32, psum=True)
        ptr = ring('pt', 1, [128, 8, 128], BF16, psum=True)
        def stage_a(ti):
            t0 = ti * 128
            hT, hTk = hr.next()
            yT, yTk = yr.next()
            x, xk = xr.next()
            S.dma('sp', hT[:], self.hT_d[:, :, t0:t0 + 128], writes=[hTk])
            S.dma('sp', yT[:], self.yT_d[:, :, t0:t0 + 128], writes=[yTk])
            S.dma('sp', x[:], x_src[t0:t0 + 128, :], writes=[xk])
            m, mk = mr.next()
            for b_ in range(4):
                for cb in range(2):
                    c0 = b_ * 1024 + cb * 512
                    pg, pgk = pgr.next()
                    for k in range(8):
                        self.mm(pg[:], hT[:, k, :], wg[:, k, c0:c0 + 512], k == 0, k == 7, [hTk, ('wg', k)], [pgk])
                    py, pyk = pyr.next()
                    for kk in range(2):
                        self.mm(py[:], yT[:, b_ * 2 + kk, :], wbr[:, b_ * 2 + kk, cb * 512:(cb + 1) * 512], kk == 0, kk == 1,
                                [yTk, ('wbr', b_)], [pyk])
                    gs, gsk = gsr.next()
                    self.tt('dve', gs[:], pg[:], bgb[:, c0:c0 + 512], ALU.add, [pgk, 'bgb'], [gsk])
                    self.act(gs[:], gs[:], AF.Sigmoid, [gsk], [gsk])
                    ms = m[:, cb * 512:(cb + 1) * 512]
                    if b_ == 0:
                        self.tt('dve', ms, gs[:], py[:], ALU.mult, [gsk, pyk], [(mk, cb)])
                    else:
                        tp, tpk = tpr.next()
                        self.tt('dve', tp[:], gs[:], py[:], ALU.mult, [gsk, pyk], [tpk])
                        self.tt('pool', ms, ms, tp[:], ALU.add, [(mk, cb), tpk], [(mk, cb)])
            mb, mbk = mbr.next()
            self.cp('act', mb[:], m[:], [(mk, 0), (mk, 1)], [mbk])
            return (t0, x, xk, mb, mbk)

        def stage_b(st):
            t0, x, xk, mb, mbk = st
            p, pk = ptr.next()
            for k in range(8):
                self.tr(p[:, k, :], mb[:, k * 128:(k + 1) * 128], self.identb[:], [mbk, 'identb'], [pk])
            mT, mTk = mTr.next()
            self.cp('act', mT[:], p[:], [pk], [mTk])
            xo, xok = xor_.next()
            for cb in range(2):
                po, pok = pgr.next()
                for k in range(8):
                    self.mm(po[:], mT[:, k, :], wo[:, k, cb * 512:(cb + 1) * 512], k == 0, k == 7, [mTk, ('wo', k)], [pok])
                self.tt('dve', xo[:, cb * 512:(cb + 1) * 512], x[:, cb * 512:(cb + 1) * 512], po[:], ALU.add,
                        [xk, pok], [xok])
            S.dma('sp', self.xmid_d[t0:t0 + 128, :], xo[:], reads=[xok], defer=True)

        prev = None
        for ti in range(NT):
            cur = stage_a(ti)
            if prev is not None:
                stage_b(prev)
            prev = cur
        stage_b(prev)
        S.barrier()


Prog.phase_merge = phase_merge


def phase_ffn(self, l, x_dst, final):
    nc, S, T = self.nc, self.S, self.T
    W = self.w[l]
    NS = 256
    with ExitStack() as es:
        sb, pt, ring = _phase_tools(self, es)
        wf1 = sb('wf1', [128, 8, 4096], BF16)
        wf2 = sb('wf2', [128, 32, 1024], BF16)
        g2b = sb('g2b', [128, 1024], F32)
        gfb = sb('gfb', [128, 1024], F32)
        stage = ring('stg', 2, [128, 1024], F32)
        S.dma('sp', g2b[:], W['bvec'][:, BV_N2:BV_N2 + 1024].partition_broadcast(128), writes=['g2b'])
        S.dma('sp', gfb[:], W['bvec'][:, BV_FN:BV_FN + 1024].partition_broadcast(128), writes=['gfb'])
        for k in range(8):
            for c0 in range(0, 4096, 1024):
                self.load_cast(stage, wf1[:, k, c0:c0 + 1024], ('wf1', k), W['w_ff1'][k * 128:(k + 1) * 128, c0:c0 + 1024], [1024])
        for f in range(32):
            self.load_cast(stage, wf2[:, f, :], ('wf2', f), W['w_ff2'][f * 128:(f + 1) * 128, :], [1024])
        xr = ring('xm', 4, [128, 1024], F32)
        junk = sb('junk', [128, 1024], F32)
        hr = ring('h2', 2, [128, 1024], BF16)
        hTr = ring('h2T', 2, [128, 8, NS], BF16)
        uTr = ring('uT', 1, [128, 32, NS], BF16)
        sqr = ring('sq', 2, [128, NS], F32)
        st1 = ring('st', 4, [128, 4], F32)
        xor_ = ring('xo', 2, [128, 1024], F32)
        ptr = ring('pt', 2, [128, 8, 128], BF16, psum=True)
        pur = ring('pu', 3, [128, 512], F32, psum=True)
        por = ring('po', 3, [128, 512], F32, psum=True)
        def stage_a(si):
            tok0 = si * NS
            hT, hTk = hTr.next()
            xs = []
            for j in range(NS // 128):
                t0 = tok0 + j * 128
                x, xk = xr.next()
                xs.append((x, xk))
                S.dma('sp', x[:], self.xmid_d[t0:t0 + 128, :], writes=[xk])
                s1, s1k = st1.next()
                self.act(junk[:], x[:], AF.Square, [xk], ['junk', s1k], accum=s1[:, 0:1])
                self.act(s1[:, 1:2], s1[:, 0:1], AF.Sqrt, [s1k], [s1k], bias=EPS, scale=1.0 / 1024)
                S.op('dve', lambda e, s1=s1: e.reciprocal(out=s1[:, 2:3], in_=s1[:, 1:2]), [s1k], [s1k])
                h, hk = hr.next()
                self.stt(h[:], x[:], s1[:, 2:3], g2b[:], ALU.mult, ALU.mult, [xk, s1k, 'g2b'], [hk])
                p, pk = ptr.next()
                for k in range(8):
                    self.tr(p[:, k, :], h[:, k * 128:(k + 1) * 128], self.identb[:], [hk, 'identb'], [pk])
                self.cp('act', hT[:, :, j * 128:(j + 1) * 128], p[:], [pk], [hTk])
            return (tok0, hT, hTk, xs)

        def ff1(st):
            tok0, hT, hTk, xs = st
            uT, uTk = uTr.next()
            for f in range(32):
                pu, puk = pur.next()
                for k in range(8):
                    self.mm(pu[:, 0:NS], wf1[:, k, f * 128:(f + 1) * 128], hT[:, k, :], k == 0, k == 7,
                            [hTk, ('wf1', k)], [puk])
                sq, sqk = sqr.next()
                self.act(sq[:], pu[:, 0:NS], AF.Square, [puk], [sqk])
                self.stt(uT[:, f, :], pu[:, 0:NS], 0.0, sq[:], ALU.is_gt, ALU.mult, [puk, sqk], [(uTk, f)])
            return uT, uTk

        def ff2(st, uT, uTk):
            tok0, hT, hTk, xs = st
            for j in range(NS // 128):
                t0 = tok0 + j * 128
                x, xk = xs[j]
                xo, xok = xor_.next()
                for cb in range(2):
                    po, pok = por.next()
                    for f in range(32):
                        self.mm(po[:], uT[:, f, j * 128:(j + 1) * 128], wf2[:, f, cb * 512:(cb + 1) * 512], f == 0, f == 31,
                                [(uTk, f), ('wf2', f)], [pok])
                    self.tt('dve', xo[:, cb * 512:(cb + 1) * 512], x[:, cb * 512:(cb + 1) * 512], po[:], ALU.add,
                            [xk, pok], [xok])
                if final:
                    s1, s1k = st1.next()
                    self.act(junk[:], xo[:], AF.Square, [xok], ['junk', s1k], accum=s1[:, 0:1])
                    self.act(s1[:, 1:2], s1[:, 0:1], AF.Sqrt, [s1k], [s1k], bias=EPS, scale=1.0 / 1024)
                    S.op('dve', lambda e, s1=s1: e.reciprocal(out=s1[:, 2:3], in_=s1[:, 1:2]), [s1k], [s1k])
                    self.stt(xo[:], xo[:], s1[:, 2:3], gfb[:], ALU.mult, ALU.mult, [xok, s1k, 'gfb'], [xok])
                S.dma('sp', x_dst[t0:t0 + 128, :], xo[:], reads=[xok], defer=True)

        nS = T // NS
        cur = stage_a(0)
        for si in range(nS):
            uT, uTk = ff1(cur)
            nxt = stage_a(si + 1) if si + 1 < nS else None
            ff2(cur, uT, uTk)
            cur = nxt
        S.barrier()


Prog.phase_ffn = phase_ffn


_CACHE = {}


def kernel(**inputs):
    inp = {k: np.asarray(v) for k, v in inputs.items()}
    B, T, D = inp['x'].shape
    P = Prog(T, depth=DEPTH)
    nc = P.build()
    in_maps = [make_in_map(inp, b) for b in range(B)]
    res = run_bass_kernel_spmd(nc, in_maps, core_ids=list(range(B)))
    out = np.stack([np.asarray(r['out'], dtype=np.float32) for r in res.results], axis=0)
    return out
```
